# Optimizing a Trainium2 kernel written in Bass

```python
import jax, jax.numpy as jnp
from jax import lax
import numpy as np

D_MODEL = 2048
BATCH = 1
SEQ = 8192
DEPTH = 2

N_META = 16
Q_BLOCK = 128
MLA_HEADS = 8
MLA_Q_LORA = 512
MLA_KV_LORA = 512
MLA_NOPE_DIM = 128
MLA_ROPE_DIM = 64
MLA_V_DIM = 128
ROPE_THETA = 10000.0
FOX_HEADS = 8
FOX_HEAD_DIM = 128
_ATTN_SIZES = (MLA_Q_LORA, MLA_KV_LORA, MLA_ROPE_DIM,
               FOX_HEADS * FOX_HEAD_DIM, FOX_HEADS * FOX_HEAD_DIM, FOX_HEADS * FOX_HEAD_DIM,
               FOX_HEADS)
ATTN_IN = int(sum(_ATTN_SIZES))
ATTN_SPLITS = tuple(int(v) for v in np.cumsum(_ATTN_SIZES)[:-1])
ATTN_OUT = MLA_HEADS * MLA_V_DIM + FOX_HEADS * FOX_HEAD_DIM
POOL_WINDOWS = (2, 4, 8, 16)
POOL_WIDTH = D_MODEL
POOL_GROUP = POOL_WIDTH // len(POOL_WINDOWS)
FFN_DENSE = 5632
N_EXPERTS = 8
TOP_K = 2
FFN_EXPERT = 7168
MOE_BLOCK = 128
ALPHA = (2 * DEPTH) ** 0.25
BETA = (8 * DEPTH) ** -0.25
LN_EPS = 1e-5
RMS_EPS = 1e-6
NEG_INF = -1e30
N_EVEN = (DEPTH + 1) // 2
N_ODD = DEPTH // 2

kernel_name = "hybrid_mla_fox_pool_moe_deepnorm"


def layer_norm(x, g, b):
    xf = x.astype(jnp.float32)
    mu = jnp.mean(xf, axis=-1, keepdims=True)
    var = jnp.mean(jnp.square(xf - mu), axis=-1, keepdims=True)
    return ((xf - mu) * lax.rsqrt(var + LN_EPS) * g + b).astype(x.dtype)


def rms_norm(x, g):
    xf = x.astype(jnp.float32)
    return (xf * lax.rsqrt(jnp.mean(jnp.square(xf), axis=-1, keepdims=True) + RMS_EPS) * g).astype(x.dtype)


def rope(x, cos, sin):
    x1, x2 = jnp.split(x, 2, axis=-1)
    return jnp.concatenate([x1 * cos - x2 * sin, x2 * cos + x1 * sin], axis=-1)


def causal_block_attention(q, k, v, scale, log_decay_cum=None):
    B, H, L, dk = q.shape
    dv = v.shape[-1]
    lead = (-N_META) % Q_BLOCK
    lq = lead + L
    nb = -(-lq // Q_BLOCK)
    tail = nb * Q_BLOCK - lq
    qp = jnp.pad(q, ((0, 0), (0, 0), (lead, tail), (0, 0)))
    q_blocks = qp.reshape(B, H, nb, Q_BLOCK, dk).transpose(2, 0, 1, 3, 4)
    q_pos = (jnp.arange(nb * Q_BLOCK) - lead).reshape(nb, Q_BLOCK)
    k_pos = jnp.arange(L)
    xs = (q_blocks, q_pos)
    if log_decay_cum is not None:
        cp = jnp.pad(log_decay_cum, ((0, 0), (0, 0), (lead, tail)))
        xs = xs + (cp.reshape(B, H, nb, Q_BLOCK).transpose(2, 0, 1, 3),)
        k_is_meta = (k_pos < N_META)[None, None, None, :]

    def one_block(blk):
        qb, pos = blk[0], blk[1]
        s = jnp.einsum('bhtd,bhsd->bhts', qb, k).astype(jnp.float32) * scale
        if log_decay_cum is not None:
            decay = blk[2][..., :, None] - log_decay_cum[:, :, None, :]
            s = s + jnp.where(k_is_meta, 0.0, decay)
        mask = k_pos[None, :] <= pos[:, None]
        s = jnp.where(mask, s, NEG_INF)
        p = jax.nn.softmax(s, axis=-1).astype(v.dtype)
        return jnp.einsum('bhts,bhsd->bhtd', p, v)

    o = lax.map(one_block, xs)
    o = o.transpose(1, 2, 0, 3, 4).reshape(B, H, nb * Q_BLOCK, dv)
    return o[:, :, lead:lead + L]


def mla_fox_mixer(x, w_in, fox_b_f, q_norm, kv_norm, w_uq, w_ukv, w_out, cos, sin):
    B, L, _ = x.shape
    h = x @ w_in
    c_q, c_kv, k_r, q_f, k_f, v_f, f_logit = jnp.split(h, ATTN_SPLITS, axis=-1)
    q = (rms_norm(c_q, q_norm) @ w_uq).reshape(B, L, MLA_HEADS, MLA_NOPE_DIM + MLA_ROPE_DIM)
    q_nope, q_rot = q[..., :MLA_NOPE_DIM], q[..., MLA_NOPE_DIM:]
    q_rot = rope(q_rot, cos[:, None, :], sin[:, None, :])
    kv = (rms_norm(c_kv, kv_norm) @ w_ukv).reshape(B, L, MLA_HEADS, MLA_NOPE_DIM + MLA_V_DIM)
    k_nope, v_m = kv[..., :MLA_NOPE_DIM], kv[..., MLA_NOPE_DIM:]
    k_rot = rope(k_r, cos, sin)
    k_rot = jnp.broadcast_to(k_rot[:, :, None, :], (B, L, MLA_HEADS, MLA_ROPE_DIM))
    q_m = jnp.concatenate([q_nope, q_rot], axis=-1).transpose(0, 2, 1, 3)
    k_m = jnp.concatenate([k_nope, k_rot], axis=-1).transpose(0, 2, 1, 3)
    o_mla = causal_block_attention(q_m, k_m, v_m.transpose(0, 2, 1, 3),
                                   (MLA_NOPE_DIM + MLA_ROPE_DIM) ** -0.5)
    to_heads = lambda t: t.reshape(B, L, FOX_HEADS, FOX_HEAD_DIM).transpose(0, 2, 1, 3)
    log_f = jax.nn.log_sigmoid(f_logit.astype(jnp.float32) + fox_b_f)
    cum = jnp.cumsum(log_f, axis=1).transpose(0, 2, 1)
    o_fox = causal_block_attention(to_heads(q_f), to_heads(k_f), to_heads(v_f),
                                   FOX_HEAD_DIM ** -0.5, cum)
    o = jnp.concatenate([o_mla, o_fox], axis=1).transpose(0, 2, 1, 3).reshape(B, L, ATTN_OUT)
    return o @ w_out


def pool_mixer(x, w_in, w_group, scale, w_out):
    B, L, _ = x.shape
    h = (x @ w_in).reshape(B, L, len(POOL_WINDOWS), POOL_GROUP)
    count = jnp.arange(1, L + 1, dtype=jnp.float32)
    outs = []
    for g, w in enumerate(POOL_WINDOWS):
        hg = h[:, :, g].astype(jnp.float32)
        cs = jnp.cumsum(hg, axis=1)
        lower = jnp.pad(cs[:, :L - w], ((0, 0), (w, 0), (0, 0)))
        mean = (cs - lower) / jnp.minimum(count, float(w))[None, :, None]
        outs.append((mean - hg).astype(x.dtype) @ w_group[g])
    y = jnp.concatenate(outs, axis=-1) * scale
    return y @ w_out


def swiglu(x, w_gate, w_up, w_down):
    return (jax.nn.silu(x @ w_gate) * (x @ w_up)) @ w_down


def moe_swiglu(x, w_router, b_router, w_gate, w_up, w_down):
    B, L, D = x.shape
    n = B * L
    xt = x.reshape(n, D)
    logits = (xt @ w_router).astype(jnp.float32) + b_router
    top_val, top_idx = lax.top_k(logits, TOP_K)
    gates = jax.nn.softmax(top_val, axis=-1)
    a = n * TOP_K
    expert_of = top_idx.reshape(a)
    token_of = jnp.arange(a, dtype=jnp.int32) // TOP_K
    gate_of = gates.reshape(a)
    order = jnp.argsort(expert_of)
    e_sorted = expert_of[order]
    counts = jnp.bincount(expert_of, length=N_EXPERTS)
    padded = (counts + MOE_BLOCK - 1) // MOE_BLOCK * MOE_BLOCK
    seg_end = jnp.cumsum(padded)
    seg_start = seg_end - padded
    first = jnp.cumsum(counts) - counts
    dest = seg_start[e_sorted] + jnp.arange(a) - first[e_sorted]
    n_blocks = -(-a // MOE_BLOCK) + N_EXPERTS
    cap = n_blocks * MOE_BLOCK
    row_token = jnp.full((cap,), n, jnp.int32).at[dest].set(token_of[order])
    row_gate = jnp.zeros((cap,), jnp.float32).at[dest].set(gate_of[order])
    block_expert = jnp.minimum(
        jnp.searchsorted(seg_end, jnp.arange(n_blocks) * MOE_BLOCK, side='right'), N_EXPERTS - 1)
    x_rows = jnp.concatenate([xt, jnp.zeros((1, D), xt.dtype)], axis=0)[row_token]
    x_rows = x_rows.reshape(n_blocks, MOE_BLOCK, D)

    def expert_block(args):
        xb, e = args
        return (jax.nn.silu(xb @ w_gate[e]) * (xb @ w_up[e])) @ w_down[e]

    y_rows = lax.map(expert_block, (x_rows, block_expert)).reshape(cap, D)
    y = jax.ops.segment_sum(y_rows.astype(jnp.float32) * row_gate[:, None], row_token,
                            num_segments=n + 1)[:n]
    return y.astype(x.dtype).reshape(B, L, D)


def setup_inputs(seed: int = 0) -> dict:
    key = jax.random.key(seed)
    ks = iter(jax.random.split(key, 32))

    def nrm(shape, scale):
        return jax.random.normal(next(ks), shape, jnp.float32) * scale

    D = D_MODEL
    return {
        "x": nrm((BATCH, SEQ, D), 1.0),
        "meta_tokens": nrm((N_META, D), 1.0),
        "attn_w_in": nrm((N_EVEN, D, ATTN_IN), D ** -0.5),
        "fox_forget_bias": 3.0 + nrm((N_EVEN, FOX_HEADS), 0.5),
        "mla_q_norm": 1.0 + nrm((N_EVEN, MLA_Q_LORA), 0.1),
        "mla_kv_norm": 1.0 + nrm((N_EVEN, MLA_KV_LORA), 0.1),
        "mla_w_uq": nrm((N_EVEN, MLA_Q_LORA, MLA_HEADS * (MLA_NOPE_DIM + MLA_ROPE_DIM)), MLA_Q_LORA ** -0.5),
        "mla_w_ukv": nrm((N_EVEN, MLA_KV_LORA, MLA_HEADS * (MLA_NOPE_DIM + MLA_V_DIM)), MLA_KV_LORA ** -0.5),
        "attn_w_out": nrm((N_EVEN, ATTN_OUT, D), BETA * ATTN_OUT ** -0.5),
        "pool_w_in": nrm((N_ODD, D, POOL_WIDTH), D ** -0.5),
        "pool_w_group": nrm((N_ODD, len(POOL_WINDOWS), POOL_GROUP, POOL_GROUP), POOL_GROUP ** -0.5),
        "pool_scale": 1.0 + nrm((N_ODD, POOL_WIDTH), 0.1),
        "pool_w_out": nrm((N_ODD, POOL_WIDTH, D), BETA * POOL_WIDTH ** -0.5),
        "ffn_w_gate": nrm((N_EVEN, D, FFN_DENSE), D ** -0.5),
        "ffn_w_up": nrm((N_EVEN, D, FFN_DENSE), D ** -0.5),
        "ffn_w_down": nrm((N_EVEN, FFN_DENSE, D), BETA * FFN_DENSE ** -0.5),
        "moe_w_router": nrm((N_ODD, D, N_EXPERTS), D ** -0.5),
        "moe_b_router": nrm((N_ODD, N_EXPERTS), 0.01),
        "moe_w_gate": nrm((N_ODD, N_EXPERTS, D, FFN_EXPERT), D ** -0.5),
        "moe_w_up": nrm((N_ODD, N_EXPERTS, D, FFN_EXPERT), D ** -0.5),
        "moe_w_down": nrm((N_ODD, N_EXPERTS, FFN_EXPERT, D), BETA * FFN_EXPERT ** -0.5),
        "ln_mix_g": 1.0 + nrm((DEPTH, D), 0.1),
        "ln_mix_b": nrm((DEPTH, D), 0.02),
        "ln_ffn_g": 1.0 + nrm((DEPTH, D), 0.1),
        "ln_ffn_b": nrm((DEPTH, D), 0.02),
    }


def reference(x, meta_tokens, attn_w_in, fox_forget_bias, mla_q_norm, mla_kv_norm, mla_w_uq,
              mla_w_ukv, attn_w_out, pool_w_in, pool_w_group, pool_scale, pool_w_out,
              ffn_w_gate, ffn_w_up, ffn_w_down, moe_w_router, moe_b_router, moe_w_gate,
              moe_w_up, moe_w_down, ln_mix_g, ln_mix_b, ln_ffn_g, ln_ffn_b):
    B = x.shape[0]
    meta = jnp.broadcast_to(meta_tokens[None].astype(x.dtype), (B, N_META, D_MODEL))
    h = jnp.concatenate([meta, x], axis=1)
    L = h.shape[1]
    pos = jnp.arange(L, dtype=jnp.float32)
    inv_freq = ROPE_THETA ** (-jnp.arange(0, MLA_ROPE_DIM, 2, dtype=jnp.float32) / MLA_ROPE_DIM)
    ang = pos[:, None] * inv_freq[None, :]
    cos = jnp.cos(ang).astype(x.dtype)
    sin = jnp.sin(ang).astype(x.dtype)
    for i in range(DEPTH):
        j = i // 2
        if i % 2 == 0:
            m = mla_fox_mixer(h, attn_w_in[j], fox_forget_bias[j], mla_q_norm[j], mla_kv_norm[j],
                              mla_w_uq[j], mla_w_ukv[j], attn_w_out[j], cos, sin)
        else:
            m = pool_mixer(h, pool_w_in[j], pool_w_group[j], pool_scale[j], pool_w_out[j])
        h = layer_norm(ALPHA * h + m, ln_mix_g[i], ln_mix_b[i])
        if i % 2 == 0:
            f = swiglu(h, ffn_w_gate[j], ffn_w_up[j], ffn_w_down[j])
        else:
            f = moe_swiglu(h, moe_w_router[j], moe_b_router[j], moe_w_gate[j], moe_w_up[j], moe_w_down[j])
        h = layer_norm(ALPHA * h + f, ln_ffn_g[i], ln_ffn_b[i])
    return h[:, N_META:]
```

```python
from contextlib import ExitStack
import numpy as np
import concourse.bass as bass
import concourse.mybir as mybir
from concourse.bass_utils import run_bass_kernel_spmd

F32 = mybir.dt.float32
BF16 = mybir.dt.bfloat16
AF = mybir.ActivationFunctionType
ALU = mybir.AluOpType
AX = mybir.AxisListType

D = 2048
DC = 16
NCORE = 8
NOWN = 1024
NHALO = 16
NT = NOWN + NHALO
LFULL = 8192 + 16
NOTH = LFULL - NT
ALPHA = 4 ** 0.25
LN_EPS = 1e-5
RMS_EPS = 1e-6
FFN_DENSE = 5632
FFN_EXPERT = 7168
NEXP = 8
NEG = -1e30


class EngW:
    def __init__(self, m, e, name):
        self.m = m
        self.e = e
        self.name = name
        self.sem = m.es.enter_context(m.nc.semaphore("s_" + name))
        self.n = 0
        self.seen = {}

    def wait(self, toks):
        best = {}
        for t in toks:
            if t is None:
                continue
            sem, val = t
            if best.get(sem, 0) < val:
                best[sem] = val
        for sem, val in best.items():
            if self.seen.get(sem, 0) >= val:
                continue
            self.e.wait_ge(sem, val)
            self.seen[sem] = val

    def done(self, ins):
        self.n += 1
        ins.then_inc(self.sem, 1)
        return (self.sem, self.n)


class Buf:
    __slots__ = ("w", "r", "name")

    def __init__(self, name=""):
        self.w = None
        self.r = {}
        self.name = name

    def add_read(self, tok):
        sem, val = tok
        if self.r.get(sem, 0) < val:
            self.r[sem] = val


class MK:
    def __init__(self):
        self.es = ExitStack()
        self.nc = bass.Bass("TRN2", target_bir_lowering=False)
        nc = self.nc
        self.PE = EngW(self, nc.tensor, "pe")
        self.ACT = EngW(self, nc.scalar, "act")
        self.DVE = EngW(self, nc.vector, "dve")
        self.POOL = EngW(self, nc.gpsimd, "pool")
        self.SP = EngW(self, nc.sync, "sp")
        self.engs = [self.PE, self.ACT, self.DVE, self.POOL, self.SP]
        self.dsem = {}
        for q, n in ((self.SP, 20), (self.POOL, 20), (self.ACT, 8)):
            self.dsem[q.name] = [[self.es.enter_context(nc.semaphore("d_%s%d" % (q.name, i))), 0] for i in range(n)]
        self.dptr = {"sp": 0, "pool": 0, "act": 0}
        self.bufs = []

    def buf(self, name=""):
        b = Buf(name)
        self.bufs.append(b)
        return b

    def sb(self, name, shape, dt):
        return self.es.enter_context(self.nc.sbuf_tensor(name, shape, dt))

    def _deps(self, reads, writes):
        toks = []
        for b in reads:
            toks.append(b.w)
        for b in writes:
            toks.append(b.w)
            for sem, val in b.r.items():
                toks.append((sem, val))
        return toks

    def _commit(self, tok, reads, writes):
        for b in reads:
            b.add_read(tok)
        for b in writes:
            b.w = tok
            b.r = {}

    def op(self, eng, fn, reads=(), writes=()):
        eng.wait(self._deps(reads, writes))
        ins = fn()
        tok = eng.done(ins)
        self._commit(tok, reads, writes)
        return tok

    def dma(self, q, out, in_, reads=(), writes=()):
        lst = self.dsem[q.name]
        i = self.dptr[q.name]
        self.dptr[q.name] = (i + 1) % len(lst)
        sem, tot = lst[i]
        deps = self._deps(reads, writes)
        if tot > 0:
            deps.append((sem, tot))
        q.wait(deps)
        q.e.dma_start(out=out, in_=in_).then_inc(sem, 16)
        lst[i][1] = tot + 16
        tok = (sem, tot + 16)
        self._commit(tok, reads, writes)
        return tok

    def all_tokens(self):
        toks = []
        for e in self.engs:
            if e.n > 0:
                toks.append((e.sem, e.n))
        for name, lst in self.dsem.items():
            for sem, tot in lst:
                if tot > 0:
                    toks.append((sem, tot))
        return toks

    def barrier(self):
        toks = self.all_tokens()
        for e in self.engs:
            e.wait(toks)
        for b in self.bufs:
            b.w = None
            b.r = {}
        self.bufs = []


def tok_blocks(ntok):
    out = []
    r = ntok % 128
    t = 0
    if r:
        out.append((0, r))
        t = r
    while t < ntok:
        out.append((t, 128))
        t += 128
    return out


def tok_groups(ntok, maxn=512):
    ng = -(-ntok // maxn)
    base = -(-ntok // ng)
    out = []
    t = 0
    while t < ntok:
        n = min(base, ntok - t)
        out.append((t, n))
        t += n
    return out


class Ctx:
    pass


def make_identity(m, ident_bf, ident_f32=None):
    nc = m.nc
    b = m.buf("ident")
    def f():
        nc.gpsimd.memset(ident_bf[:], 1.0)
        return nc.gpsimd.affine_select(out=ident_bf[:], in_=ident_bf[:], pattern=[[-1, 128]],
                                       compare_op=ALU.is_equal, fill=0.0, base=0, channel_multiplier=1)
    m.op(m.POOL, f, writes=[b])
    b2 = None
    if ident_f32 is not None:
        b2 = m.buf("identf")
        def g():
            nc.gpsimd.memset(ident_f32[:], 1.0)
            return nc.gpsimd.affine_select(out=ident_f32[:], in_=ident_f32[:], pattern=[[-1, 128]],
                                           compare_op=ALU.is_equal, fill=0.0, base=0, channel_multiplier=1)
        m.op(m.POOL, g, writes=[b2])
    return b, b2


def load_fm(m, H_dram, row0, ntok, hT, hT_buf, ident, ident_buf, stage, stage_bufs, ps_t, ps_bufs, Hbuf=None, q=None):
    nc = m.nc
    k = 0
    first = True
    for bi, (t0, n) in enumerate(tok_blocks(ntok)):
        st, sb_ = stage[bi % 2], stage_bufs[bi % 2]
        m.dma(q or m.POOL, st[:n, :], H_dram[row0 + t0: row0 + t0 + n, :], reads=([Hbuf] if Hbuf else []), writes=[sb_])
        for half in range(2):
            ps, pb = ps_t[k % 2], ps_bufs[k % 2]
            k += 1
            def f():
                ins = None
                for j in range(8):
                    c = half * 8 + j
                    ins = nc.tensor.transpose(ps[:, j * 128: j * 128 + n], st[:n, c * 128:(c + 1) * 128], ident[:n, :n])
                return ins
            m.op(m.PE, f, reads=[sb_, ident_buf], writes=[pb])
            src = ps[:, :].rearrange("p (j t) -> p j t", j=8)[:, :, :n]
            dst = hT[:, half * 8:(half + 1) * 8, t0:t0 + n]
            if k % 2 == 0:
                m.op(m.DVE, lambda: nc.vector.tensor_copy(out=dst, in_=src), reads=[pb], writes=[hT_buf] if first else [hT_buf])
            else:
                m.op(m.ACT, lambda: nc.scalar.copy(out=dst, in_=src), reads=[pb], writes=[hT_buf])
            first = False


def layer_norm_block(m, cx, src_ps, src_bufs, Hin_dram, row, n, Hout_dram, gam, bet, out_row=None):
    nc = m.nc
    i = cx.ln_i
    cx.ln_i += 1
    hres, hb = cx.hres[i % 2], cx.hres_bufs[i % 2]
    tln, tb = cx.tln[i % 2], cx.tln_bufs[i % 2]
    st, sbf = cx.stat[i % 2], cx.stat_bufs[i % 2]
    m.dma(m.SP, hres[:n, :], Hin_dram[row:row + n, :], reads=cx.Hin_bufs, writes=[hb])
    m.op(m.DVE, lambda: nc.vector.scalar_tensor_tensor(out=tln[:n, :], in0=hres[:n, :], scalar=ALPHA, in1=src_ps,
                                                       op0=ALU.mult, op1=ALU.add),
         reads=[hb] + list(src_bufs), writes=[tb])
    def fstats():
        ins = None
        for j in range(4):
            ins = nc.vector.bn_stats(out=st[:n, j * 6:(j + 1) * 6], in_=tln[:n, j * 512:(j + 1) * 512])
        return ins
    m.op(m.DVE, fstats, reads=[tb], writes=[sbf])
    m.op(m.DVE, lambda: nc.vector.bn_aggr(out=st[:n, 24:26], in_=st[:n, 0:24]), reads=[sbf], writes=[sbf])
    m.op(m.DVE, lambda: nc.vector.tensor_scalar(out=st[:n, 28:29], in0=st[:n, 25:26], scalar1=LN_EPS, scalar2=None,
                                                op0=ALU.add), reads=[sbf], writes=[sbf])
    m.op(m.ACT, lambda: nc.scalar.sqrt(out=st[:n, 29:30], in_=st[:n, 28:29]), reads=[sbf], writes=[sbf])
    m.op(m.DVE, lambda: nc.vector.reciprocal(out=st[:n, 26:27], in_=st[:n, 29:30]), reads=[sbf], writes=[sbf])
    m.op(m.DVE, lambda: nc.vector.scalar_tensor_tensor(out=st[:n, 27:28], in0=st[:n, 24:25], scalar=-1.0, in1=st[:n, 26:27],
                                                       op0=ALU.mult, op1=ALU.mult), reads=[sbf], writes=[sbf])
    m.op(m.ACT, lambda: nc.scalar.activation(out=tln[:n, :], in_=tln[:n, :], func=AF.Identity,
                                             bias=st[:n, 27:28], scale=st[:n, 26:27]), reads=[sbf], writes=[tb])
    m.op(m.POOL, lambda: nc.gpsimd.tensor_tensor(out=tln[:n, :], in0=tln[:n, :], in1=gam[:n, :], op=ALU.mult),
         reads=[cx.gb_buf], writes=[tb])
    m.op(m.POOL, lambda: nc.gpsimd.tensor_tensor(out=tln[:n, :], in0=tln[:n, :], in1=bet[:n, :], op=ALU.add),
         reads=[cx.gb_buf], writes=[tb])
    orow = row if out_row is None else out_row
    m.dma(m.SP, Hout_dram[orow:orow + n, :], tln[:n, :], reads=[tb], writes=[cx.Hout_buf])


def ln_setup(m, cx, es, tag, g_dram, b_dram, Hin_bufs, Hout_buf):
    nc = m.nc
    cx.ln_i = 0
    cx.hres = [es.enter_context(nc.sbuf_tensor("hres%s%d" % (tag, i), [128, D], F32)) for i in range(2)]
    cx.tln = [es.enter_context(nc.sbuf_tensor("tln%s%d" % (tag, i), [128, D], F32)) for i in range(2)]
    cx.stat = [es.enter_context(nc.sbuf_tensor("stat%s%d" % (tag, i), [128, 32], F32)) for i in range(2)]
    cx.hres_bufs = [m.buf() for _ in range(2)]
    cx.tln_bufs = [m.buf() for _ in range(2)]
    cx.stat_bufs = [m.buf() for _ in range(2)]
    cx.gam = es.enter_context(nc.sbuf_tensor("gam" + tag, [128, D], F32))
    cx.bet = es.enter_context(nc.sbuf_tensor("bet" + tag, [128, D], F32))
    cx.gb_buf = m.buf()
    m.dma(m.SP, cx.gam[:, :], g_dram.partition_broadcast(128), writes=[cx.gb_buf])
    m.dma(m.SP, cx.bet[:, :], b_dram.partition_broadcast(128), writes=[cx.gb_buf])
    cx.Hin_bufs = Hin_bufs
    cx.Hout_buf = Hout_buf


def swiglu_phase(m, tag, Hin, Hin_buf, row0, ntok, wg, wu, wd, F, g_dram, b_dram, Hout, Hout_buf,
                 gates=None, n_exp=1):
    nc = m.nc
    FC = 256
    nfc = F // FC
    with ExitStack() as es:
        cx = Ctx()
        blocks = tok_blocks(ntok)
        groups = tok_groups(ntok)
        nb = len(blocks)
        hT = es.enter_context(nc.sbuf_tensor("hT" + tag, [128, DC, ntok], BF16))
        hT_buf = m.buf()
        ident = es.enter_context(nc.sbuf_tensor("ident" + tag, [128, 128], BF16))
        ident_buf, _ = make_identity(m, ident)
        yacc = es.enter_context(nc.sbuf_tensor("yacc" + tag, [128, nb, D], F32))
        yacc_bufs = [m.buf() for _ in range(nb)]
        esw = ExitStack()
        wgt = [esw.enter_context(nc.sbuf_tensor("wg%s%d" % (tag, i), [128, DC, FC], BF16)) for i in range(2)]
        wut = [esw.enter_context(nc.sbuf_tensor("wu%s%d" % (tag, i), [128, DC, FC], BF16)) for i in range(2)]
        wdt = [esw.enter_context(nc.sbuf_tensor("wd%s%d" % (tag, i), [128, FC // 128, D], BF16)) for i in range(2)]
        wg_b = [m.buf() for _ in range(2)]
        wu_b = [m.buf() for _ in range(2)]
        wd_b = [m.buf() for _ in range(2)]
        hid = [esw.enter_context(nc.sbuf_tensor("hid%s%d" % (tag, i), [128, FC // 128, ntok], BF16)) for i in range(2)]
        hid_b = [m.buf() for _ in range(2)]
        gs = [esw.enter_context(nc.sbuf_tensor("gs%s%d" % (tag, i), [128, 512], F32)) for i in range(2)]
        gs_b = [m.buf() for _ in range(2)]
        pg = [es.enter_context(nc.psum_tensor("pg%s%d" % (tag, i), [128, 512], F32)) for i in range(2)]
        pu = [es.enter_context(nc.psum_tensor("pu%s%d" % (tag, i), [128, 512], F32)) for i in range(2)]
        py = [es.enter_context(nc.psum_tensor("py%s%d" % (tag, i), [128, 1024], F32)) for i in range(2)]
        pg_b = [m.buf() for _ in range(2)]
        pu_b = [m.buf() for _ in range(2)]
        py_b = [m.buf() for _ in range(2)]
        gate_tile = None
        if gates is not None:
            gate_tile = esw.enter_context(nc.sbuf_tensor("gatet" + tag, [128, nb, n_exp], F32))
            cx.gate_tile = gate_tile
            cx.pg, cx.pg_b, cx.pu, cx.pu_b = pg, pg_b, pu, pu_b
        with ExitStack() as es2:
            stage = [es2.enter_context(nc.sbuf_tensor("stg%s%d" % (tag, i), [128, D], BF16)) for i in range(2)]
            stage_b = [m.buf() for _ in range(2)]
            pst = [pg[i][:, :].bitcast(BF16) for i in range(2)]
            load_fm(m, Hin, row0, ntok, hT, hT_buf, ident, ident_buf, stage, stage_b, pst, pg_b, Hbuf=Hin_buf)
            m.barrier()
        if gates is not None:
            gates(cx, es, hT, hT_buf, blocks)
        kk = 0
        gi = 0
        for e in range(n_exp):
            wg_e = wg[e] if n_exp > 1 else wg
            wu_e = wu[e] if n_exp > 1 else wu
            wd_e = wd[e] if n_exp > 1 else wd
            wg_v = wg_e.rearrange("(c p) f -> p c f", p=128)
            wu_v = wu_e.rearrange("(c p) f -> p c f", p=128)
            wd_v = wd_e.rearrange("(j p) d -> p j d", p=128)
            for fc in range(nfc):
                s = kk % 2
                kk += 1
                f0 = fc * FC
                for c0 in range(0, DC, 4):
                    m.dma(m.POOL, wgt[s][:, c0:c0 + 4, :], wg_v[:, c0:c0 + 4, f0:f0 + FC], writes=[wg_b[s]])
                    m.dma(m.POOL, wut[s][:, c0:c0 + 4, :], wu_v[:, c0:c0 + 4, f0:f0 + FC], writes=[wu_b[s]])
                m.dma(m.POOL, wdt[s][:, :, :], wd_v[:, f0 // 128: f0 // 128 + FC // 128, :], writes=[wd_b[s]])
                for (t0, n) in groups:
                    for fb in range(FC // 128):
                        p = gi % 2
                        gi += 1
                        def fgate():
                            ins = None
                            for c in range(DC):
                                ins = nc.tensor.matmul(pg[p][:, :n], lhsT=wgt[s][:, c, fb * 128:(fb + 1) * 128],
                                                       rhs=hT[:, c, t0:t0 + n], start=(c == 0), stop=(c == DC - 1))
                            return ins
                        m.op(m.PE, fgate, reads=[wg_b[s], hT_buf], writes=[pg_b[p]])
                        def fup():
                            ins = None
                            for c in range(DC):
                                ins = nc.tensor.matmul(pu[p][:, :n], lhsT=wut[s][:, c, fb * 128:(fb + 1) * 128],
                                                       rhs=hT[:, c, t0:t0 + n], start=(c == 0), stop=(c == DC - 1))
                            return ins
                        m.op(m.PE, fup, reads=[wu_b[s], hT_buf], writes=[pu_b[p]])
                        m.op(m.ACT, lambda: nc.scalar.activation(out=gs[p][:, :n], in_=pg[p][:, :n], func=AF.Silu),
                             reads=[pg_b[p]], writes=[gs_b[p]])
                        m.op(m.DVE, lambda: nc.vector.tensor_tensor(out=hid[s][:, fb, t0:t0 + n], in0=gs[p][:, :n],
                                                                    in1=pu[p][:, :n], op=ALU.mult),
                             reads=[gs_b[p], pu_b[p]], writes=[hid_b[s]])
                for bi, (t0, n) in enumerate(blocks):
                    for half in range(2):
                        p = gi % 2
                        gi += 1
                        def fdown():
                            ins = None
                            for q in range(2):
                                for j in range(FC // 128):
                                    ins = nc.tensor.matmul(py[p][:n, q * 512:(q + 1) * 512], lhsT=hid[s][:, j, t0:t0 + n],
                                                           rhs=wdt[s][:, j, half * 1024 + q * 512: half * 1024 + (q + 1) * 512],
                                                           start=(j == 0), stop=(j == FC // 128 - 1))
                            return ins
                        m.op(m.PE, fdown, reads=[hid_b[s], wd_b[s]], writes=[py_b[p]])
                        ya = yacc[:n, bi, half * 1024:(half + 1) * 1024]
                        if e == 0 and fc == 0:
                            if gate_tile is None:
                                m.op(m.DVE, lambda: nc.vector.tensor_copy(out=ya, in_=py[p][:n, :]),
                                     reads=[py_b[p]], writes=[yacc_bufs[bi]])
                            else:
                                m.op(m.DVE, lambda: nc.vector.tensor_scalar(out=ya, in0=py[p][:n, :],
                                                                            scalar1=gate_tile[:n, bi, e:e + 1], scalar2=None,
                                                                            op0=ALU.mult),
                                     reads=[py_b[p], cx.gate_buf], writes=[yacc_bufs[bi]])
                        else:
                            sc = 1.0 if gate_tile is None else gate_tile[:n, bi, e:e + 1]
                            rd = [py_b[p]] + ([cx.gate_buf] if gate_tile is not None else [])
                            m.op(m.DVE, lambda: nc.vector.scalar_tensor_tensor(out=ya, in0=py[p][:n, :], scalar=sc, in1=ya,
                                                                               op0=ALU.mult, op1=ALU.add),
                                 reads=rd, writes=[yacc_bufs[bi]])
        m.barrier()
        esw.close()
        ln_setup(m, cx, es, tag, g_dram, b_dram, [Hin_buf], Hout_buf)
        for bi, (t0, n) in enumerate(blocks):
            layer_norm_block(m, cx, yacc[:n, bi, :], [yacc_bufs[bi]], Hin, row0 + t0, n, Hout, cx.gam, cx.bet)
        m.barrier()


KGROUPS = [(0, 16)] + [(16 + 512 * i, 512) for i in range(16)]
SC_MLA = 192 ** -0.5
SC_FOX = 128 ** -0.5


def load_w(m, q, tile, wv, c0=0, nchunk=None, step=4, bufs=None):
    nchunk = tile.shape[1] if nchunk is None else nchunk
    for c in range(0, nchunk, step):
        e = min(nchunk, c + step)
        m.dma(q, tile[:, c:e, :], wv[:, c:e, :], writes=bufs)


def xgroup_fm(m, cx, xkv, r0, n, xT, xT_buf):
    nc = m.nc
    blocks = [(0, n)] if n <= 128 else [(i * 128, 128) for i in range(n // 128)]
    st, sb_ = cx.stage[cx.si % 2], cx.stage_b[cx.si % 2]
    cx.si += 1
    if n <= 128:
        m.dma(m.POOL, st[:n, 0, :], xkv[r0:r0 + n, :], writes=[sb_])
    else:
        m.dma(m.POOL, st[:, :n // 128, :], xkv[r0:r0 + n, :].rearrange("(b p) d -> p b d", p=128), writes=[sb_])
    for bi, (t0, bn) in enumerate(blocks):
        for half in range(2):
            k = cx.ti % 2
            cx.ti += 1
            ps, pb = cx.pst[k], cx.pst_b[k]
            def f():
                ins = None
                for j in range(8):
                    c = half * 8 + j
                    ins = nc.tensor.transpose(ps[:, j * 128: j * 128 + bn], st[:bn, bi, c * 128:(c + 1) * 128],
                                              cx.ident[:bn, :bn])
                return ins
            m.op(m.PE, f, reads=[sb_, cx.ident_buf], writes=[pb])
            src = ps[:, :].rearrange("p (j t) -> p j t", j=8)[:, :, :bn]
            dst = xT[:, half * 8:(half + 1) * 8, t0:t0 + bn]
            if k == 0:
                m.op(m.DVE, lambda: nc.vector.tensor_copy(out=dst, in_=src), reads=[pb], writes=[xT_buf])
            else:
                m.op(m.ACT, lambda: nc.scalar.copy(out=dst, in_=src), reads=[pb], writes=[xT_buf])
    return blocks


def rms_norm_tm(m, cx, src_ps, src_buf, n, gam_bc, out_bf, out_buf):
    nc = m.nc
    i = cx.ri % 2
    cx.ri += 1
    sq, sqb = cx.rsq[i], cx.rsq_b[i]
    st, stb = cx.rst[i], cx.rst_b[i]
    m.op(m.ACT, lambda: nc.scalar.activation(out=sq[:n, :], in_=src_ps, func=AF.Square, accum_out=st[:n, 0:1]),
         reads=[src_buf], writes=[sqb, stb])
    m.op(m.DVE, lambda: nc.vector.tensor_scalar(out=st[:n, 1:2], in0=st[:n, 0:1], scalar1=1.0 / 512, scalar2=RMS_EPS,
                                                op0=ALU.mult, op1=ALU.add), reads=[stb], writes=[stb])
    m.op(m.ACT, lambda: nc.scalar.sqrt(out=st[:n, 3:4], in_=st[:n, 1:2]), reads=[stb], writes=[stb])
    m.op(m.DVE, lambda: nc.vector.reciprocal(out=st[:n, 2:3], in_=st[:n, 3:4]), reads=[stb], writes=[stb])
    m.op(m.DVE, lambda: nc.vector.scalar_tensor_tensor(out=out_bf, in0=src_ps, scalar=st[:n, 2:3], in1=gam_bc[:n, :],
                                                       op0=ALU.mult, op1=ALU.mult),
         reads=[src_buf, stb, cx.g_buf], writes=[out_buf])


def transpose_tm_to_fm(m, cx, src_bf, src_buf, n, nchunk, dstT, dst_buf, t0):
    nc = m.nc
    k = cx.ti % 2
    cx.ti += 1
    ps, pb = cx.pst[k], cx.pst_b[k]
    def f():
        ins = None
        for j in range(nchunk):
            ins = nc.tensor.transpose(ps[:, j * 128: j * 128 + n], src_bf[:n, j * 128:(j + 1) * 128], cx.ident[:n, :n])
        return ins
    m.op(m.PE, f, reads=[src_buf, cx.ident_buf], writes=[pb])
    src = ps[:, 0:nchunk * 128].rearrange("p (j t) -> p j t", j=nchunk)[:, :, :n]
    m.op(m.DVE, lambda: nc.vector.tensor_copy(out=dstT[:, 0:nchunk, t0:t0 + n], in_=src), reads=[pb], writes=[dst_buf])


def rope_tm(m, cx, src, src_buf, n, nh, cs, cs_buf, out_bf, out_buf, scale):
    nc = m.nc
    i = cx.rpi % 2
    cx.rpi += 1
    t1, t1b = cx.rp[i], cx.rp_b[i]
    for h in range(nh):
        x1 = src[:, h * 64: h * 64 + 32]
        x2 = src[:, h * 64 + 32: h * 64 + 64]
        cos = cs[:n, 0:32]
        sin = cs[:n, 32:64]
        a = t1[:n, 0:32]
        b = t1[:n, 32:64]
        c_ = t1[:n, 64:96]
        d_ = t1[:n, 96:128]
        def f():
            nc.vector.tensor_tensor(out=a, in0=x1, in1=cos, op=ALU.mult)
            nc.vector.tensor_tensor(out=b, in0=x2, in1=sin, op=ALU.mult)
            nc.vector.tensor_tensor(out=c_, in0=x2, in1=cos, op=ALU.mult)
            return nc.vector.tensor_tensor(out=d_, in0=x1, in1=sin, op=ALU.mult)
        m.op(m.DVE, f, reads=[src_buf, cs_buf], writes=[t1b])
        def g():
            nc.vector.scalar_tensor_tensor(out=out_bf[:n, h * 64: h * 64 + 32], in0=a, scalar=scale, in1=a, op0=ALU.mult, op1=ALU.bypass) if False else None
            nc.vector.tensor_tensor(out=a, in0=a, in1=b, op=ALU.subtract)
            return nc.vector.tensor_tensor(out=c_, in0=c_, in1=d_, op=ALU.add)
        m.op(m.DVE, g, reads=[t1b], writes=[t1b])
        def h2():
            nc.vector.tensor_scalar(out=out_bf[:n, h * 64: h * 64 + 32], in0=a, scalar1=scale, scalar2=None, op0=ALU.mult)
            return nc.vector.tensor_scalar(out=out_bf[:n, h * 64 + 32: h * 64 + 64], in0=c_, scalar1=scale, scalar2=None,
                                           op0=ALU.mult)
        m.op(m.DVE, h2, reads=[t1b], writes=[out_buf])


def kv_common_setup(m, cx, es, tag):
    nc = m.nc
    cx.si = cx.ti = cx.ri = cx.rpi = cx.ei = 0
    cx.ident = es.enter_context(nc.sbuf_tensor("ident" + tag, [128, 128], BF16))
    cx.ident_buf, _ = make_identity(m, cx.ident)
    cx.stage = [es.enter_context(nc.sbuf_tensor("stg%s%d" % (tag, i), [128, 4, D], BF16)) for i in range(2)]
    cx.stage_b = [m.buf() for _ in range(2)]
    cx.pst_f = [es.enter_context(nc.psum_tensor("pst%s%d" % (tag, i), [128, 512], F32)) for i in range(2)]
    cx.pst = [t[:, :].bitcast(BF16) for t in cx.pst_f]
    cx.pst_b = [m.buf() for _ in range(2)]
    cx.pm = [es.enter_context(nc.psum_tensor("pm%s%d" % (tag, i), [128, 1024], F32)) for i in range(3)]
    cx.pm_b = [m.buf() for _ in range(3)]
    cx.pmi = 0
    cx.ev = [es.enter_context(nc.sbuf_tensor("ev%s%d" % (tag, i), [128, 1024], BF16)) for i in range(3)]
    cx.ev_b = [m.buf() for _ in range(3)]


def next_pm(cx):
    i = cx.pmi % 3
    cx.pmi += 1
    return cx.pm[i], cx.pm_b[i]


def next_ev(cx):
    i = cx.ei % 3
    cx.ei += 1
    return cx.ev[i], cx.ev_b[i]


def evac(m, cx, k, out, in_, reads, writes, scale=None):
    nc = m.nc
    if k % 2 == 0:
        if scale is None:
            m.op(m.DVE, lambda: nc.vector.tensor_copy(out=out, in_=in_), reads=reads, writes=writes)
        else:
            m.op(m.DVE, lambda: nc.vector.tensor_scalar(out=out, in0=in_, scalar1=scale, scalar2=None, op0=ALU.mult),
                 reads=reads, writes=writes)
    else:
        if scale is None:
            m.op(m.ACT, lambda: nc.scalar.copy(out=out, in_=in_), reads=reads, writes=writes)
        else:
            m.op(m.ACT, lambda: nc.scalar.mul(out=out, in_=in_, mul=scale), reads=reads, writes=writes)


def fm_heads(m, cx, w_t, w_buf, nkc, rhsT, rhs_buf, n, heads, dst_dram, col0, dst_buf, scale=None, hw=128):
    nc = m.nc
    for h in range(heads):
        ps, pb = next_pm(cx)
        def f():
            ins = None
            for kc in range(nkc):
                ins = nc.tensor.matmul(ps[:hw, :n], lhsT=w_t[:, kc, h * hw:(h + 1) * hw], rhs=rhsT[:, kc, :n],
                                       start=(kc == 0), stop=(kc == nkc - 1))
            return ins
        m.op(m.PE, f, reads=[w_buf, rhs_buf], writes=[pb])
        ev, evb = next_ev(cx)
        evac(m, cx, h, ev[:hw, :n], ps[:hw, :n], [pb], [evb], scale)
        m.dma(m.SP, dst_dram[h, :, col0:col0 + n], ev[:hw, :n], reads=[evb], writes=[dst_buf])


def tm_out(m, cx, lhsT, lhs_buf, nkc, t0, bn, w_t, w_buf, wc0, ncols):
    nc = m.nc
    ps, pb = next_pm(cx)
    def f():
        ins = None
        for q0 in range(0, ncols, 512):
            qn = min(512, ncols - q0)
            for kc in range(nkc):
                ins = nc.tensor.matmul(ps[:bn, q0:q0 + qn], lhsT=lhsT[:, kc, t0:t0 + bn],
                                       rhs=w_t[:, kc, wc0 + q0: wc0 + q0 + qn], start=(kc == 0), stop=(kc == nkc - 1))
        return ins
    m.op(m.PE, f, reads=[lhs_buf, w_buf], writes=[pb])
    return ps, pb


def kv_pass_mla(m, xkv, cs_all, w_in, w_ukv_p, w_uq_p, kvn, qn_, kvalid, S):
    nc = m.nc
    with ExitStack() as es0:
        es = es0
        kvs = es.enter_context(nc.sbuf_tensor("kvs", [1, LFULL], F32))
        kvs16 = es.enter_context(nc.sbuf_tensor("kvs16", [1, LFULL], BF16))
        kvs_b = m.buf()
        m.dma(m.SP, kvs[:, :], kvalid.rearrange("(o n) -> o n", o=1), writes=[kvs_b])
        m.op(m.DVE, lambda: nc.vector.tensor_copy(out=kvs16[:, :], in_=kvs[:, :]), reads=[kvs_b], writes=[kvs_b])
        m.dma(m.SP, S["KAUX_M"][64:65, :], kvs16[:, :], reads=[kvs_b], writes=[S["KAUX_M_b"]])
        m.op(m.DVE, lambda: nc.vector.memset(kvs16[:, 0:NT], 1.0), writes=[kvs_b])
        for h in range(8):
            m.dma(m.SP, S["QAUX_M"][h, 64:65, :], kvs16[:, 0:NT], reads=[kvs_b], writes=[S["QAUX_M_b"]])
        m.barrier()
    persist(m, S)
    with ExitStack() as es:
        cx = Ctx()
        kv_common_setup(m, cx, es, "a1")
        win_v = w_in.rearrange("(c p) f -> p c f", p=128)
        wa = es.enter_context(nc.sbuf_tensor("wa", [128, DC, 1088], BF16))
        wukv = es.enter_context(nc.sbuf_tensor("wukv", [128, 4, 2048], BF16))
        wuq = es.enter_context(nc.sbuf_tensor("wuq", [128, 4, 1536], BF16))
        w_b = m.buf()
        load_w(m, m.POOL, wa, win_v[:, :, 0:1088], bufs=[w_b])
        load_w(m, m.POOL, wukv, w_ukv_p.rearrange("(c p) f -> p c f", p=128), bufs=[w_b])
        load_w(m, m.POOL, wuq, w_uq_p.rearrange("(c p) f -> p c f", p=128), bufs=[w_b])
        gkv = es.enter_context(nc.sbuf_tensor("gkv", [128, 512], F32))
        gq = es.enter_context(nc.sbuf_tensor("gq", [128, 512], F32))
        cx.g_buf = m.buf()
        m.dma(m.SP, gkv[:, :], kvn.partition_broadcast(128), writes=[cx.g_buf])
        m.dma(m.SP, gq[:, :], qn_.partition_broadcast(128), writes=[cx.g_buf])
        xT = [es.enter_context(nc.sbuf_tensor("xTa%d" % i, [128, DC, 512], BF16)) for i in range(2)]
        xT_b = [m.buf() for _ in range(2)]
        cT = [es.enter_context(nc.sbuf_tensor("cTa%d" % i, [128, 4, 512], BF16)) for i in range(2)]
        cT_b = [m.buf() for _ in range(2)]
        cqT = [es.enter_context(nc.sbuf_tensor("cqTa%d" % i, [128, 4, 512], BF16)) for i in range(2)]
        cqT_b = [m.buf() for _ in range(2)]
        cx.rsq = [es.enter_context(nc.sbuf_tensor("rsq%d" % i, [128, 512], F32)) for i in range(2)]
        cx.rsq_b = [m.buf() for _ in range(2)]
        cx.rst = [es.enter_context(nc.sbuf_tensor("rst%d" % i, [128, 4], F32)) for i in range(2)]
        cx.rst_b = [m.buf() for _ in range(2)]
        cx.rp = [es.enter_context(nc.sbuf_tensor("rp%d" % i, [128, 128], F32)) for i in range(2)]
        cx.rp_b = [m.buf() for _ in range(2)]
        cn = [es.enter_context(nc.sbuf_tensor("cn%d" % i, [128, 512], BF16)) for i in range(2)]
        cn_b = [m.buf() for _ in range(2)]
        cst = [es.enter_context(nc.sbuf_tensor("cst%d" % i, [128, 64], F32)) for i in range(2)]
        cst_b = [m.buf() for _ in range(2)]
        kr = [es.enter_context(nc.sbuf_tensor("kr%d" % i, [128, 64], BF16)) for i in range(2)]
        kr_b = [m.buf() for _ in range(2)]
        qr = [es.enter_context(nc.sbuf_tensor("qr%d" % i, [128, 512], BF16)) for i in range(2)]
        qr_b = [m.buf() for _ in range(2)]
        krT = [es.enter_context(nc.sbuf_tensor("krT%d" % i, [64, 512], BF16)) for i in range(2)]
        krT_b = [m.buf() for _ in range(2)]
        qrT = [es.enter_context(nc.sbuf_tensor("qrT%d" % i, [64, 8, 128], BF16)) for i in range(2)]
        qrT_b = [m.buf() for _ in range(2)]
        bi_glob = 0
        for gi, (r0, n) in enumerate(KGROUPS):
            own = r0 < NT
            x_t, x_b = xT[gi % 2], xT_b[gi % 2]
            c_t, c_b = cT[gi % 2], cT_b[gi % 2]
            blocks = xgroup_fm(m, cx, xkv, r0, n, x_t, x_b)
            for (t0, bn) in blocks:
                j = bi_glob % 2
                bi_glob += 1
                m.dma(m.SP, cst[j][:bn, :], cs_all[r0 + t0: r0 + t0 + bn, :], writes=[cst_b[j]])
                ps, pb = tm_out(m, cx, x_t, x_b, DC, t0, bn, wa, w_b, 512, 576)
                rms_norm_tm(m, cx, ps[:bn, 0:512], pb, bn, gkv, cn[j][:bn, :], cn_b[j])
                transpose_tm_to_fm(m, cx, cn[j], cn_b[j], bn, 4, c_t, c_b, t0)
                rope_tm(m, cx, ps[:bn, 512:576], pb, bn, 1, cst[j], cst_b[j], kr[j], kr_b[j], 1.0)
                k = cx.ti % 2
                cx.ti += 1
                pt, ptb = cx.pst[k], cx.pst_b[k]
                m.op(m.PE, lambda: nc.tensor.transpose(pt[:64, :bn], kr[j][:bn, 0:64], cx.ident[:bn, :bn]),
                     reads=[kr_b[j], cx.ident_buf], writes=[ptb])
                m.op(m.ACT, lambda: nc.scalar.copy(out=krT[gi % 2][:, t0:t0 + bn], in_=pt[:64, :bn]), reads=[ptb],
                     writes=[krT_b[gi % 2]])
                ps2, pb2 = tm_out(m, cx, c_t, c_b, 4, t0, bn, wukv, w_b, 1024, 1024)
                ev, evb = next_ev(cx)
                evac(m, cx, bi_glob, ev[:bn, :], ps2[:bn, :], [pb2], [evb])
                m.dma(m.SP, S["VV"][r0 + t0: r0 + t0 + bn, 0:8, :], ev[:bn, :].rearrange("p (h d) -> p h d", h=8),
                      reads=[evb], writes=[S["VV_b"]])
                if own:
                    cq_t, cq_b = cqT[gi % 2], cqT_b[gi % 2]
                    ps3, pb3 = tm_out(m, cx, x_t, x_b, DC, t0, bn, wa, w_b, 0, 512)
                    rms_norm_tm(m, cx, ps3[:bn, 0:512], pb3, bn, gq, cn[j][:bn, :], cn_b[j])
                    transpose_tm_to_fm(m, cx, cn[j], cn_b[j], bn, 4, cq_t, cq_b, t0)
                    ps4, pb4 = tm_out(m, cx, cq_t, cq_b, 4, t0, bn, wuq, w_b, 1024, 512)
                    rope_tm(m, cx, ps4[:bn, 0:512], pb4, bn, 8, cst[j], cst_b[j], qr[j], qr_b[j], SC_MLA)
                    k = cx.ti % 2
                    cx.ti += 1
                    pt, ptb = cx.pst[k], cx.pst_b[k]
                    def ftr():
                        ins = None
                        for h in range(8):
                            ins = nc.tensor.transpose(pt[:64, h * 128: h * 128 + bn], qr[j][:bn, h * 64:(h + 1) * 64],
                                                      cx.ident[:bn, :bn])
                        return ins
                    m.op(m.PE, ftr, reads=[qr_b[j], cx.ident_buf], writes=[ptb])
                    qq, qqb = qrT[j], qrT_b[j]
                    m.op(m.ACT, lambda: nc.scalar.copy(out=qq[:, :, :bn],
                                                       in_=pt[:64, :].rearrange("p (h t) -> p h t", h=8)[:, :, :bn]),
                         reads=[ptb], writes=[qqb])
                    m.dma(m.SP, S["QAUX_M"][:, 0:64, r0 + t0: r0 + t0 + bn].rearrange("h r t -> r h t"), qq[:, :, :bn],
                          reads=[qqb], writes=[S["QAUX_M_b"]])
            m.dma(m.SP, S["KAUX_M"][0:64, r0:r0 + n], krT[gi % 2][:, :n], reads=[krT_b[gi % 2]], writes=[S["KAUX_M_b"]])
            fm_heads(m, cx, wukv, w_b, 4, c_t, c_b, n, 8, S["KM"][0:8], r0, S["KM_b"])
            if own:
                fm_heads(m, cx, wuq, w_b, 4, cqT[gi % 2], cqT_b[gi % 2], n, 8, S["QM"][0:8], r0, S["QM_b"], scale=SC_MLA)
        m.barrier()


def kv_pass_fox(m, xkv, w_in, fox_b, kvalid, kind, S):
    nc = m.nc
    with ExitStack() as es:
        cx = Ctx()
        FL = es.enter_context(nc.sbuf_tensor("FL", [8, LFULL], F32))
        FL_b = m.buf()
        esw = ExitStack()
        kv_common_setup(m, cx, esw, "a2")
        win_v = w_in.rearrange("(c p) f -> p c f", p=128)
        wq = esw.enter_context(nc.sbuf_tensor("wqf", [128, DC, 1024], BF16))
        wk = esw.enter_context(nc.sbuf_tensor("wkf", [128, DC, 1024], BF16))
        wv = esw.enter_context(nc.sbuf_tensor("wvf", [128, DC, 1024], BF16))
        wf = esw.enter_context(nc.sbuf_tensor("wff", [128, DC, 8], BF16))
        w_b = m.buf()
        load_w(m, m.POOL, wq, win_v[:, :, 1088:2112], bufs=[w_b])
        load_w(m, m.POOL, wk, win_v[:, :, 2112:3136], bufs=[w_b])
        load_w(m, m.POOL, wv, win_v[:, :, 3136:4160], bufs=[w_b])
        load_w(m, m.POOL, wf, win_v[:, :, 4160:4168], step=16, bufs=[w_b])
        xT = esw.enter_context(nc.sbuf_tensor("xTf", [128, DC, 512], BF16))
        xT_b = m.buf()
        bi_glob = 0
        for gi, (r0, n) in enumerate(KGROUPS):
            own = r0 < NT
            blocks = xgroup_fm(m, cx, xkv, r0, n, xT, xT_b)
            fm_heads(m, cx, wk, w_b, DC, xT, xT_b, n, 8, S["KM"][8:16], r0, S["KM_b"])
            if own:
                fm_heads(m, cx, wq, w_b, DC, xT, xT_b, n, 8, S["QM"][8:16], r0, S["QM_b"], scale=SC_FOX)
            ps, pb = next_pm(cx)
            def f():
                ins = None
                for kc in range(DC):
                    ins = nc.tensor.matmul(ps[:8, :n], lhsT=wf[:, kc, 0:8], rhs=xT[:, kc, :n], start=(kc == 0), stop=(kc == DC - 1))
                return ins
            m.op(m.PE, f, reads=[w_b, xT_b], writes=[pb])
            m.op(m.DVE, lambda: nc.vector.tensor_copy(out=FL[:, r0:r0 + n], in_=ps[:8, :n]), reads=[pb], writes=[FL_b])
            for (t0, bn) in blocks:
                bi_glob += 1
                ps2, pb2 = tm_out(m, cx, xT, xT_b, DC, t0, bn, wv, w_b, 0, 1024)
                ev, evb = next_ev(cx)
                evac(m, cx, bi_glob, ev[:bn, :], ps2[:bn, :], [pb2], [evb])
                m.dma(m.SP, S["VV"][r0 + t0: r0 + t0 + bn, 8:16, :], ev[:bn, :].rearrange("p (h d) -> p h d", h=8),
                      reads=[evb], writes=[S["VV_b"]])
        m.barrier()
        esw.close()
        fb = es.enter_context(nc.sbuf_tensor("fb", [8, 2], F32))
        T1 = es.enter_context(nc.sbuf_tensor("T1", [8, LFULL], F32))
        T2 = es.enter_context(nc.sbuf_tensor("T2", [8, LFULL], F32))
        KV = es.enter_context(nc.sbuf_tensor("KVd", [8, LFULL], BF16))
        KI = es.enter_context(nc.sbuf_tensor("KId", [8, LFULL], BF16))
        B1 = es.enter_context(nc.sbuf_tensor("B1", [8, LFULL], BF16))
        B2 = es.enter_context(nc.sbuf_tensor("B2", [8, LFULL], BF16))
        B3 = es.enter_context(nc.sbuf_tensor("B3", [8, LFULL], BF16))
        tb = m.buf()
        m.dma(m.SP, fb[:, 0:1], fox_b.rearrange("(h o) -> h o", o=1), writes=[tb])
        m.dma(m.POOL, KV[:, :], kvalid.partition_broadcast(8), writes=[tb])
        m.dma(m.POOL, KI[:, :], kind.partition_broadcast(8), writes=[tb])
        m.op(m.DVE, lambda: nc.vector.tensor_scalar(out=fb[:, 1:2], in0=fb[:, 0:1], scalar1=-1.0, scalar2=None, op0=ALU.mult),
             reads=[tb], writes=[tb])
        m.op(m.ACT, lambda: nc.scalar.activation(out=T1[:, :], in_=FL[:, :], func=AF.Exp, bias=fb[:, 1:2], scale=-1.0),
             reads=[FL_b, tb], writes=[tb])
        m.op(m.ACT, lambda: nc.scalar.activation(out=T1[:, :], in_=T1[:, :], func=AF.Ln, bias=1.0, scale=1.0),
             reads=[tb], writes=[tb])
        m.op(m.DVE, lambda: nc.vector.tensor_scalar(out=T1[:, :], in0=T1[:, :], scalar1=-1.0, scalar2=None, op0=ALU.mult),
             reads=[tb], writes=[tb])
        m.op(m.DVE, lambda: nc.vector.tensor_scalar(out=T2[:, NT:], in0=KV[:, NT:], scalar1=1e-30, scalar2=1.0, op0=ALU.mult,
                                                    op1=ALU.add), reads=[tb], writes=[tb])
        m.op(m.DVE, lambda: nc.vector.tensor_tensor(out=T2[:, NT:], in0=T2[:, NT:], in1=T1[:, NT:], op=ALU.mult),
             reads=[tb], writes=[tb])
        m.op(m.DVE, lambda: nc.vector.reduce_sum(out=fb[:, 0:1], in_=T2[:, NT:], axis=AX.X), reads=[tb], writes=[tb])
        m.op(m.DVE, lambda: nc.vector.memset(T2[:, :], 1.0), reads=[tb], writes=[tb])
        m.op(m.DVE, lambda: nc.vector.tensor_tensor_scan(out=FL[:, 0:NT], data0=T2[:, 0:NT], data1=T1[:, 0:NT],
                                                         initial=fb[:, 0:1], op0=ALU.mult, op1=ALU.add),
             reads=[tb], writes=[FL_b])
        m.op(m.DVE, lambda: nc.vector.tensor_tensor_scan(out=FL[:, NT:], data0=T2[:, NT:], data1=T1[:, NT:],
                                                         initial=0.0, op0=ALU.mult, op1=ALU.add),
             reads=[tb], writes=[FL_b])
        m.op(m.DVE, lambda: nc.vector.tensor_copy(out=B1[:, :], in_=FL[:, :]), reads=[FL_b], writes=[tb])
        m.op(m.DVE, lambda: nc.vector.tensor_tensor(out=T1[:, :], in0=FL[:, :], in1=B1[:, :], op=ALU.subtract),
             reads=[tb], writes=[tb])
        m.op(m.DVE, lambda: nc.vector.tensor_copy(out=B2[:, :], in_=T1[:, :]), reads=[tb], writes=[tb])
        m.dma(m.SP, S["QAUX_F"][:, 0, :], B1[:, 0:NT], reads=[tb], writes=[S["QAUX_F_b"]])
        m.dma(m.SP, S["QAUX_F"][:, 1, :], B2[:, 0:NT], reads=[tb], writes=[S["QAUX_F_b"]])
        m.op(m.DVE, lambda: nc.vector.memset(B3[:, :], 1.0), reads=[tb], writes=[tb])
        for r in (2, 3, 4):
            m.dma(m.SP, S["QAUX_F"][:, r, :], B3[:, 0:NT], reads=[tb], writes=[S["QAUX_F_b"]])
        m.op(m.DVE, lambda: nc.vector.tensor_scalar(out=B3[:, :], in0=KI[:, :], scalar1=-1.0, scalar2=None, op0=ALU.add),
             reads=[tb], writes=[tb])
        for r in (0, 1):
            m.dma(m.SP, S["KAUX_F"][:, r, :], B3[:, :], reads=[tb], writes=[S["KAUX_F_b"]])
        m.op(m.DVE, lambda: nc.vector.scalar_tensor_tensor(out=T1[:, :], in0=B1[:, :], scalar=-1.0, in1=KI[:, :], op0=ALU.mult,
                                                           op1=ALU.mult), reads=[tb], writes=[tb])
        m.op(m.DVE, lambda: nc.vector.tensor_copy(out=B1[:, :], in_=T1[:, :]), reads=[tb], writes=[tb])
        m.dma(m.SP, S["KAUX_F"][:, 2, :], B1[:, :], reads=[tb], writes=[S["KAUX_F_b"]])
        m.op(m.DVE, lambda: nc.vector.scalar_tensor_tensor(out=T1[:, :], in0=B2[:, :], scalar=-1.0, in1=KI[:, :], op0=ALU.mult,
                                                           op1=ALU.mult), reads=[tb], writes=[tb])
        m.op(m.DVE, lambda: nc.vector.tensor_copy(out=B2[:, :], in_=T1[:, :]), reads=[tb], writes=[tb])
        m.dma(m.SP, S["KAUX_F"][:, 3, :], B2[:, :], reads=[tb], writes=[S["KAUX_F_b"]])
        m.dma(m.SP, S["KAUX_F"][:, 4, :], KV[:, :], reads=[tb], writes=[S["KAUX_F_b"]])
        m.barrier()


def attention_phase(m, S):
    nc = m.nc
    NKB = 65
    with ExitStack() as es:
        ident = es.enter_context(nc.sbuf_tensor("identat", [128, 128], BF16))
        ident_buf, _ = make_identity(m, ident)
        km = [es.enter_context(nc.sbuf_tensor("km%d" % i, [128, LFULL], BF16)) for i in range(2)]
        ka = [es.enter_context(nc.sbuf_tensor("ka%d" % i, [65, LFULL], BF16)) for i in range(2)]
        vv = [es.enter_context(nc.sbuf_tensor("vv%d" % i, [128, NKB, 128], BF16)) for i in range(2)]
        qm = [es.enter_context(nc.sbuf_tensor("qm%d" % i, [128, NT], BF16)) for i in range(2)]
        qa = [es.enter_context(nc.sbuf_tensor("qa%d" % i, [65, NT], BF16)) for i in range(2)]
        km_b = [m.buf() for _ in range(2)]
        ka_b = [m.buf() for _ in range(2)]
        vv_b = [m.buf() for _ in range(2)]
        qm_b = [m.buf() for _ in range(2)]
        qa_b = [m.buf() for _ in range(2)]
        Sall = es.enter_context(nc.sbuf_tensor("Sall", [128, LFULL], F32))
        Sall_b = m.buf()
        Pt = [es.enter_context(nc.sbuf_tensor("Pt%d" % i, [128, LFULL], BF16)) for i in range(2)]
        Pt_b = [m.buf() for _ in range(2)]
        PT = [es.enter_context(nc.sbuf_tensor("PTs%d" % i, [128, 8, 128], BF16)) for i in range(2)]
        PT_b = [m.buf() for _ in range(2)]
        mask = es.enter_context(nc.sbuf_tensor("maskT", [128, 2 * NT], F32))
        mask_b = m.buf()
        def fmask():
            nc.gpsimd.memset(mask[:, :], 0.0)
            return nc.gpsimd.affine_select(out=mask[:, :], in_=mask[:, :], pattern=[[-1, 2 * NT]], compare_op=ALU.is_ge,
                                           fill=NEG, base=NT, channel_multiplier=1)
        m.op(m.POOL, fmask, writes=[mask_b])
        stt = [es.enter_context(nc.sbuf_tensor("stt%d" % i, [128, 16], F32)) for i in range(2)]
        stt_b = [m.buf() for _ in range(2)]
        osb = [es.enter_context(nc.sbuf_tensor("osb%d" % i, [128, 128], BF16)) for i in range(2)]
        osb_b = [m.buf() for _ in range(2)]
        ps_s = [es.enter_context(nc.psum_tensor("ps_s%d" % i, [128, 512], F32)) for i in range(4)]
        ps_s_b = [m.buf() for _ in range(4)]
        ps_t_f = [es.enter_context(nc.psum_tensor("ps_t%d" % i, [128, 512], F32)) for i in range(2)]
        ps_t = [t[:, :].bitcast(BF16) for t in ps_t_f]
        ps_t_b = [m.buf() for _ in range(2)]
        ps_o = [es.enter_context(nc.psum_tensor("ps_o%d" % i, [128, 128], F32)) for i in range(2)]
        ps_o_b = [m.buf() for _ in range(2)]
        qblocks = tok_blocks(NT)
        ci = 0
        ti = 0
        it = 0
        mla_aux_loaded = False
        for h in range(16):
            s = h % 2
            mla = h < 8
            ra = 65 if mla else 5
            m.dma(m.SP, km[s][:, :], S["KM"][h], reads=[S["KM_b"]], writes=[km_b[s]])
            m.dma(m.SP, qm[s][:, :], S["QM"][h], reads=[S["QM_b"]], writes=[qm_b[s]])
            if mla:
                if not mla_aux_loaded:
                    m.dma(m.SP, ka[0][:, :], S["KAUX_M"][:, :], reads=[S["KAUX_M_b"]], writes=[ka_b[0]])
                    mla_aux_loaded = True
                ka_t, ka_tb = ka[0], ka_b[0]
                m.dma(m.SP, qa[s][:65, :], S["QAUX_M"][h], reads=[S["QAUX_M_b"]], writes=[qa_b[s]])
            else:
                ka_t, ka_tb = ka[s], ka_b[s]
                m.dma(m.SP, ka_t[:5, :], S["KAUX_F"][h - 8], reads=[S["KAUX_F_b"]], writes=[ka_tb])
                m.dma(m.SP, qa[s][:5, :], S["QAUX_F"][h - 8], reads=[S["QAUX_F_b"]], writes=[qa_b[s]])
            m.dma(m.SP, vv[s][:16, 0, :], S["VV"][0:16, h, :], reads=[S["VV_b"]], writes=[vv_b[s]])
            for b0 in range(0, 64, 16):
                m.dma(m.SP, vv[s][:, 1 + b0: 1 + b0 + 16, :],
                      S["VV"][16 + b0 * 128: 16 + (b0 + 16) * 128, h, :].rearrange("(b p) d -> p b d", p=128),
                      reads=[S["VV_b"]], writes=[vv_b[s]])
            for qb, (q0, n) in enumerate(qblocks):
                nk_own = q0 + n
                chunks = []
                c0 = 0
                while c0 < nk_own:
                    ln = min(512, nk_own - c0)
                    chunks.append((c0, ln, c0, True))
                    c0 += ln
                for j in range(14):
                    chunks.append((NT + 512 * j, 512, nk_own + 512 * j, False))
                slen = nk_own + NOTH
                for (k0, ln, d0, is_own) in chunks:
                    p = ci % 4
                    ci += 1
                    def fqk():
                        nc.tensor.matmul(ps_s[p][:n, :ln], lhsT=qm[s][:, q0:q0 + n], rhs=km[s][:, k0:k0 + ln], start=True, stop=False)
                        return nc.tensor.matmul(ps_s[p][:n, :ln], lhsT=qa[s][:ra, q0:q0 + n], rhs=ka_t[:ra, k0:k0 + ln],
                                                start=False, stop=True)
                    m.op(m.PE, fqk, reads=[qm_b[s], km_b[s], qa_b[s], ka_tb], writes=[ps_s_b[p]])
                    if is_own:
                        mo = (NT - q0) + k0
                        m.op(m.DVE, lambda: nc.vector.tensor_tensor(out=Sall[:n, d0:d0 + ln], in0=ps_s[p][:n, :ln],
                                                                    in1=mask[:n, mo:mo + ln], op=ALU.add),
                             reads=[ps_s_b[p], mask_b], writes=[Sall_b])
                    elif ci % 2 == 0:
                        m.op(m.DVE, lambda: nc.vector.tensor_copy(out=Sall[:n, d0:d0 + ln], in_=ps_s[p][:n, :ln]),
                             reads=[ps_s_b[p]], writes=[Sall_b])
                    else:
                        m.op(m.ACT, lambda: nc.scalar.copy(out=Sall[:n, d0:d0 + ln], in_=ps_s[p][:n, :ln]),
                             reads=[ps_s_b[p]], writes=[Sall_b])
                sti = it % 2
                it += 1
                st, stb = stt[sti], stt_b[sti]
                pt_, ptb_ = Pt[sti], Pt_b[sti]
                m.op(m.DVE, lambda: nc.vector.reduce_max(out=st[:n, 0:1], in_=Sall[:n, 0:slen], axis=AX.X),
                     reads=[Sall_b], writes=[stb])
                m.op(m.DVE, lambda: nc.vector.tensor_scalar(out=st[:n, 1:2], in0=st[:n, 0:1], scalar1=-1.0, scalar2=None,
                                                            op0=ALU.mult), reads=[stb], writes=[stb])
                npieces = 4
                pl = -(-slen // npieces)
                for pi in range(npieces):
                    a = pi * pl
                    b = min(slen, a + pl)
                    m.op(m.ACT, lambda: nc.scalar.activation(out=pt_[:n, a:b], in_=Sall[:n, a:b], func=AF.Exp,
                                                             bias=st[:n, 1:2], scale=1.0, accum_out=st[:n, 4 + pi:5 + pi]),
                         reads=[Sall_b, stb], writes=[ptb_, stb])
                m.op(m.DVE, lambda: nc.vector.reduce_sum(out=st[:n, 2:3], in_=st[:n, 4:4 + npieces], axis=AX.X),
                     reads=[stb], writes=[stb])
                m.op(m.DVE, lambda: nc.vector.reciprocal(out=st[:n, 3:4], in_=st[:n, 2:3]), reads=[stb], writes=[stb])
                kblocks = [(0, 16, 0)] + [(16 + 128 * (j - 1), 128, j) for j in range(1, qb + 1)]
                for j in range(56):
                    kblocks.append((nk_own + 128 * j, 128, 9 + j))
                po, pob = ps_o[sti], ps_o_b[sti]
                nkb = len(kblocks)
                first_pv = True
                for g0 in range(0, nkb, 8):
                    grp = kblocks[g0:g0 + 8]
                    tp = ti % 2
                    ti += 1
                    def ftr():
                        ins = None
                        for jj, (pc, kl, vb) in enumerate(grp):
                            ins = nc.tensor.transpose(ps_t[tp][:kl, jj * 128: jj * 128 + n], pt_[:n, pc:pc + kl], ident[:n, :n])
                        return ins
                    m.op(m.PE, ftr, reads=[ptb_, ident_buf], writes=[ps_t_b[tp]])
                    kl0 = grp[0][1]
                    ng = len(grp)
                    if kl0 == 16:
                        m.op(m.DVE, lambda: nc.vector.tensor_copy(out=PT[tp][:16, 0, :n], in_=ps_t[tp][:16, 0:n]),
                             reads=[ps_t_b[tp]], writes=[PT_b[tp]])
                        if ng > 1:
                            m.op(m.DVE, lambda: nc.vector.tensor_copy(
                                out=PT[tp][:, 1:ng, :n],
                                in_=ps_t[tp][:, 128:ng * 128].rearrange("p (j t) -> p j t", j=ng - 1)[:, :, :n]),
                                reads=[ps_t_b[tp]], writes=[PT_b[tp]])
                    elif tp == 0:
                        m.op(m.DVE, lambda: nc.vector.tensor_copy(
                            out=PT[tp][:, 0:ng, :n], in_=ps_t[tp][:, 0:ng * 128].rearrange("p (j t) -> p j t", j=ng)[:, :, :n]),
                            reads=[ps_t_b[tp]], writes=[PT_b[tp]])
                    else:
                        m.op(m.ACT, lambda: nc.scalar.copy(
                            out=PT[tp][:, 0:ng, :n], in_=ps_t[tp][:, 0:ng * 128].rearrange("p (j t) -> p j t", j=ng)[:, :, :n]),
                            reads=[ps_t_b[tp]], writes=[PT_b[tp]])
                    last_grp = (g0 + 8 >= nkb)
                    def fpv():
                        ins = None
                        for jj, (pc, kl, vb) in enumerate(grp):
                            ins = nc.tensor.matmul(po[:n, :], lhsT=PT[tp][:kl, jj, :n], rhs=vv[s][:kl, vb, :],
                                                   start=(first_pv and jj == 0), stop=(last_grp and jj == ng - 1))
                        return ins
                    m.op(m.PE, fpv, reads=[PT_b[tp], vv_b[s]], writes=[pob])
                    first_pv = False
                ob, obb = osb[sti], osb_b[sti]
                m.op(m.ACT, lambda: nc.scalar.mul(out=ob[:n, :], in_=po[:n, :], mul=st[:n, 3:4]), reads=[pob, stb], writes=[obb])
                m.dma(m.SP, S["AO"][q0:q0 + n, h * 128:(h + 1) * 128], ob[:n, :], reads=[obb], writes=[S["AO_b"]])
        m.barrier()


def outproj_phase(m, tag, A_dram, A_buf, a_is_bf16, ntok, arow0, w_out, Hin, Hin_buf, hrow0, g_dram, b_dram, Hout, Hout_buf, orow0,
                  pre=None):
    nc = m.nc
    with ExitStack() as es:
        cx = Ctx()
        blocks = tok_blocks(ntok)
        aT = es.enter_context(nc.sbuf_tensor("aT" + tag, [128, DC, ntok], BF16))
        aT_b = m.buf()
        ident = es.enter_context(nc.sbuf_tensor("ident" + tag, [128, 128], BF16))
        ident_buf, _ = make_identity(m, ident)
        wo = es.enter_context(nc.sbuf_tensor("wo" + tag, [128, DC, D], BF16))
        wo_b = m.buf()
        load_w(m, m.POOL, wo, w_out.rearrange("(c p) f -> p c f", p=128), step=2, bufs=[wo_b])
        py = [es.enter_context(nc.psum_tensor("py%s%d" % (tag, i), [128, 2048], F32)) for i in range(1)]
        py_b = [m.buf() for _ in range(1)]
        pt_f = [es.enter_context(nc.psum_tensor("pt%s%d" % (tag, i), [128, 512], F32)) for i in range(2)]
        pt_b = [m.buf() for _ in range(2)]
        with ExitStack() as es2:
            stage = [es2.enter_context(nc.sbuf_tensor("stg%s%d" % (tag, i), [128, D], BF16)) for i in range(2)]
            stage_b = [m.buf() for _ in range(2)]
            pst = [t[:, :].bitcast(BF16) for t in pt_f]
            load_fm(m, A_dram, arow0, ntok, aT, aT_b, ident, ident_buf, stage, stage_b, pst, pt_b, Hbuf=A_buf,
                    q=(m.SP if a_is_bf16 else m.POOL))
            m.barrier()
        ln_setup(m, cx, es, tag, g_dram, b_dram, [Hin_buf], Hout_buf)
        for bi, (t0, n) in enumerate(blocks):
            def f():
                ins = None
                for q in range(4):
                    for c in range(DC):
                        ins = nc.tensor.matmul(py[0][:n, q * 512:(q + 1) * 512], lhsT=aT[:, c, t0:t0 + n],
                                               rhs=wo[:, c, q * 512:(q + 1) * 512], start=(c == 0), stop=(c == DC - 1))
                return ins
            m.op(m.PE, f, reads=[aT_b, wo_b], writes=[py_b[0]])
            layer_norm_block(m, cx, py[0][:n, :], [py_b[0]], Hin, hrow0 + t0, n, Hout, cx.gam, cx.bet, out_row=orow0 + t0)
        m.barrier()


def make_scratch(m):
    nc = m.nc
    S = {}
    def mk(name, shape, dt):
        S[name] = nc.dram_tensor("scr_" + name, shape, dt).ap()
        S[name + "_b"] = m.buf()
    mk("KM", [16, 128, LFULL], BF16)
    mk("KAUX_M", [65, LFULL], BF16)
    mk("KAUX_F", [8, 5, LFULL], BF16)
    mk("VV", [LFULL, 16, 128], BF16)
    mk("QM", [16, 128, NT], BF16)
    mk("QAUX_M", [8, 65, NT], BF16)
    mk("QAUX_F", [8, 5, NT], BF16)
    mk("AO", [NT, D], BF16)
    mk("H1", [NT, D], F32)
    mk("H2", [NT, D], F32)
    mk("H3", [NOWN, D], F32)
    mk("PM", [NOWN, D], BF16)
    return S


def persist(m, S):
    for k, v in S.items():
        if k.endswith("_b"):
            m.bufs.append(v)


def layer0_mixer(m, I, S):
    kv_pass_mla(m, I["xkv"], I["cs_all"], I["attn_w_in"], I["w_ukv_p"], I["w_uq_p"], I["mla_kv_norm"], I["mla_q_norm"],
                I["kvalid"], S)
    persist(m, S)
    kv_pass_fox(m, I["xkv"], I["attn_w_in"], I["fox_forget_bias"], I["kvalid"], I["kind"], S)
    persist(m, S)
    attention_phase(m, S)
    persist(m, S)
    xb = m.buf()
    outproj_phase(m, "op0", S["AO"], S["AO_b"], True, NT, 0, I["attn_w_out"], I["xkv"], xb, 0, I["ln_mix_g0"], I["ln_mix_b0"],
                  S["H1"], S["H1_b"], 0)
    persist(m, S)


def declare_inputs(nc, names_shapes):
    I = {}
    for name, shape in names_shapes:
        I[name] = nc.dram_tensor(name, list(shape), F32, kind="ExternalInput").ap()
    return I


L0_INPUTS = [("xkv", (LFULL, D)), ("cs_all", (LFULL, 64)), ("attn_w_in", (D, 4168)), ("w_ukv_p", (512, 2048)),
             ("w_uq_p", (512, 1536)), ("mla_kv_norm", (512,)), ("mla_q_norm", (512,)), ("kvalid", (LFULL,)),
             ("kind", (LFULL,)), ("fox_forget_bias", (8,)), ("attn_w_out", (D, D)), ("ln_mix_g0", (D,)), ("ln_mix_b0", (D,))]


def host_prep_l0(inp):
    x = np.asarray(inp["x"], np.float32)[0]
    hfull = np.concatenate([np.asarray(inp["meta_tokens"], np.float32), x], axis=0)
    pos = np.arange(LFULL, dtype=np.float32)
    inv_freq = (10000.0 ** (-np.arange(0, 64, 2, dtype=np.float32) / 64)).astype(np.float32)
    ang = pos[:, None] * inv_freq[None, :]
    cs = np.concatenate([np.cos(ang), np.sin(ang)], axis=1).astype(np.float32)
    w_ukv = np.asarray(inp["mla_w_ukv"], np.float32)[0].reshape(512, 8, 256)
    w_ukv_p = np.ascontiguousarray(np.concatenate([w_ukv[:, :, :128].reshape(512, 1024), w_ukv[:, :, 128:].reshape(512, 1024)], 1))
    w_uq = np.asarray(inp["mla_w_uq"], np.float32)[0].reshape(512, 8, 192)
    w_uq_p = np.ascontiguousarray(np.concatenate([w_uq[:, :, :128].reshape(512, 1024), w_uq[:, :, 128:].reshape(512, 512)], 1))
    common = {
        "attn_w_in": np.ascontiguousarray(np.asarray(inp["attn_w_in"], np.float32)[0]),
        "w_ukv_p": w_ukv_p, "w_uq_p": w_uq_p,
        "mla_kv_norm": np.asarray(inp["mla_kv_norm"], np.float32)[0], "mla_q_norm": np.asarray(inp["mla_q_norm"], np.float32)[0],
        "fox_forget_bias": np.asarray(inp["fox_forget_bias"], np.float32)[0],
        "attn_w_out": np.ascontiguousarray(np.asarray(inp["attn_w_out"], np.float32)[0]),
        "ln_mix_g0": np.asarray(inp["ln_mix_g"], np.float32)[0], "ln_mix_b0": np.asarray(inp["ln_mix_b"], np.float32)[0],
    }
    maps = []
    for c in range(NCORE):
        own = np.arange(1024 * c, 1024 * c + NT)
        oth = np.concatenate([np.arange(0, 1024 * c), np.arange(1024 * c + NT, LFULL)])
        perm = np.concatenate([own, oth])
        d = dict(common)
        d["xkv"] = np.ascontiguousarray(hfull[perm])
        d["cs_all"] = np.ascontiguousarray(cs[perm])
        kvalid = np.zeros(LFULL, np.float32)
        kvalid[NT:] = np.where(oth < 1024 * c, 0.0, NEG)
        d["kvalid"] = kvalid
        d["kind"] = (perm >= 16).astype(np.float32)
        maps.append(d)
    return maps


def pool_phase(m, I, S):
    nc = m.nc
    H2, H2_b = S["H2"], S["H2_b"]
    groups_all = tok_groups(NT)
    with ExitStack() as es:
        dT = es.enter_context(nc.sbuf_tensor("dTp", [128, DC, NOWN], BF16))
        dT_b = m.buf()
        with ExitStack() as es1:
            hT = es1.enter_context(nc.sbuf_tensor("hTp", [128, DC, NT], BF16))
            hT_b = m.buf()
            ident = es1.enter_context(nc.sbuf_tensor("identp", [128, 128], BF16))
            ident_buf, _ = make_identity(m, ident)
            wpi = es1.enter_context(nc.sbuf_tensor("wpi", [128, DC, D], BF16))
            wpi_b = m.buf()
            load_w(m, m.POOL, wpi, I["pool_w_in"].rearrange("(c p) f -> p c f", p=128), step=2, bufs=[wpi_b])
            pT = es1.enter_context(nc.sbuf_tensor("pTp", [128, 4, NT], F32))
            A = es1.enter_context(nc.sbuf_tensor("Ap", [128, 4, NT], F32))
            B = es1.enter_context(nc.sbuf_tensor("Bp", [128, 4, NT], F32))
            pT_b, A_b, B_b = m.buf(), m.buf(), m.buf()
            pp = [es1.enter_context(nc.psum_tensor("ppp%d" % i, [128, 512], F32)) for i in range(4)]
            pp_b = [m.buf() for _ in range(4)]
            with ExitStack() as es2:
                stage = [es2.enter_context(nc.sbuf_tensor("stgp%d" % i, [128, D], BF16)) for i in range(2)]
                stage_b = [m.buf() for _ in range(2)]
                pst = [pp[i][:, :].bitcast(BF16) for i in range(2)]
                load_fm(m, H2, 0, NT, hT, hT_b, ident, ident_buf, stage, stage_b, pst, pp_b[0:2], Hbuf=H2_b)
                m.barrier()
            k = 0
            for g, w in enumerate((2, 4, 8, 16)):
                for oc in range(4):
                    ch = 4 * g + oc
                    for (t0, n) in groups_all:
                        p = k % 4
                        k += 1
                        def f():
                            ins = None
                            for c in range(DC):
                                ins = nc.tensor.matmul(pp[p][:, :n], lhsT=wpi[:, c, ch * 128:(ch + 1) * 128], rhs=hT[:, c, t0:t0 + n],
                                                       start=(c == 0), stop=(c == DC - 1))
                            return ins
                        m.op(m.PE, f, reads=[wpi_b, hT_b], writes=[pp_b[p]])
                        evac(m, None, k, pT[:, oc, t0:t0 + n], pp[p][:, :n], [pp_b[p]], [pT_b])
                cur, cur_b = pT, pT_b
                dst = [(A, A_b), (B, B_b)]
                sh = 1
                lvl = 0
                while sh < w:
                    o, o_b = dst[lvl % 2]
                    lo = 2 * sh - 1
                    src, src_b = cur, cur_b
                    m.op(m.DVE, lambda: nc.vector.tensor_tensor(out=o[:, :, lo:NT], in0=src[:, :, lo:NT], in1=src[:, :, lo - sh:NT - sh],
                                                                op=ALU.add), reads=[src_b], writes=[o_b])
                    cur, cur_b = o, o_b
                    sh *= 2
                    lvl += 1
                sw, sw_b = cur, cur_b
                m.op(m.DVE, lambda: nc.vector.scalar_tensor_tensor(out=dT[:, 4 * g:4 * g + 4, :], in0=sw[:, :, NHALO:NT], scalar=1.0 / w,
                                                                   in1=pT[:, :, NHALO:NT], op0=ALU.mult, op1=ALU.subtract),
                     reads=[sw_b, pT_b], writes=[dT_b])
            m.barrier()
        wgr = es.enter_context(nc.sbuf_tensor("wgr", [128, 16, 512], BF16))
        wgr_b = m.buf()
        load_w(m, m.POOL, wgr, I["pool_w_group"].rearrange("g (c p) f -> p (g c) f", p=128), bufs=[wgr_b])
        scl = es.enter_context(nc.sbuf_tensor("sclp", [128, D], F32))
        scl_b = m.buf()
        m.dma(m.SP, scl[:, :], I["pool_scale"].partition_broadcast(128), writes=[scl_b])
        ysb = [es.enter_context(nc.sbuf_tensor("ysbp%d" % i, [128, D], BF16)) for i in range(2)]
        ysb_b = [m.buf() for _ in range(2)]
        pq = [es.enter_context(nc.psum_tensor("pqp%d" % i, [128, 512], F32)) for i in range(4)]
        pq_b = [m.buf() for _ in range(4)]
        k = 0
        for bi, (t0, n) in enumerate(tok_blocks(NOWN)):
            y, y_b = ysb[bi % 2], ysb_b[bi % 2]
            for g in range(4):
                p = k % 4
                k += 1
                def f():
                    ins = None
                    for kc in range(4):
                        ins = nc.tensor.matmul(pq[p][:n, :], lhsT=dT[:, 4 * g + kc, t0:t0 + n], rhs=wgr[:, 4 * g + kc, :],
                                               start=(kc == 0), stop=(kc == 3))
                    return ins
                m.op(m.PE, f, reads=[dT_b, wgr_b], writes=[pq_b[p]])
                m.op(m.DVE, lambda: nc.vector.tensor_tensor(out=y[:n, g * 512:(g + 1) * 512], in0=pq[p][:n, :],
                                                            in1=scl[:n, g * 512:(g + 1) * 512], op=ALU.mult),
                     reads=[pq_b[p], scl_b], writes=[y_b])
            m.dma(m.SP, S["PM"][t0:t0 + n, :], y[:n, :], reads=[y_b], writes=[S["PM_b"]])
        m.barrier()
    persist(m, S)
    outproj_phase(m, "op1", S["PM"], S["PM_b"], True, NOWN, 0, I["pool_w_out"], S["H2"], S["H2_b"], NHALO,
                  I["ln_mix_g1"], I["ln_mix_b1"], S["H3"], S["H3_b"], 0)
    persist(m, S)


def make_moe_gates(m, I, S):
    def gates(cx, es, hT_unused, hT_buf_unused, blocks):
        nc = m.nc
        nb = len(blocks)
        gate_tile = cx.gate_tile
        cx.gate_buf = m.buf()
        with ExitStack() as e3:
            identf = e3.enter_context(nc.sbuf_tensor("identf", [128, 128], F32))
            idb = m.buf()
            def fi():
                nc.gpsimd.memset(identf[:], 1.0)
                return nc.gpsimd.affine_select(out=identf[:], in_=identf[:], pattern=[[-1, 128]], compare_op=ALU.is_equal,
                                               fill=0.0, base=0, channel_multiplier=1)
            m.op(m.POOL, fi, writes=[idb])
            wr = e3.enter_context(nc.sbuf_tensor("wrt", [128, DC, NEXP], F32))
            br = e3.enter_context(nc.sbuf_tensor("brt", [128, NEXP], F32))
            wr_b = m.buf()
            m.dma(m.SP, wr[:, :, :], I["moe_w_router"].rearrange("(c p) e -> p c e", p=128), writes=[wr_b])
            m.dma(m.SP, br[:, :], I["moe_b_router"].partition_broadcast(128), writes=[wr_b])
            hr = [e3.enter_context(nc.sbuf_tensor("hrt%d" % i, [128, D], F32)) for i in range(2)]
            hr_b = [m.buf() for _ in range(2)]
            hTf = e3.enter_context(nc.sbuf_tensor("hTft", [128, DC, 128], F32))
            hTf_b = m.buf()
            gsm = e3.enter_context(nc.sbuf_tensor("gsm", [128, 64], F32))
            gsm_b = m.buf()
            ptf, ptf_b = cx.pg, cx.pg_b
            plg, plg_b = cx.pu[0], cx.pu_b[0]
            k = 0
            for bi, (t0, n) in enumerate(blocks):
                h, hb = hr[bi % 2], hr_b[bi % 2]
                m.dma(m.SP, h[:n, :], S["H3"][t0:t0 + n, :], reads=[S["H3_b"]], writes=[hb])
                for q in range(4):
                    p = k % 2
                    k += 1
                    def f():
                        ins = None
                        for j in range(4):
                            c = q * 4 + j
                            ins = nc.tensor.transpose(ptf[p][:, j * 128: j * 128 + n], h[:n, c * 128:(c + 1) * 128], identf[:n, :n])
                        return ins
                    m.op(m.PE, f, reads=[hb, idb], writes=[ptf_b[p]])
                    m.op(m.DVE, lambda: nc.vector.tensor_copy(out=hTf[:, q * 4:(q + 1) * 4, :n],
                                                              in_=ptf[p][:, :].rearrange("p (j t) -> p j t", j=4)[:, :, :n]),
                         reads=[ptf_b[p]], writes=[hTf_b])
                def fl():
                    ins = None
                    for c in range(DC):
                        ins = nc.tensor.matmul(plg[:n, 0:NEXP], lhsT=hTf[:, c, :n], rhs=wr[:, c, :], start=(c == 0), stop=(c == DC - 1))
                    return ins
                m.op(m.PE, fl, reads=[hTf_b, wr_b], writes=[plg_b])
                lg, eq1, lg2, eq2 = gsm[:n, 0:8], gsm[:n, 8:16], gsm[:n, 16:24], gsm[:n, 24:32]
                m1, m2, dd, g1, g2 = gsm[:n, 32:33], gsm[:n, 33:34], gsm[:n, 34:35], gsm[:n, 35:36], gsm[:n, 36:37]
                G = gate_tile[:n, bi, :]
                m.op(m.DVE, lambda: nc.vector.tensor_tensor(out=lg, in0=plg[:n, 0:NEXP], in1=br[:n, :], op=ALU.add),
                     reads=[plg_b, wr_b], writes=[gsm_b])
                m.op(m.DVE, lambda: nc.vector.reduce_max(out=m1, in_=lg, axis=AX.X), reads=[gsm_b], writes=[gsm_b])
                m.op(m.DVE, lambda: nc.vector.tensor_scalar(out=eq1, in0=lg, scalar1=m1, scalar2=None, op0=ALU.is_equal),
                     reads=[gsm_b], writes=[gsm_b])
                m.op(m.DVE, lambda: nc.vector.scalar_tensor_tensor(out=lg2, in0=eq1, scalar=NEG, in1=lg, op0=ALU.mult, op1=ALU.add),
                     reads=[gsm_b], writes=[gsm_b])
                m.op(m.DVE, lambda: nc.vector.reduce_max(out=m2, in_=lg2, axis=AX.X), reads=[gsm_b], writes=[gsm_b])
                m.op(m.DVE, lambda: nc.vector.tensor_scalar(out=eq2, in0=lg2, scalar1=m2, scalar2=None, op0=ALU.is_equal),
                     reads=[gsm_b], writes=[gsm_b])
                m.op(m.DVE, lambda: nc.vector.tensor_tensor(out=dd, in0=m1, in1=m2, op=ALU.subtract), reads=[gsm_b], writes=[gsm_b])
                m.op(m.ACT, lambda: nc.scalar.activation(out=g1, in_=dd, func=AF.Sigmoid), reads=[gsm_b], writes=[gsm_b])
                m.op(m.DVE, lambda: nc.vector.tensor_scalar(out=g2, in0=g1, scalar1=-1.0, scalar2=1.0, op0=ALU.mult, op1=ALU.add),
                     reads=[gsm_b], writes=[gsm_b])
                m.op(m.DVE, lambda: nc.vector.tensor_scalar(out=G, in0=eq1, scalar1=g1, scalar2=None, op0=ALU.mult),
                     reads=[gsm_b], writes=[cx.gate_buf])
                m.op(m.DVE, lambda: nc.vector.scalar_tensor_tensor(out=G, in0=eq2, scalar=g2, in1=G, op0=ALU.mult, op1=ALU.add),
                     reads=[gsm_b], writes=[cx.gate_buf])
            m.barrier()
        m.bufs.append(cx.gate_buf)
        return gate_tile
    return gates


REST_INPUTS = [("ffn_w_gate", (D, FFN_DENSE)), ("ffn_w_up", (D, FFN_DENSE)), ("ffn_w_down", (FFN_DENSE, D)),
               ("ln_ffn_g0", (D,)), ("ln_ffn_b0", (D,)),
               ("pool_w_in", (D, D)), ("pool_w_group", (4, 512, 512)), ("pool_scale", (D,)), ("pool_w_out", (D, D)),
               ("ln_mix_g1", (D,)), ("ln_mix_b1", (D,)),
               ("moe_w_router", (D, NEXP)), ("moe_b_router", (NEXP,)),
               ("moe_w_gate", (NEXP, D, FFN_EXPERT)), ("moe_w_up", (NEXP, D, FFN_EXPERT)), ("moe_w_down", (NEXP, FFN_EXPERT, D)),
               ("ln_ffn_g1", (D,)), ("ln_ffn_b1", (D,))]


def build_full(stop_after=None):
    m = MK()
    nc = m.nc
    I = declare_inputs(nc, L0_INPUTS + REST_INPUTS)
    out = nc.dram_tensor("out", [NOWN, D], F32, kind="ExternalOutput").ap()
    out_b = m.buf()
    S = make_scratch(m)
    layer0_mixer(m, I, S)
    swiglu_phase(m, "f0", S["H1"], S["H1_b"], 0, NT, I["ffn_w_gate"], I["ffn_w_up"], I["ffn_w_down"], FFN_DENSE,
                 I["ln_ffn_g0"], I["ln_ffn_b0"], S["H2"], S["H2_b"])
    persist(m, S)
    pool_phase(m, I, S)
    swiglu_phase(m, "f1", S["H3"], S["H3_b"], 0, NOWN, I["moe_w_gate"], I["moe_w_up"], I["moe_w_down"], FFN_EXPERT,
                 I["ln_ffn_g1"], I["ln_ffn_b1"], out, out_b, gates=make_moe_gates(m, I, S), n_exp=NEXP)
    m.SP.wait(m.all_tokens())
    return m


def host_prep_all(inp):
    maps = host_prep_l0(inp)
    g = lambda k: np.asarray(inp[k], np.float32)
    common = {
        "ffn_w_gate": g("ffn_w_gate")[0], "ffn_w_up": g("ffn_w_up")[0], "ffn_w_down": g("ffn_w_down")[0],
        "ln_ffn_g0": g("ln_ffn_g")[0], "ln_ffn_b0": g("ln_ffn_b")[0],
        "pool_w_in": g("pool_w_in")[0], "pool_w_group": g("pool_w_group")[0], "pool_scale": g("pool_scale")[0],
        "pool_w_out": g("pool_w_out")[0],
        "ln_mix_g1": g("ln_mix_g")[1], "ln_mix_b1": g("ln_mix_b")[1],
        "moe_w_router": g("moe_w_router")[0], "moe_b_router": g("moe_b_router")[0],
        "moe_w_gate": g("moe_w_gate")[0], "moe_w_up": g("moe_w_up")[0], "moe_w_down": g("moe_w_down")[0],
        "ln_ffn_g1": g("ln_ffn_g")[1], "ln_ffn_b1": g("ln_ffn_b")[1],
    }
    for d in maps:
        d.update(common)
    return maps


_NC_CACHE = {}


def kernel(**inputs):
    if "m" not in _NC_CACHE:
        _NC_CACHE["m"] = build_full()
    m = _NC_CACHE["m"]
    maps = host_prep_all(inputs)
    res = run_bass_kernel_spmd(m.nc, maps, core_ids=list(range(NCORE)))
    out = np.concatenate([np.asarray(res.results[c]["out"], np.float32) for c in range(NCORE)], axis=0)
    return out.reshape(1, NCORE * NOWN, D)
```

```python
from contextlib import ExitStack
import numpy as np
import concourse.bass as bass
import concourse.mybir as mybir
from concourse.bass_utils import run_bass_kernel_spmd

F32 = mybir.dt.float32
BF16 = mybir.dt.bfloat16
AF = mybir.ActivationFunctionType
ALU = mybir.AluOpType
AX = mybir.AxisListType

D = 2048
DC = 16
NCORE = 8
NOWN = 1024
NHALO = 16
NT = NOWN + NHALO
LFULL = 8192 + 16
NOTH = LFULL - NT
ALPHA = 4 ** 0.25
LN_EPS = 1e-5
RMS_EPS = 1e-6
FFN_DENSE = 5632
FFN_EXPERT = 7168
NEXP = 8
NEG = -1e30


class EngW:
    def __init__(self, m, e, name):
        self.m = m
        self.e = e
        self.name = name
        self.sem = m.es.enter_context(m.nc.semaphore("s_" + name))
        self.n = 0
        self.seen = {}

    def wait(self, toks):
        best = {}
        for t in toks:
            if t is None:
                continue
            sem, val = t
            if best.get(sem, 0) < val:
                best[sem] = val
        for sem, val in best.items():
            if self.seen.get(sem, 0) >= val:
                continue
            self.e.wait_ge(sem, val)
            self.seen[sem] = val

    def done(self, ins):
        self.n += 1
        ins.then_inc(self.sem, 1)
        return (self.sem, self.n)


class Buf:
    __slots__ = ("w", "r", "name")

    def __init__(self, name=""):
        self.w = None
        self.r = {}
        self.name = name

    def add_read(self, tok):
        sem, val = tok
        if self.r.get(sem, 0) < val:
            self.r[sem] = val


class MK:
    def __init__(self):
        self.es = ExitStack()
        self.nc = bass.Bass("TRN2", target_bir_lowering=False)
        nc = self.nc
        self.PE = EngW(self, nc.tensor, "pe")
        self.ACT = EngW(self, nc.scalar, "act")
        self.DVE = EngW(self, nc.vector, "dve")
        self.POOL = EngW(self, nc.gpsimd, "pool")
        self.SP = EngW(self, nc.sync, "sp")
        self.engs = [self.PE, self.ACT, self.DVE, self.POOL, self.SP]
        self.dsem = {}
        for q, n in ((self.SP, 20), (self.POOL, 20), (self.ACT, 8)):
            self.dsem[q.name] = [[self.es.enter_context(nc.semaphore("d_%s%d" % (q.name, i))), 0] for i in range(n)]
        self.dptr = {"sp": 0, "pool": 0, "act": 0}
        self.bufs = []

    def buf(self, name=""):
        b = Buf(name)
        self.bufs.append(b)
        return b

    def sb(self, name, shape, dt):
        return self.es.enter_context(self.nc.sbuf_tensor(name, shape, dt))

    def _deps(self, reads, writes):
        toks = []
        for b in reads:
            toks.append(b.w)
        for b in writes:
            toks.append(b.w)
            for sem, val in b.r.items():
                toks.append((sem, val))
        return toks

    def _commit(self, tok, reads, writes):
        for b in reads:
            b.add_read(tok)
        for b in writes:
            b.w = tok
            b.r = {}

    def op(self, eng, fn, reads=(), writes=()):
        eng.wait(self._deps(reads, writes))
        ins = fn()
        tok = eng.done(ins)
        self._commit(tok, reads, writes)
        return tok

    def dma(self, q, out, in_, reads=(), writes=()):
        lst = self.dsem[q.name]
        i = self.dptr[q.name]
        self.dptr[q.name] = (i + 1) % len(lst)
        sem, tot = lst[i]
        deps = self._deps(reads, writes)
        if tot > 0:
            deps.append((sem, tot))
        q.wait(deps)
        q.e.dma_start(out=out, in_=in_).then_inc(sem, 16)
        lst[i][1] = tot + 16
        tok = (sem, tot + 16)
        self._commit(tok, reads, writes)
        return tok

    def all_tokens(self):
        toks = []
        for e in self.engs:
            if e.n > 0:
                toks.append((e.sem, e.n))
        for name, lst in self.dsem.items():
            for sem, tot in lst:
                if tot > 0:
                    toks.append((sem, tot))
        return toks

    def barrier(self):
        toks = self.all_tokens()
        for e in self.engs:
            e.wait(toks)
        for b in self.bufs:
            b.w = None
            b.r = {}
        self.bufs = []


def tok_blocks(ntok):
    out = []
    r = ntok % 128
    t = 0
    if r:
        out.append((0, r))
        t = r
    while t < ntok:
        out.append((t, 128))
        t += 128
    return out


def tok_groups(ntok, maxn=512):
    ng = -(-ntok // maxn)
    base = -(-ntok // ng)
    out = []
    t = 0
    while t < ntok:
        n = min(base, ntok - t)
        out.append((t, n))
        t += n
    return out


class Ctx:
    pass


def make_identity(m, ident_bf, ident_f32=None):
    nc = m.nc
    b = m.buf("ident")
    def f():
        nc.gpsimd.memset(ident_bf[:], 1.0)
        return nc.gpsimd.affine_select(out=ident_bf[:], in_=ident_bf[:], pattern=[[-1, 128]],
                                       compare_op=ALU.is_equal, fill=0.0, base=0, channel_multiplier=1)
    m.op(m.POOL, f, writes=[b])
    b2 = None
    if ident_f32 is not None:
        b2 = m.buf("identf")
        def g():
            nc.gpsimd.memset(ident_f32[:], 1.0)
            return nc.gpsimd.affine_select(out=ident_f32[:], in_=ident_f32[:], pattern=[[-1, 128]],
                                           compare_op=ALU.is_equal, fill=0.0, base=0, channel_multiplier=1)
        m.op(m.POOL, g, writes=[b2])
    return b, b2


def load_fm(m, H_dram, row0, ntok, hT, hT_buf, ident, ident_buf, stage, stage_bufs, ps_t, ps_bufs, Hbuf=None, q=None):
    nc = m.nc
    k = 0
    first = True
    for bi, (t0, n) in enumerate(tok_blocks(ntok)):
        st, sb_ = stage[bi % 2], stage_bufs[bi % 2]
        m.dma(q or m.POOL, st[:n, :], H_dram[row0 + t0: row0 + t0 + n, :], reads=([Hbuf] if Hbuf else []), writes=[sb_])
        for half in range(2):
            ps, pb = ps_t[k % 2], ps_bufs[k % 2]
            k += 1
            def f():
                ins = None
                for j in range(8):
                    c = half * 8 + j
                    ins = nc.tensor.transpose(ps[:, j * 128: j * 128 + n], st[:n, c * 128:(c + 1) * 128], ident[:n, :n])
                return ins
            m.op(m.PE, f, reads=[sb_, ident_buf], writes=[pb])
            src = ps[:, :].rearrange("p (j t) -> p j t", j=8)[:, :, :n]
            dst = hT[:, half * 8:(half + 1) * 8, t0:t0 + n]
            if k % 2 == 0:
                m.op(m.DVE, lambda: nc.vector.tensor_copy(out=dst, in_=src), reads=[pb], writes=[hT_buf] if first else [hT_buf])
            else:
                m.op(m.ACT, lambda: nc.scalar.copy(out=dst, in_=src), reads=[pb], writes=[hT_buf])
            first = False


def layer_norm_block(m, cx, src_ps, src_bufs, Hin_dram, row, n, Hout_dram, gam, bet, out_row=None):
    nc = m.nc
    i = cx.ln_i
    cx.ln_i += 1
    hres, hb = cx.hres[i % 2], cx.hres_bufs[i % 2]
    tln, tb = cx.tln[i % 2], cx.tln_bufs[i % 2]
    st, sbf = cx.stat[i % 2], cx.stat_bufs[i % 2]
    m.dma(m.SP, hres[:n, :], Hin_dram[row:row + n, :], reads=cx.Hin_bufs, writes=[hb])
    m.op(m.DVE, lambda: nc.vector.scalar_tensor_tensor(out=tln[:n, :], in0=hres[:n, :], scalar=ALPHA, in1=src_ps,
                                                       op0=ALU.mult, op1=ALU.add),
         reads=[hb] + list(src_bufs), writes=[tb])
    def fstats():
        ins = None
        for j in range(4):
            ins = nc.vector.bn_stats(out=st[:n, j * 6:(j + 1) * 6], in_=tln[:n, j * 512:(j + 1) * 512])
        return ins
    m.op(m.DVE, fstats, reads=[tb], writes=[sbf])
    m.op(m.DVE, lambda: nc.vector.bn_aggr(out=st[:n, 24:26], in_=st[:n, 0:24]), reads=[sbf], writes=[sbf])
    m.op(m.DVE, lambda: nc.vector.tensor_scalar(out=st[:n, 28:29], in0=st[:n, 25:26], scalar1=LN_EPS, scalar2=None,
                                                op0=ALU.add), reads=[sbf], writes=[sbf])
    m.op(m.ACT, lambda: nc.scalar.sqrt(out=st[:n, 29:30], in_=st[:n, 28:29]), reads=[sbf], writes=[sbf])
    m.op(m.DVE, lambda: nc.vector.reciprocal(out=st[:n, 26:27], in_=st[:n, 29:30]), reads=[sbf], writes=[sbf])
    m.op(m.DVE, lambda: nc.vector.scalar_tensor_tensor(out=st[:n, 27:28], in0=st[:n, 24:25], scalar=-1.0, in1=st[:n, 26:27],
                                                       op0=ALU.mult, op1=ALU.mult), reads=[sbf], writes=[sbf])
    m.op(m.ACT, lambda: nc.scalar.activation(out=tln[:n, :], in_=tln[:n, :], func=AF.Identity,
                                             bias=st[:n, 27:28], scale=st[:n, 26:27]), reads=[sbf], writes=[tb])
    m.op(m.POOL, lambda: nc.gpsimd.tensor_tensor(out=tln[:n, :], in0=tln[:n, :], in1=gam[:n, :], op=ALU.mult),
         reads=[cx.gb_buf], writes=[tb])
    m.op(m.POOL, lambda: nc.gpsimd.tensor_tensor(out=tln[:n, :], in0=tln[:n, :], in1=bet[:n, :], op=ALU.add),
         reads=[cx.gb_buf], writes=[tb])
    orow = row if out_row is None else out_row
    m.dma(m.SP, Hout_dram[orow:orow + n, :], tln[:n, :], reads=[tb], writes=[cx.Hout_buf])


def ln_setup(m, cx, es, tag, g_dram, b_dram, Hin_bufs, Hout_buf):
    nc = m.nc
    cx.ln_i = 0
    cx.hres = [es.enter_context(nc.sbuf_tensor("hres%s%d" % (tag, i), [128, D], F32)) for i in range(2)]
    cx.tln = [es.enter_context(nc.sbuf_tensor("tln%s%d" % (tag, i), [128, D], F32)) for i in range(2)]
    cx.stat = [es.enter_context(nc.sbuf_tensor("stat%s%d" % (tag, i), [128, 32], F32)) for i in range(2)]
    cx.hres_bufs = [m.buf() for _ in range(2)]
    cx.tln_bufs = [m.buf() for _ in range(2)]
    cx.stat_bufs = [m.buf() for _ in range(2)]
    cx.gam = es.enter_context(nc.sbuf_tensor("gam" + tag, [128, D], F32))
    cx.bet = es.enter_context(nc.sbuf_tensor("bet" + tag, [128, D], F32))
    cx.gb_buf = m.buf()
    m.dma(m.SP, cx.gam[:, :], g_dram.partition_broadcast(128), writes=[cx.gb_buf])
    m.dma(m.SP, cx.bet[:, :], b_dram.partition_broadcast(128), writes=[cx.gb_buf])
    cx.Hin_bufs = Hin_bufs
    cx.Hout_buf = Hout_buf


def swiglu_phase(m, tag, Hin, Hin_buf, row0, ntok, wg, wu, wd, F, g_dram, b_dram, Hout, Hout_buf,
                 gates=None, n_exp=1):
    nc = m.nc
    FC = 256
    nfc = F // FC
    with ExitStack() as es:
        cx = Ctx()
        blocks = tok_blocks(ntok)
        groups = tok_groups(ntok)
        nb = len(blocks)
        hT = es.enter_context(nc.sbuf_tensor("hT" + tag, [128, DC, ntok], BF16))
        hT_buf = m.buf()
        ident = es.enter_context(nc.sbuf_tensor("ident" + tag, [128, 128], BF16))
        ident_buf, _ = make_identity(m, ident)
        yacc = es.enter_context(nc.sbuf_tensor("yacc" + tag, [128, nb, D], F32))
        yacc_bufs = [m.buf() for _ in range(nb)]
        esw = ExitStack()
        wgt = [esw.enter_context(nc.sbuf_tensor("wg%s%d" % (tag, i), [128, DC, FC], BF16)) for i in range(2)]
        wut = [esw.enter_context(nc.sbuf_tensor("wu%s%d" % (tag, i), [128, DC, FC], BF16)) for i in range(2)]
        wdt = [esw.enter_context(nc.sbuf_tensor("wd%s%d" % (tag, i), [128, FC // 128, D], BF16)) for i in range(2)]
        wg_b = [m.buf() for _ in range(2)]
        wu_b = [m.buf() for _ in range(2)]
        wd_b = [m.buf() for _ in range(2)]
        hid = [esw.enter_context(nc.sbuf_tensor("hid%s%d" % (tag, i), [128, FC // 128, ntok], BF16)) for i in range(2)]
        hid_b = [m.buf() for _ in range(2)]
        gs = [esw.enter_context(nc.sbuf_tensor("gs%s%d" % (tag, i), [128, 512], F32)) for i in range(2)]
        gs_b = [m.buf() for _ in range(2)]
        pg = [es.enter_context(nc.psum_tensor("pg%s%d" % (tag, i), [128, 512], F32)) for i in range(2)]
        pu = [es.enter_context(nc.psum_tensor("pu%s%d" % (tag, i), [128, 512], F32)) for i in range(2)]
        py = [es.enter_context(nc.psum_tensor("py%s%d" % (tag, i), [128, 1024], F32)) for i in range(2)]
        pg_b = [m.buf() for _ in range(2)]
        pu_b = [m.buf() for _ in range(2)]
        py_b = [m.buf() for _ in range(2)]
        gate_tile = None
        if gates is not None:
            gate_tile = esw.enter_context(nc.sbuf_tensor("gatet" + tag, [128, nb, n_exp], F32))
            cx.gate_tile = gate_tile
            cx.pg, cx.pg_b, cx.pu, cx.pu_b = pg, pg_b, pu, pu_b
        with ExitStack() as es2:
            stage = [es2.enter_context(nc.sbuf_tensor("stg%s%d" % (tag, i), [128, D], BF16)) for i in range(2)]
            stage_b = [m.buf() for _ in range(2)]
            pst = [pg[i][:, :].bitcast(BF16) for i in range(2)]
            load_fm(m, Hin, row0, ntok, hT, hT_buf, ident, ident_buf, stage, stage_b, pst, pg_b, Hbuf=Hin_buf)
            m.barrier()
        if gates is not None:
            gates(cx, es, hT, hT_buf, blocks)
        kk = 0
        gi = 0
        for e in range(n_exp):
            wg_e = wg[e] if n_exp > 1 else wg
            wu_e = wu[e] if n_exp > 1 else wu
            wd_e = wd[e] if n_exp > 1 else wd
            wg_v = wg_e.rearrange("(c p) f -> p c f", p=128)
            wu_v = wu_e.rearrange("(c p) f -> p c f", p=128)
            wd_v = wd_e.rearrange("(j p) d -> p j d", p=128)
            for fc in range(nfc):
                s = kk % 2
                kk += 1
                f0 = fc * FC
                for c0 in range(0, DC, 4):
                    m.dma(m.POOL, wgt[s][:, c0:c0 + 4, :], wg_v[:, c0:c0 + 4, f0:f0 + FC], writes=[wg_b[s]])
                    m.dma(m.POOL, wut[s][:, c0:c0 + 4, :], wu_v[:, c0:c0 + 4, f0:f0 + FC], writes=[wu_b[s]])
                m.dma(m.POOL, wdt[s][:, :, :], wd_v[:, f0 // 128: f0 // 128 + FC // 128, :], writes=[wd_b[s]])
                for (t0, n) in groups:
                    for fb in range(FC // 128):
                        p = gi % 2
                        gi += 1
                        def fgate():
                            ins = None
                            for c in range(DC):
                                ins = nc.tensor.matmul(pg[p][:, :n], lhsT=wgt[s][:, c, fb * 128:(fb + 1) * 128],
                                                       rhs=hT[:, c, t0:t0 + n], start=(c == 0), stop=(c == DC - 1))
                            return ins
                        m.op(m.PE, fgate, reads=[wg_b[s], hT_buf], writes=[pg_b[p]])
                        def fup():
                            ins = None
                            for c in range(DC):
                                ins = nc.tensor.matmul(pu[p][:, :n], lhsT=wut[s][:, c, fb * 128:(fb + 1) * 128],
                                                       rhs=hT[:, c, t0:t0 + n], start=(c == 0), stop=(c == DC - 1))
                            return ins
                        m.op(m.PE, fup, reads=[wu_b[s], hT_buf], writes=[pu_b[p]])
                        m.op(m.ACT, lambda: nc.scalar.activation(out=gs[p][:, :n], in_=pg[p][:, :n], func=AF.Silu),
                             reads=[pg_b[p]], writes=[gs_b[p]])
                        m.op(m.DVE, lambda: nc.vector.tensor_tensor(out=hid[s][:, fb, t0:t0 + n], in0=gs[p][:, :n],
                                                                    in1=pu[p][:, :n], op=ALU.mult),
                             reads=[gs_b[p], pu_b[p]], writes=[hid_b[s]])
                for bi, (t0, n) in enumerate(blocks):
                    for half in range(2):
                        p = gi % 2
                        gi += 1
                        def fdown():
                            ins = None
                            for q in range(2):
                                for j in range(FC // 128):
                                    ins = nc.tensor.matmul(py[p][:n, q * 512:(q + 1) * 512], lhsT=hid[s][:, j, t0:t0 + n],
                                                           rhs=wdt[s][:, j, half * 1024 + q * 512: half * 1024 + (q + 1) * 512],
                                                           start=(j == 0), stop=(j == FC // 128 - 1))
                            return ins
                        m.op(m.PE, fdown, reads=[hid_b[s], wd_b[s]], writes=[py_b[p]])
                        ya = yacc[:n, bi, half * 1024:(half + 1) * 1024]
                        if e == 0 and fc == 0:
                            if gate_tile is None:
                                m.op(m.DVE, lambda: nc.vector.tensor_copy(out=ya, in_=py[p][:n, :]),
                                     reads=[py_b[p]], writes=[yacc_bufs[bi]])
                            else:
                                m.op(m.DVE, lambda: nc.vector.tensor_scalar(out=ya, in0=py[p][:n, :],
                                                                            scalar1=gate_tile[:n, bi, e:e + 1], scalar2=None,
                                                                            op0=ALU.mult),
                                     reads=[py_b[p], cx.gate_buf], writes=[yacc_bufs[bi]])
                        else:
                            sc = 1.0 if gate_tile is None else gate_tile[:n, bi, e:e + 1]
                            rd = [py_b[p]] + ([cx.gate_buf] if gate_tile is not None else [])
                            m.op(m.DVE, lambda: nc.vector.scalar_tensor_tensor(out=ya, in0=py[p][:n, :], scalar=sc, in1=ya,
                                                                               op0=ALU.mult, op1=ALU.add),
                                 reads=rd, writes=[yacc_bufs[bi]])
        m.barrier()
        esw.close()
        ln_setup(m, cx, es, tag, g_dram, b_dram, [Hin_buf], Hout_buf)
        for bi, (t0, n) in enumerate(blocks):
            layer_norm_block(m, cx, yacc[:n, bi, :], [yacc_bufs[bi]], Hin, row0 + t0, n, Hout, cx.gam, cx.bet)
        m.barrier()


KGROUPS = [(0, 16)] + [(16 + 512 * i, 512) for i in range(16)]
SC_MLA = 192 ** -0.5
SC_FOX = 128 ** -0.5


def load_w(m, q, tile, wv, c0=0, nchunk=None, step=4, bufs=None):
    nchunk = tile.shape[1] if nchunk is None else nchunk
    for c in range(0, nchunk, step):
        e = min(nchunk, c + step)
        m.dma(q, tile[:, c:e, :], wv[:, c:e, :], writes=bufs)


def xgroup_fm(m, cx, xkv, r0, n, xT, xT_buf):
    nc = m.nc
    blocks = [(0, n)] if n <= 128 else [(i * 128, 128) for i in range(n // 128)]
    st, sb_ = cx.stage[cx.si % 2], cx.stage_b[cx.si % 2]
    cx.si += 1
    if n <= 128:
        m.dma(m.POOL, st[:n, 0, :], xkv[r0:r0 + n, :], writes=[sb_])
    else:
        m.dma(m.POOL, st[:, :n // 128, :], xkv[r0:r0 + n, :].rearrange("(b p) d -> p b d", p=128), writes=[sb_])
    for bi, (t0, bn) in enumerate(blocks):
        for half in range(2):
            k = cx.ti % 2
            cx.ti += 1
            ps, pb = cx.pst[k], cx.pst_b[k]
            def f():
                ins = None
                for j in range(8):
                    c = half * 8 + j
                    ins = nc.tensor.transpose(ps[:, j * 128: j * 128 + bn], st[:bn, bi, c * 128:(c + 1) * 128],
                                              cx.ident[:bn, :bn])
                return ins
            m.op(m.PE, f, reads=[sb_, cx.ident_buf], writes=[pb])
            src = ps[:, :].rearrange("p (j t) -> p j t", j=8)[:, :, :bn]
            dst = xT[:, half * 8:(half + 1) * 8, t0:t0 + bn]
            if k == 0:
                m.op(m.DVE, lambda: nc.vector.tensor_copy(out=dst, in_=src), reads=[pb], writes=[xT_buf])
            else:
                m.op(m.ACT, lambda: nc.scalar.copy(out=dst, in_=src), reads=[pb], writes=[xT_buf])
    return blocks


def rms_norm_tm(m, cx, src_ps, src_buf, n, gam_bc, out_bf, out_buf):
    nc = m.nc
    i = cx.ri % 2
    cx.ri += 1
    sq, sqb = cx.rsq[i], cx.rsq_b[i]
    st, stb = cx.rst[i], cx.rst_b[i]
    m.op(m.ACT, lambda: nc.scalar.activation(out=sq[:n, :], in_=src_ps, func=AF.Square, accum_out=st[:n, 0:1]),
         reads=[src_buf], writes=[sqb, stb])
    m.op(m.DVE, lambda: nc.vector.tensor_scalar(out=st[:n, 1:2], in0=st[:n, 0:1], scalar1=1.0 / 512, scalar2=RMS_EPS,
                                                op0=ALU.mult, op1=ALU.add), reads=[stb], writes=[stb])
    m.op(m.ACT, lambda: nc.scalar.sqrt(out=st[:n, 3:4], in_=st[:n, 1:2]), reads=[stb], writes=[stb])
    m.op(m.DVE, lambda: nc.vector.reciprocal(out=st[:n, 2:3], in_=st[:n, 3:4]), reads=[stb], writes=[stb])
    m.op(m.DVE, lambda: nc.vector.scalar_tensor_tensor(out=out_bf, in0=src_ps, scalar=st[:n, 2:3], in1=gam_bc[:n, :],
                                                       op0=ALU.mult, op1=ALU.mult),
         reads=[src_buf, stb, cx.g_buf], writes=[out_buf])


def transpose_tm_to_fm(m, cx, src_bf, src_buf, n, nchunk, dstT, dst_buf, t0):
    nc = m.nc
    k = cx.ti % 2
    cx.ti += 1
    ps, pb = cx.pst[k], cx.pst_b[k]
    def f():
        ins = None
        for j in range(nchunk):
            ins = nc.tensor.transpose(ps[:, j * 128: j * 128 + n], src_bf[:n, j * 128:(j + 1) * 128], cx.ident[:n, :n])
        return ins
    m.op(m.PE, f, reads=[src_buf, cx.ident_buf], writes=[pb])
    src = ps[:, 0:nchunk * 128].rearrange("p (j t) -> p j t", j=nchunk)[:, :, :n]
    m.op(m.DVE, lambda: nc.vector.tensor_copy(out=dstT[:, 0:nchunk, t0:t0 + n], in_=src), reads=[pb], writes=[dst_buf])


def rope_tm(m, cx, src, src_buf, n, nh, cs, cs_buf, out_bf, out_buf, scale):
    nc = m.nc
    i = cx.rpi % 2
    cx.rpi += 1
    t1, t1b = cx.rp[i], cx.rp_b[i]
    for h in range(nh):
        x1 = src[:, h * 64: h * 64 + 32]
        x2 = src[:, h * 64 + 32: h * 64 + 64]
        cos = cs[:n, 0:32]
        sin = cs[:n, 32:64]
        a = t1[:n, 0:32]
        b = t1[:n, 32:64]
        c_ = t1[:n, 64:96]
        d_ = t1[:n, 96:128]
        def f():
            nc.vector.tensor_tensor(out=a, in0=x1, in1=cos, op=ALU.mult)
            nc.vector.tensor_tensor(out=b, in0=x2, in1=sin, op=ALU.mult)
            nc.vector.tensor_tensor(out=c_, in0=x2, in1=cos, op=ALU.mult)
            return nc.vector.tensor_tensor(out=d_, in0=x1, in1=sin, op=ALU.mult)
        m.op(m.DVE, f, reads=[src_buf, cs_buf], writes=[t1b])
        def g():
            nc.vector.scalar_tensor_tensor(out=out_bf[:n, h * 64: h * 64 + 32], in0=a, scalar=scale, in1=a, op0=ALU.mult, op1=ALU.bypass) if False else None
            nc.vector.tensor_tensor(out=a, in0=a, in1=b, op=ALU.subtract)
            return nc.vector.tensor_tensor(out=c_, in0=c_, in1=d_, op=ALU.add)
        m.op(m.DVE, g, reads=[t1b], writes=[t1b])
        def h2():
            nc.vector.tensor_scalar(out=out_bf[:n, h * 64: h * 64 + 32], in0=a, scalar1=scale, scalar2=None, op0=ALU.mult)
            return nc.vector.tensor_scalar(out=out_bf[:n, h * 64 + 32: h * 64 + 64], in0=c_, scalar1=scale, scalar2=None,
                                           op0=ALU.mult)
        m.op(m.DVE, h2, reads=[t1b], writes=[out_buf])


def kv_common_setup(m, cx, es, tag):
    nc = m.nc
    cx.si = cx.ti = cx.ri = cx.rpi = cx.ei = 0
    cx.ident = es.enter_context(nc.sbuf_tensor("ident" + tag, [128, 128], BF16))
    cx.ident_buf, _ = make_identity(m, cx.ident)
    cx.stage = [es.enter_context(nc.sbuf_tensor("stg%s%d" % (tag, i), [128, 4, D], BF16)) for i in range(2)]
    cx.stage_b = [m.buf() for _ in range(2)]
    cx.pst_f = [es.enter_context(nc.psum_tensor("pst%s%d" % (tag, i), [128, 512], F32)) for i in range(2)]
    cx.pst = [t[:, :].bitcast(BF16) for t in cx.pst_f]
    cx.pst_b = [m.buf() for _ in range(2)]
    cx.pm = [es.enter_context(nc.psum_tensor("pm%s%d" % (tag, i), [128, 1024], F32)) for i in range(3)]
    cx.pm_b = [m.buf() for _ in range(3)]
    cx.pmi = 0
    cx.ev = [es.enter_context(nc.sbuf_tensor("ev%s%d" % (tag, i), [128, 1024], BF16)) for i in range(3)]
    cx.ev_b = [m.buf() for _ in range(3)]


def next_pm(cx):
    i = cx.pmi % 3
    cx.pmi += 1
    return cx.pm[i], cx.pm_b[i]


def next_ev(cx):
    i = cx.ei % 3
    cx.ei += 1
    return cx.ev[i], cx.ev_b[i]


def evac(m, cx, k, out, in_, reads, writes, scale=None):
    nc = m.nc
    if k % 2 == 0:
        if scale is None:
            m.op(m.DVE, lambda: nc.vector.tensor_copy(out=out, in_=in_), reads=reads, writes=writes)
        else:
            m.op(m.DVE, lambda: nc.vector.tensor_scalar(out=out, in0=in_, scalar1=scale, scalar2=None, op0=ALU.mult),
                 reads=reads, writes=writes)
    else:
        if scale is None:
            m.op(m.ACT, lambda: nc.scalar.copy(out=out, in_=in_), reads=reads, writes=writes)
        else:
            m.op(m.ACT, lambda: nc.scalar.mul(out=out, in_=in_, mul=scale), reads=reads, writes=writes)


def fm_heads(m, cx, w_t, w_buf, nkc, rhsT, rhs_buf, n, heads, dst_dram, col0, dst_buf, scale=None, hw=128):
    nc = m.nc
    for h in range(heads):
        ps, pb = next_pm(cx)
        def f():
            ins = None
            for kc in range(nkc):
                ins = nc.tensor.matmul(ps[:hw, :n], lhsT=w_t[:, kc, h * hw:(h + 1) * hw], rhs=rhsT[:, kc, :n],
                                       start=(kc == 0), stop=(kc == nkc - 1))
            return ins
        m.op(m.PE, f, reads=[w_buf, rhs_buf], writes=[pb])
        ev, evb = next_ev(cx)
        evac(m, cx, h, ev[:hw, :n], ps[:hw, :n], [pb], [evb], scale)
        m.dma(m.SP, dst_dram[h, :, col0:col0 + n], ev[:hw, :n], reads=[evb], writes=[dst_buf])


def tm_out(m, cx, lhsT, lhs_buf, nkc, t0, bn, w_t, w_buf, wc0, ncols):
    nc = m.nc
    ps, pb = next_pm(cx)
    def f():
        ins = None
        for q0 in range(0, ncols, 512):
            qn = min(512, ncols - q0)
            for kc in range(nkc):
                ins = nc.tensor.matmul(ps[:bn, q0:q0 + qn], lhsT=lhsT[:, kc, t0:t0 + bn],
                                       rhs=w_t[:, kc, wc0 + q0: wc0 + q0 + qn], start=(kc == 0), stop=(kc == nkc - 1))
        return ins
    m.op(m.PE, f, reads=[lhs_buf, w_buf], writes=[pb])
    return ps, pb


def kv_pass_mla(m, xkv, cs_all, w_in, w_ukv_p, w_uq_p, kvn, qn_, kvalid, S):
    nc = m.nc
    with ExitStack() as es0:
        es = es0
        kvs = es.enter_context(nc.sbuf_tensor("kvs", [1, LFULL], F32))
        kvs16 = es.enter_context(nc.sbuf_tensor("kvs16", [1, LFULL], BF16))
        kvs_b = m.buf()
        m.dma(m.SP, kvs[:, :], kvalid.rearrange("(o n) -> o n", o=1), writes=[kvs_b])
        m.op(m.DVE, lambda: nc.vector.tensor_copy(out=kvs16[:, :], in_=kvs[:, :]), reads=[kvs_b], writes=[kvs_b])
        m.dma(m.SP, S["KAUX_M"][64:65, :], kvs16[:, :], reads=[kvs_b], writes=[S["KAUX_M_b"]])
        m.op(m.DVE, lambda: nc.vector.memset(kvs16[:, 0:NT], 1.0), writes=[kvs_b])
        for h in range(8):
            m.dma(m.SP, S["QAUX_M"][h, 64:65, :], kvs16[:, 0:NT], reads=[kvs_b], writes=[S["QAUX_M_b"]])
        m.barrier()
    persist(m, S)
    with ExitStack() as es:
        cx = Ctx()
        kv_common_setup(m, cx, es, "a1")
        win_v = w_in.rearrange("(c p) f -> p c f", p=128)
        wa = es.enter_context(nc.sbuf_tensor("wa", [128, DC, 1088], BF16))
        wukv = es.enter_context(nc.sbuf_tensor("wukv", [128, 4, 2048], BF16))
        wuq = es.enter_context(nc.sbuf_tensor("wuq", [128, 4, 1536], BF16))
        w_b = m.buf()
        load_w(m, m.POOL, wa, win_v[:, :, 0:1088], bufs=[w_b])
        load_w(m, m.POOL, wukv, w_ukv_p.rearrange("(c p) f -> p c f", p=128), bufs=[w_b])
        load_w(m, m.POOL, wuq, w_uq_p.rearrange("(c p) f -> p c f", p=128), bufs=[w_b])
        gkv = es.enter_context(nc.sbuf_tensor("gkv", [128, 512], F32))
        gq = es.enter_context(nc.sbuf_tensor("gq", [128, 512], F32))
        cx.g_buf = m.buf()
        m.dma(m.SP, gkv[:, :], kvn.partition_broadcast(128), writes=[cx.g_buf])
        m.dma(m.SP, gq[:, :], qn_.partition_broadcast(128), writes=[cx.g_buf])
        xT = [es.enter_context(nc.sbuf_tensor("xTa%d" % i, [128, DC, 512], BF16)) for i in range(2)]
        xT_b = [m.buf() for _ in range(2)]
        cT = [es.enter_context(nc.sbuf_tensor("cTa%d" % i, [128, 4, 512], BF16)) for i in range(2)]
        cT_b = [m.buf() for _ in range(2)]
        cqT = [es.enter_context(nc.sbuf_tensor("cqTa%d" % i, [128, 4, 512], BF16)) for i in range(2)]
        cqT_b = [m.buf() for _ in range(2)]
        cx.rsq = [es.enter_context(nc.sbuf_tensor("rsq%d" % i, [128, 512], F32)) for i in range(2)]
        cx.rsq_b = [m.buf() for _ in range(2)]
        cx.rst = [es.enter_context(nc.sbuf_tensor("rst%d" % i, [128, 4], F32)) for i in range(2)]
        cx.rst_b = [m.buf() for _ in range(2)]
        cx.rp = [es.enter_context(nc.sbuf_tensor("rp%d" % i, [128, 128], F32)) for i in range(2)]
        cx.rp_b = [m.buf() for _ in range(2)]
        cn = [es.enter_context(nc.sbuf_tensor("cn%d" % i, [128, 512], BF16)) for i in range(2)]
        cn_b = [m.buf() for _ in range(2)]
        cst = [es.enter_context(nc.sbuf_tensor("cst%d" % i, [128, 64], F32)) for i in range(2)]
        cst_b = [m.buf() for _ in range(2)]
        kr = [es.enter_context(nc.sbuf_tensor("kr%d" % i, [128, 64], BF16)) for i in range(2)]
        kr_b = [m.buf() for _ in range(2)]
        qr = [es.enter_context(nc.sbuf_tensor("qr%d" % i, [128, 512], BF16)) for i in range(2)]
        qr_b = [m.buf() for _ in range(2)]
        krT = [es.enter_context(nc.sbuf_tensor("krT%d" % i, [64, 512], BF16)) for i in range(2)]
        krT_b = [m.buf() for _ in range(2)]
        qrT = [es.enter_context(nc.sbuf_tensor("qrT%d" % i, [64, 8, 128], BF16)) for i in range(2)]
        qrT_b = [m.buf() for _ in range(2)]
        bi_glob = 0
        for gi, (r0, n) in enumerate(KGROUPS):
            own = r0 < NT
            x_t, x_b = xT[gi % 2], xT_b[gi % 2]
            c_t, c_b = cT[gi % 2], cT_b[gi % 2]
            blocks = xgroup_fm(m, cx, xkv, r0, n, x_t, x_b)
            for (t0, bn) in blocks:
                j = bi_glob % 2
                bi_glob += 1
                m.dma(m.SP, cst[j][:bn, :], cs_all[r0 + t0: r0 + t0 + bn, :], writes=[cst_b[j]])
                ps, pb = tm_out(m, cx, x_t, x_b, DC, t0, bn, wa, w_b, 512, 576)
                rms_norm_tm(m, cx, ps[:bn, 0:512], pb, bn, gkv, cn[j][:bn, :], cn_b[j])
                transpose_tm_to_fm(m, cx, cn[j], cn_b[j], bn, 4, c_t, c_b, t0)
                rope_tm(m, cx, ps[:bn, 512:576], pb, bn, 1, cst[j], cst_b[j], kr[j], kr_b[j], 1.0)
                k = cx.ti % 2
                cx.ti += 1
                pt, ptb = cx.pst[k], cx.pst_b[k]
                m.op(m.PE, lambda: nc.tensor.transpose(pt[:64, :bn], kr[j][:bn, 0:64], cx.ident[:bn, :bn]),
                     reads=[kr_b[j], cx.ident_buf], writes=[ptb])
                m.op(m.ACT, lambda: nc.scalar.copy(out=krT[gi % 2][:, t0:t0 + bn], in_=pt[:64, :bn]), reads=[ptb],
                     writes=[krT_b[gi % 2]])
                ps2, pb2 = tm_out(m, cx, c_t, c_b, 4, t0, bn, wukv, w_b, 1024, 1024)
                ev, evb = next_ev(cx)
                evac(m, cx, bi_glob, ev[:bn, :], ps2[:bn, :], [pb2], [evb])
                m.dma(m.SP, S["VV"][r0 + t0: r0 + t0 + bn, 0:8, :], ev[:bn, :].rearrange("p (h d) -> p h d", h=8),
                      reads=[evb], writes=[S["VV_b"]])
                if own:
                    cq_t, cq_b = cqT[gi % 2], cqT_b[gi % 2]
                    ps3, pb3 = tm_out(m, cx, x_t, x_b, DC, t0, bn, wa, w_b, 0, 512)
                    rms_norm_tm(m, cx, ps3[:bn, 0:512], pb3, bn, gq, cn[j][:bn, :], cn_b[j])
                    transpose_tm_to_fm(m, cx, cn[j], cn_b[j], bn, 4, cq_t, cq_b, t0)
                    ps4, pb4 = tm_out(m, cx, cq_t, cq_b, 4, t0, bn, wuq, w_b, 1024, 512)
                    rope_tm(m, cx, ps4[:bn, 0:512], pb4, bn, 8, cst[j], cst_b[j], qr[j], qr_b[j], SC_MLA)
                    k = cx.ti % 2
                    cx.ti += 1
                    pt, ptb = cx.pst[k], cx.pst_b[k]
                    def ftr():
                        ins = None
                        for h in range(8):
                            ins = nc.tensor.transpose(pt[:64, h * 128: h * 128 + bn], qr[j][:bn, h * 64:(h + 1) * 64],
                                                      cx.ident[:bn, :bn])
                        return ins
                    m.op(m.PE, ftr, reads=[qr_b[j], cx.ident_buf], writes=[ptb])
                    qq, qqb = qrT[j], qrT_b[j]
                    m.op(m.ACT, lambda: nc.scalar.copy(out=qq[:, :, :bn],
                                                       in_=pt[:64, :].rearrange("p (h t) -> p h t", h=8)[:, :, :bn]),
                         reads=[ptb], writes=[qqb])
                    m.dma(m.SP, S["QAUX_M"][:, 0:64, r0 + t0: r0 + t0 + bn].rearrange("h r t -> r h t"), qq[:, :, :bn],
                          reads=[qqb], writes=[S["QAUX_M_b"]])
            m.dma(m.SP, S["KAUX_M"][0:64, r0:r0 + n], krT[gi % 2][:, :n], reads=[krT_b[gi % 2]], writes=[S["KAUX_M_b"]])
            fm_heads(m, cx, wukv, w_b, 4, c_t, c_b, n, 8, S["KM"][0:8], r0, S["KM_b"])
            if own:
                fm_heads(m, cx, wuq, w_b, 4, cqT[gi % 2], cqT_b[gi % 2], n, 8, S["QM"][0:8], r0, S["QM_b"], scale=SC_MLA)
        m.barrier()


def kv_pass_fox(m, xkv, w_in, fox_b, kvalid, kind, S):
    nc = m.nc
    with ExitStack() as es:
        cx = Ctx()
        FL = es.enter_context(nc.sbuf_tensor("FL", [8, LFULL], F32))
        FL_b = m.buf()
        esw = ExitStack()
        kv_common_setup(m, cx, esw, "a2")
        win_v = w_in.rearrange("(c p) f -> p c f", p=128)
        wq = esw.enter_context(nc.sbuf_tensor("wqf", [128, DC, 1024], BF16))
        wk = esw.enter_context(nc.sbuf_tensor("wkf", [128, DC, 1024], BF16))
        wv = esw.enter_context(nc.sbuf_tensor("wvf", [128, DC, 1024], BF16))
        wf = esw.enter_context(nc.sbuf_tensor("wff", [128, DC, 8], BF16))
        w_b = m.buf()
        load_w(m, m.POOL, wq, win_v[:, :, 1088:2112], bufs=[w_b])
        load_w(m, m.POOL, wk, win_v[:, :, 2112:3136], bufs=[w_b])
        load_w(m, m.POOL, wv, win_v[:, :, 3136:4160], bufs=[w_b])
        load_w(m, m.POOL, wf, win_v[:, :, 4160:4168], step=16, bufs=[w_b])
        xT = esw.enter_context(nc.sbuf_tensor("xTf", [128, DC, 512], BF16))
        xT_b = m.buf()
        bi_glob = 0
        for gi, (r0, n) in enumerate(KGROUPS):
            own = r0 < NT
            blocks = xgroup_fm(m, cx, xkv, r0, n, xT, xT_b)
            fm_heads(m, cx, wk, w_b, DC, xT, xT_b, n, 8, S["KM"][8:16], r0, S["KM_b"])
            if own:
                fm_heads(m, cx, wq, w_b, DC, xT, xT_b, n, 8, S["QM"][8:16], r0, S["QM_b"], scale=SC_FOX)
            ps, pb = next_pm(cx)
            def f():
                ins = None
                for kc in range(DC):
                    ins = nc.tensor.matmul(ps[:8, :n], lhsT=wf[:, kc, 0:8], rhs=xT[:, kc, :n], start=(kc == 0), stop=(kc == DC - 1))
                return ins
            m.op(m.PE, f, reads=[w_b, xT_b], writes=[pb])
            m.op(m.DVE, lambda: nc.vector.tensor_copy(out=FL[:, r0:r0 + n], in_=ps[:8, :n]), reads=[pb], writes=[FL_b])
            for (t0, bn) in blocks:
                bi_glob += 1
                ps2, pb2 = tm_out(m, cx, xT, xT_b, DC, t0, bn, wv, w_b, 0, 1024)
                ev, evb = next_ev(cx)
                evac(m, cx, bi_glob, ev[:bn, :], ps2[:bn, :], [pb2], [evb])
                m.dma(m.SP, S["VV"][r0 + t0: r0 + t0 + bn, 8:16, :], ev[:bn, :].rearrange("p (h d) -> p h d", h=8),
                      reads=[evb], writes=[S["VV_b"]])
        m.barrier()
        esw.close()
        fb = es.enter_context(nc.sbuf_tensor("fb", [8, 2], F32))
        T1 = es.enter_context(nc.sbuf_tensor("T1", [8, LFULL], F32))
        T2 = es.enter_context(nc.sbuf_tensor("T2", [8, LFULL], F32))
        KV = es.enter_context(nc.sbuf_tensor("KVd", [8, LFULL], BF16))
        KI = es.enter_context(nc.sbuf_tensor("KId", [8, LFULL], BF16))
        B1 = es.enter_context(nc.sbuf_tensor("B1", [8, LFULL], BF16))
        B2 = es.enter_context(nc.sbuf_tensor("B2", [8, LFULL], BF16))
        B3 = es.enter_context(nc.sbuf_tensor("B3", [8, LFULL], BF16))
        tb = m.buf()
        m.dma(m.SP, fb[:, 0:1], fox_b.rearrange("(h o) -> h o", o=1), writes=[tb])
        m.dma(m.POOL, KV[:, :], kvalid.partition_broadcast(8), writes=[tb])
        m.dma(m.POOL, KI[:, :], kind.partition_broadcast(8), writes=[tb])
        m.op(m.DVE, lambda: nc.vector.tensor_scalar(out=fb[:, 1:2], in0=fb[:, 0:1], scalar1=-1.0, scalar2=None, op0=ALU.mult),
             reads=[tb], writes=[tb])
        m.op(m.ACT, lambda: nc.scalar.activation(out=T1[:, :], in_=FL[:, :], func=AF.Exp, bias=fb[:, 1:2], scale=-1.0),
             reads=[FL_b, tb], writes=[tb])
        m.op(m.ACT, lambda: nc.scalar.activation(out=T1[:, :], in_=T1[:, :], func=AF.Ln, bias=1.0, scale=1.0),
             reads=[tb], writes=[tb])
        m.op(m.DVE, lambda: nc.vector.tensor_scalar(out=T1[:, :], in0=T1[:, :], scalar1=-1.0, scalar2=None, op0=ALU.mult),
             reads=[tb], writes=[tb])
        m.op(m.DVE, lambda: nc.vector.tensor_scalar(out=T2[:, NT:], in0=KV[:, NT:], scalar1=1e-30, scalar2=1.0, op0=ALU.mult,
                                                    op1=ALU.add), reads=[tb], writes=[tb])
        m.op(m.DVE, lambda: nc.vector.tensor_tensor(out=T2[:, NT:], in0=T2[:, NT:], in1=T1[:, NT:], op=ALU.mult),
             reads=[tb], writes=[tb])
        m.op(m.DVE, lambda: nc.vector.reduce_sum(out=fb[:, 0:1], in_=T2[:, NT:], axis=AX.X), reads=[tb], writes=[tb])
        m.op(m.DVE, lambda: nc.vector.memset(T2[:, :], 1.0), reads=[tb], writes=[tb])
        m.op(m.DVE, lambda: nc.vector.tensor_tensor_scan(out=FL[:, 0:NT], data0=T2[:, 0:NT], data1=T1[:, 0:NT],
                                                         initial=fb[:, 0:1], op0=ALU.mult, op1=ALU.add),
             reads=[tb], writes=[FL_b])
        m.op(m.DVE, lambda: nc.vector.tensor_tensor_scan(out=FL[:, NT:], data0=T2[:, NT:], data1=T1[:, NT:],
                                                         initial=0.0, op0=ALU.mult, op1=ALU.add),
             reads=[tb], writes=[FL_b])
        m.op(m.DVE, lambda: nc.vector.tensor_copy(out=B1[:, :], in_=FL[:, :]), reads=[FL_b], writes=[tb])
        m.op(m.DVE, lambda: nc.vector.tensor_tensor(out=T1[:, :], in0=FL[:, :], in1=B1[:, :], op=ALU.subtract),
             reads=[tb], writes=[tb])
        m.op(m.DVE, lambda: nc.vector.tensor_copy(out=B2[:, :], in_=T1[:, :]), reads=[tb], writes=[tb])
        m.dma(m.SP, S["QAUX_F"][:, 0, :], B1[:, 0:NT], reads=[tb], writes=[S["QAUX_F_b"]])
        m.dma(m.SP, S["QAUX_F"][:, 1, :], B2[:, 0:NT], reads=[tb], writes=[S["QAUX_F_b"]])
        m.op(m.DVE, lambda: nc.vector.memset(B3[:, :], 1.0), reads=[tb], writes=[tb])
        for r in (2, 3, 4):
            m.dma(m.SP, S["QAUX_F"][:, r, :], B3[:, 0:NT], reads=[tb], writes=[S["QAUX_F_b"]])
        m.op(m.DVE, lambda: nc.vector.tensor_scalar(out=B3[:, :], in0=KI[:, :], scalar1=-1.0, scalar2=None, op0=ALU.add),
             reads=[tb], writes=[tb])
        for r in (0, 1):
            m.dma(m.SP, S["KAUX_F"][:, r, :], B3[:, :], reads=[tb], writes=[S["KAUX_F_b"]])
        m.op(m.DVE, lambda: nc.vector.scalar_tensor_tensor(out=T1[:, :], in0=B1[:, :], scalar=-1.0, in1=KI[:, :], op0=ALU.mult,
                                                           op1=ALU.mult), reads=[tb], writes=[tb])
        m.op(m.DVE, lambda: nc.vector.tensor_copy(out=B1[:, :], in_=T1[:, :]), reads=[tb], writes=[tb])
        m.dma(m.SP, S["KAUX_F"][:, 2, :], B1[:, :], reads=[tb], writes=[S["KAUX_F_b"]])
        m.op(m.DVE, lambda: nc.vector.scalar_tensor_tensor(out=T1[:, :], in0=B2[:, :], scalar=-1.0, in1=KI[:, :], op0=ALU.mult,
                                                           op1=ALU.mult), reads=[tb], writes=[tb])
        m.op(m.DVE, lambda: nc.vector.tensor_copy(out=B2[:, :], in_=T1[:, :]), reads=[tb], writes=[tb])
        m.dma(m.SP, S["KAUX_F"][:, 3, :], B2[:, :], reads=[tb], writes=[S["KAUX_F_b"]])
        m.dma(m.SP, S["KAUX_F"][:, 4, :], KV[:, :], reads=[tb], writes=[S["KAUX_F_b"]])
        m.barrier()


def attention_phase(m, S):
    nc = m.nc
    NKB = 65
    with ExitStack() as es:
        ident = es.enter_context(nc.sbuf_tensor("identat", [128, 128], BF16))
        ident_buf, _ = make_identity(m, ident)
        km = es.enter_context(nc.sbuf_tensor("km", [128, LFULL], BF16))
        ka = [es.enter_context(nc.sbuf_tensor("ka%d" % i, [65, LFULL], BF16)) for i in range(2)]
        vv = es.enter_context(nc.sbuf_tensor("vv", [128, NKB, 128], BF16))
        qm = [es.enter_context(nc.sbuf_tensor("qm%d" % i, [128, NT], BF16)) for i in range(2)]
        qa = [es.enter_context(nc.sbuf_tensor("qa%d" % i, [65, NT], BF16)) for i in range(2)]
        km_b = m.buf()
        ka_b = [m.buf() for _ in range(2)]
        vv_b = m.buf()
        qm_b = [m.buf() for _ in range(2)]
        qa_b = [m.buf() for _ in range(2)]
        Sall = [es.enter_context(nc.sbuf_tensor("Sall%d" % i, [128, LFULL], F32)) for i in range(2)]
        Sall_b = [m.buf() for _ in range(2)]
        Pt = [es.enter_context(nc.sbuf_tensor("Pt%d" % i, [128, LFULL], BF16)) for i in range(2)]
        Pt_b = [m.buf() for _ in range(2)]
        PT = [es.enter_context(nc.sbuf_tensor("PTs%d" % i, [128, 8, 128], BF16)) for i in range(2)]
        PT_b = [m.buf() for _ in range(2)]
        mask = es.enter_context(nc.sbuf_tensor("maskT", [128, 2 * NT], F32))
        mask_b = m.buf()
        def fmask():
            nc.gpsimd.memset(mask[:, :], 0.0)
            return nc.gpsimd.affine_select(out=mask[:, :], in_=mask[:, :], pattern=[[-1, 2 * NT]], compare_op=ALU.is_ge,
                                           fill=NEG, base=NT, channel_multiplier=1)
        m.op(m.POOL, fmask, writes=[mask_b])
        stt = [es.enter_context(nc.sbuf_tensor("stt%d" % i, [128, 16], F32)) for i in range(2)]
        stt_b = [m.buf() for _ in range(2)]
        osb = [es.enter_context(nc.sbuf_tensor("osb%d" % i, [128, 128], BF16)) for i in range(2)]
        osb_b = [m.buf() for _ in range(2)]
        ps_s = [es.enter_context(nc.psum_tensor("ps_s%d" % i, [128, 512], F32)) for i in range(4)]
        ps_s_b = [m.buf() for _ in range(4)]
        ps_t_f = [es.enter_context(nc.psum_tensor("ps_t%d" % i, [128, 512], F32)) for i in range(2)]
        ps_t = [t[:, :].bitcast(BF16) for t in ps_t_f]
        ps_t_b = [m.buf() for _ in range(2)]
        ps_o = [es.enter_context(nc.psum_tensor("ps_o%d" % i, [128, 128], F32)) for i in range(2)]
        ps_o_b = [m.buf() for _ in range(2)]
        qblocks = tok_blocks(NT)
        items = [(h, qb) for h in range(16) for qb in range(len(qblocks))]
        st8 = {"ci": 0, "ti": 0, "mla_aux": False, "kh": -1, "vh": -1}

        def head_aux(h):
            s = h % 2
            mla = h < 8
            if mla:
                return ka[0], ka_b[0], 65, s
            return ka[s], ka_b[s], 5, s

        def load_head_k(h):
            s = h % 2
            mla = h < 8
            m.dma(m.SP, km[:, :], S["KM"][h], reads=[S["KM_b"]], writes=[km_b])
            m.dma(m.SP, qm[s][:, :], S["QM"][h], reads=[S["QM_b"]], writes=[qm_b[s]])
            if mla:
                if not st8["mla_aux"]:
                    m.dma(m.SP, ka[0][:, :], S["KAUX_M"][:, :], reads=[S["KAUX_M_b"]], writes=[ka_b[0]])
                    st8["mla_aux"] = True
                m.dma(m.SP, qa[s][:65, :], S["QAUX_M"][h], reads=[S["QAUX_M_b"]], writes=[qa_b[s]])
            else:
                m.dma(m.SP, ka[s][:5, :], S["KAUX_F"][h - 8], reads=[S["KAUX_F_b"]], writes=[ka_b[s]])
                m.dma(m.SP, qa[s][:5, :], S["QAUX_F"][h - 8], reads=[S["QAUX_F_b"]], writes=[qa_b[s]])

        def load_head_v(h):
            m.dma(m.SP, vv[:16, 0, :], S["VV"][0:16, h, :], reads=[S["VV_b"]], writes=[vv_b])
            for b0 in range(0, 64, 16):
                m.dma(m.SP, vv[:, 1 + b0: 1 + b0 + 16, :],
                      S["VV"][16 + b0 * 128: 16 + (b0 + 16) * 128, h, :].rearrange("(b p) d -> p b d", p=128),
                      reads=[S["VV_b"]], writes=[vv_b])

        def pass1(idx):
            h, qb = items[idx]
            q0, n = qblocks[qb]
            if st8["kh"] != h:
                load_head_k(h)
                st8["kh"] = h
            ka_t, ka_tb, ra, s = head_aux(h)
            sa, sab = Sall[idx % 2], Sall_b[idx % 2]
            nk_own = q0 + n
            chunks = []
            c0 = 0
            while c0 < nk_own:
                ln = min(512, nk_own - c0)
                chunks.append((c0, ln, c0, True))
                c0 += ln
            for j in range(14):
                chunks.append((NT + 512 * j, 512, nk_own + 512 * j, False))
            for (k0, ln, d0, is_own) in chunks:
                p = st8["ci"] % 4
                st8["ci"] += 1
                def fqk():
                    nc.tensor.matmul(ps_s[p][:n, :ln], lhsT=qm[s][:, q0:q0 + n], rhs=km[:, k0:k0 + ln], start=True, stop=False)
                    return nc.tensor.matmul(ps_s[p][:n, :ln], lhsT=qa[s][:ra, q0:q0 + n], rhs=ka_t[:ra, k0:k0 + ln],
                                            start=False, stop=True)
                m.op(m.PE, fqk, reads=[qm_b[s], km_b, qa_b[s], ka_tb], writes=[ps_s_b[p]])
                if is_own:
                    mo = (NT - q0) + k0
                    m.op(m.DVE, lambda: nc.vector.tensor_tensor(out=sa[:n, d0:d0 + ln], in0=ps_s[p][:n, :ln],
                                                                in1=mask[:n, mo:mo + ln], op=ALU.add),
                         reads=[ps_s_b[p], mask_b], writes=[sab])
                elif st8["ci"] % 2 == 0:
                    m.op(m.DVE, lambda: nc.vector.tensor_copy(out=sa[:n, d0:d0 + ln], in_=ps_s[p][:n, :ln]),
                         reads=[ps_s_b[p]], writes=[sab])
                else:
                    m.op(m.ACT, lambda: nc.scalar.copy(out=sa[:n, d0:d0 + ln], in_=ps_s[p][:n, :ln]),
                         reads=[ps_s_b[p]], writes=[sab])

        def pass2(idx):
            h, qb = items[idx]
            q0, n = qblocks[qb]
            nk_own = q0 + n
            slen = nk_own + NOTH
            sti = idx % 2
            sa, sab = Sall[sti], Sall_b[sti]
            st, stb = stt[sti], stt_b[sti]
            pt_, ptb_ = Pt[sti], Pt_b[sti]
            m.op(m.DVE, lambda: nc.vector.reduce_max(out=st[:n, 0:1], in_=sa[:n, 0:slen], axis=AX.X),
                 reads=[sab], writes=[stb])
            m.op(m.DVE, lambda: nc.vector.tensor_scalar(out=st[:n, 1:2], in0=st[:n, 0:1], scalar1=-1.0, scalar2=None,
                                                        op0=ALU.mult), reads=[stb], writes=[stb])
            npieces = 4
            pl = -(-slen // npieces)
            for pi in range(npieces):
                a_ = pi * pl
                b_ = min(slen, a_ + pl)
                m.op(m.ACT, lambda: nc.scalar.activation(out=pt_[:n, a_:b_], in_=sa[:n, a_:b_], func=AF.Exp,
                                                         bias=st[:n, 1:2], scale=1.0, accum_out=st[:n, 4 + pi:5 + pi]),
                     reads=[sab, stb], writes=[ptb_, stb])
            m.op(m.DVE, lambda: nc.vector.reduce_sum(out=st[:n, 2:3], in_=st[:n, 4:4 + npieces], axis=AX.X),
                 reads=[stb], writes=[stb])
            m.op(m.DVE, lambda: nc.vector.reciprocal(out=st[:n, 3:4], in_=st[:n, 2:3]), reads=[stb], writes=[stb])
            if st8["vh"] != h:
                load_head_v(h)
                st8["vh"] = h
            kblocks = [(0, 16, 0)] + [(16 + 128 * (j - 1), 128, j) for j in range(1, qb + 1)]
            for j in range(56):
                kblocks.append((nk_own + 128 * j, 128, 9 + j))
            po, pob = ps_o[sti], ps_o_b[sti]
            nkb = len(kblocks)
            first_pv = True
            for g0 in range(0, nkb, 8):
                grp = kblocks[g0:g0 + 8]
                tp = st8["ti"] % 2
                st8["ti"] += 1
                def ftr():
                    ins = None
                    for jj, (pc, kl, vb) in enumerate(grp):
                        ins = nc.tensor.transpose(ps_t[tp][:kl, jj * 128: jj * 128 + n], pt_[:n, pc:pc + kl], ident[:n, :n])
                    return ins
                m.op(m.PE, ftr, reads=[ptb_, ident_buf], writes=[ps_t_b[tp]])
                kl0 = grp[0][1]
                ng = len(grp)
                if kl0 == 16:
                    m.op(m.DVE, lambda: nc.vector.tensor_copy(out=PT[tp][:16, 0, :n], in_=ps_t[tp][:16, 0:n]),
                         reads=[ps_t_b[tp]], writes=[PT_b[tp]])
                    if ng > 1:
                        m.op(m.DVE, lambda: nc.vector.tensor_copy(
                            out=PT[tp][:, 1:ng, :n],
                            in_=ps_t[tp][:, 128:ng * 128].rearrange("p (j t) -> p j t", j=ng - 1)[:, :, :n]),
                            reads=[ps_t_b[tp]], writes=[PT_b[tp]])
                elif tp == 0:
                    m.op(m.DVE, lambda: nc.vector.tensor_copy(
                        out=PT[tp][:, 0:ng, :n], in_=ps_t[tp][:, 0:ng * 128].rearrange("p (j t) -> p j t", j=ng)[:, :, :n]),
                        reads=[ps_t_b[tp]], writes=[PT_b[tp]])
                else:
                    m.op(m.ACT, lambda: nc.scalar.copy(
                        out=PT[tp][:, 0:ng, :n], in_=ps_t[tp][:, 0:ng * 128].rearrange("p (j t) -> p j t", j=ng)[:, :, :n]),
                        reads=[ps_t_b[tp]], writes=[PT_b[tp]])
                last_grp = (g0 + 8 >= nkb)
                def fpv():
                    ins = None
                    for jj, (pc, kl, vb) in enumerate(grp):
                        ins = nc.tensor.matmul(po[:n, :], lhsT=PT[tp][:kl, jj, :n], rhs=vv[:kl, vb, :],
                                               start=(first_pv and jj == 0), stop=(last_grp and jj == ng - 1))
                    return ins
                m.op(m.PE, fpv, reads=[PT_b[tp], vv_b], writes=[pob])
                first_pv = False
            ob, obb = osb[sti], osb_b[sti]
            m.op(m.ACT, lambda: nc.scalar.mul(out=ob[:n, :], in_=po[:n, :], mul=st[:n, 3:4]), reads=[pob, stb], writes=[obb])
            m.dma(m.SP, S["AO"][q0:q0 + n, h * 128:(h + 1) * 128], ob[:n, :], reads=[obb], writes=[S["AO_b"]])

        pass1(0)
        for idx in range(len(items)):
            if idx + 1 < len(items):
                pass1(idx + 1)
            pass2(idx)
        m.barrier()


def outproj_phase(m, tag, A_dram, A_buf, a_is_bf16, ntok, arow0, w_out, Hin, Hin_buf, hrow0, g_dram, b_dram, Hout, Hout_buf, orow0,
                  pre=None):
    nc = m.nc
    with ExitStack() as es:
        cx = Ctx()
        blocks = tok_blocks(ntok)
        aT = es.enter_context(nc.sbuf_tensor("aT" + tag, [128, DC, ntok], BF16))
        aT_b = m.buf()
        ident = es.enter_context(nc.sbuf_tensor("ident" + tag, [128, 128], BF16))
        ident_buf, _ = make_identity(m, ident)
        wo = es.enter_context(nc.sbuf_tensor("wo" + tag, [128, DC, D], BF16))
        wo_b = m.buf()
        load_w(m, m.POOL, wo, w_out.rearrange("(c p) f -> p c f", p=128), step=2, bufs=[wo_b])
        py = [es.enter_context(nc.psum_tensor("py%s%d" % (tag, i), [128, 2048], F32)) for i in range(1)]
        py_b = [m.buf() for _ in range(1)]
        pt_f = [es.enter_context(nc.psum_tensor("pt%s%d" % (tag, i), [128, 512], F32)) for i in range(2)]
        pt_b = [m.buf() for _ in range(2)]
        with ExitStack() as es2:
            stage = [es2.enter_context(nc.sbuf_tensor("stg%s%d" % (tag, i), [128, D], BF16)) for i in range(2)]
            stage_b = [m.buf() for _ in range(2)]
            pst = [t[:, :].bitcast(BF16) for t in pt_f]
            load_fm(m, A_dram, arow0, ntok, aT, aT_b, ident, ident_buf, stage, stage_b, pst, pt_b, Hbuf=A_buf,
                    q=(m.SP if a_is_bf16 else m.POOL))
            m.barrier()
        ln_setup(m, cx, es, tag, g_dram, b_dram, [Hin_buf], Hout_buf)
        for bi, (t0, n) in enumerate(blocks):
            def f():
                ins = None
                for q in range(4):
                    for c in range(DC):
                        ins = nc.tensor.matmul(py[0][:n, q * 512:(q + 1) * 512], lhsT=aT[:, c, t0:t0 + n],
                                               rhs=wo[:, c, q * 512:(q + 1) * 512], start=(c == 0), stop=(c == DC - 1))
                return ins
            m.op(m.PE, f, reads=[aT_b, wo_b], writes=[py_b[0]])
            layer_norm_block(m, cx, py[0][:n, :], [py_b[0]], Hin, hrow0 + t0, n, Hout, cx.gam, cx.bet, out_row=orow0 + t0)
        m.barrier()


def make_scratch(m):
    nc = m.nc
    S = {}
    def mk(name, shape, dt):
        S[name] = nc.dram_tensor("scr_" + name, shape, dt).ap()
        S[name + "_b"] = m.buf()
    mk("KM", [16, 128, LFULL], BF16)
    mk("KAUX_M", [65, LFULL], BF16)
    mk("KAUX_F", [8, 5, LFULL], BF16)
    mk("VV", [LFULL, 16, 128], BF16)
    mk("QM", [16, 128, NT], BF16)
    mk("QAUX_M", [8, 65, NT], BF16)
    mk("QAUX_F", [8, 5, NT], BF16)
    mk("AO", [NT, D], BF16)
    mk("H1", [NT, D], F32)
    mk("H2", [NT, D], F32)
    mk("H3", [NOWN, D], F32)
    mk("PM", [NOWN, D], BF16)
    return S


def persist(m, S):
    for k, v in S.items():
        if k.endswith("_b"):
            m.bufs.append(v)


def layer0_mixer(m, I, S):
    kv_pass_mla(m, I["xkv"], I["cs_all"], I["attn_w_in"], I["w_ukv_p"], I["w_uq_p"], I["mla_kv_norm"], I["mla_q_norm"],
                I["kvalid"], S)
    persist(m, S)
    kv_pass_fox(m, I["xkv"], I["attn_w_in"], I["fox_forget_bias"], I["kvalid"], I["kind"], S)
    persist(m, S)
    attention_phase(m, S)
    persist(m, S)
    xb = m.buf()
    outproj_phase(m, "op0", S["AO"], S["AO_b"], True, NT, 0, I["attn_w_out"], I["xkv"], xb, 0, I["ln_mix_g0"], I["ln_mix_b0"],
                  S["H1"], S["H1_b"], 0)
    persist(m, S)


def declare_inputs(nc, names_shapes):
    I = {}
    for name, shape in names_shapes:
        I[name] = nc.dram_tensor(name, list(shape), F32, kind="ExternalInput").ap()
    return I


L0_INPUTS = [("xkv", (LFULL, D)), ("cs_all", (LFULL, 64)), ("attn_w_in", (D, 4168)), ("w_ukv_p", (512, 2048)),
             ("w_uq_p", (512, 1536)), ("mla_kv_norm", (512,)), ("mla_q_norm", (512,)), ("kvalid", (LFULL,)),
             ("kind", (LFULL,)), ("fox_forget_bias", (8,)), ("attn_w_out", (D, D)), ("ln_mix_g0", (D,)), ("ln_mix_b0", (D,))]


def host_prep_l0(inp):
    x = np.asarray(inp["x"], np.float32)[0]
    hfull = np.concatenate([np.asarray(inp["meta_tokens"], np.float32), x], axis=0)
    pos = np.arange(LFULL, dtype=np.float32)
    inv_freq = (10000.0 ** (-np.arange(0, 64, 2, dtype=np.float32) / 64)).astype(np.float32)
    ang = pos[:, None] * inv_freq[None, :]
    cs = np.concatenate([np.cos(ang), np.sin(ang)], axis=1).astype(np.float32)
    w_ukv = np.asarray(inp["mla_w_ukv"], np.float32)[0].reshape(512, 8, 256)
    w_ukv_p = np.ascontiguousarray(np.concatenate([w_ukv[:, :, :128].reshape(512, 1024), w_ukv[:, :, 128:].reshape(512, 1024)], 1))
    w_uq = np.asarray(inp["mla_w_uq"], np.float32)[0].reshape(512, 8, 192)
    w_uq_p = np.ascontiguousarray(np.concatenate([w_uq[:, :, :128].reshape(512, 1024), w_uq[:, :, 128:].reshape(512, 512)], 1))
    common = {
        "attn_w_in": np.ascontiguousarray(np.asarray(inp["attn_w_in"], np.float32)[0]),
        "w_ukv_p": w_ukv_p, "w_uq_p": w_uq_p,
        "mla_kv_norm": np.asarray(inp["mla_kv_norm"], np.float32)[0], "mla_q_norm": np.asarray(inp["mla_q_norm"], np.float32)[0],
        "fox_forget_bias": np.asarray(inp["fox_forget_bias"], np.float32)[0],
        "attn_w_out": np.ascontiguousarray(np.asarray(inp["attn_w_out"], np.float32)[0]),
        "ln_mix_g0": np.asarray(inp["ln_mix_g"], np.float32)[0], "ln_mix_b0": np.asarray(inp["ln_mix_b"], np.float32)[0],
    }
    maps = []
    for c in range(NCORE):
        own = np.arange(1024 * c, 1024 * c + NT)
        oth = np.concatenate([np.arange(0, 1024 * c), np.arange(1024 * c + NT, LFULL)])
        perm = np.concatenate([own, oth])
        d = dict(common)
        d["xkv"] = np.ascontiguousarray(hfull[perm])
        d["cs_all"] = np.ascontiguousarray(cs[perm])
        kvalid = np.zeros(LFULL, np.float32)
        kvalid[NT:] = np.where(oth < 1024 * c, 0.0, NEG)
        d["kvalid"] = kvalid
        d["kind"] = (perm >= 16).astype(np.float32)
        maps.append(d)
    return maps


def pool_phase(m, I, S):
    nc = m.nc
    H2, H2_b = S["H2"], S["H2_b"]
    groups_all = tok_groups(NT)
    with ExitStack() as es:
        dT = es.enter_context(nc.sbuf_tensor("dTp", [128, DC, NOWN], BF16))
        dT_b = m.buf()
        with ExitStack() as es1:
            hT = es1.enter_context(nc.sbuf_tensor("hTp", [128, DC, NT], BF16))
            hT_b = m.buf()
            ident = es1.enter_context(nc.sbuf_tensor("identp", [128, 128], BF16))
            ident_buf, _ = make_identity(m, ident)
            wpi = es1.enter_context(nc.sbuf_tensor("wpi", [128, DC, D], BF16))
            wpi_b = m.buf()
            load_w(m, m.POOL, wpi, I["pool_w_in"].rearrange("(c p) f -> p c f", p=128), step=2, bufs=[wpi_b])
            pT = es1.enter_context(nc.sbuf_tensor("pTp", [128, 4, NT], F32))
            A = es1.enter_context(nc.sbuf_tensor("Ap", [128, 4, NT], F32))
            B = es1.enter_context(nc.sbuf_tensor("Bp", [128, 4, NT], F32))
            pT_b, A_b, B_b = m.buf(), m.buf(), m.buf()
            pp = [es1.enter_context(nc.psum_tensor("ppp%d" % i, [128, 512], F32)) for i in range(4)]
            pp_b = [m.buf() for _ in range(4)]
            with ExitStack() as es2:
                stage = [es2.enter_context(nc.sbuf_tensor("stgp%d" % i, [128, D], BF16)) for i in range(2)]
                stage_b = [m.buf() for _ in range(2)]
                pst = [pp[i][:, :].bitcast(BF16) for i in range(2)]
                load_fm(m, H2, 0, NT, hT, hT_b, ident, ident_buf, stage, stage_b, pst, pp_b[0:2], Hbuf=H2_b)
                m.barrier()
            k = 0
            for g, w in enumerate((2, 4, 8, 16)):
                for oc in range(4):
                    ch = 4 * g + oc
                    for (t0, n) in groups_all:
                        p = k % 4
                        k += 1
                        def f():
                            ins = None
                            for c in range(DC):
                                ins = nc.tensor.matmul(pp[p][:, :n], lhsT=wpi[:, c, ch * 128:(ch + 1) * 128], rhs=hT[:, c, t0:t0 + n],
                                                       start=(c == 0), stop=(c == DC - 1))
                            return ins
                        m.op(m.PE, f, reads=[wpi_b, hT_b], writes=[pp_b[p]])
                        evac(m, None, k, pT[:, oc, t0:t0 + n], pp[p][:, :n], [pp_b[p]], [pT_b])
                cur, cur_b = pT, pT_b
                dst = [(A, A_b), (B, B_b)]
                sh = 1
                lvl = 0
                while sh < w:
                    o, o_b = dst[lvl % 2]
                    lo = 2 * sh - 1
                    src, src_b = cur, cur_b
                    m.op(m.DVE, lambda: nc.vector.tensor_tensor(out=o[:, :, lo:NT], in0=src[:, :, lo:NT], in1=src[:, :, lo - sh:NT - sh],
                                                                op=ALU.add), reads=[src_b], writes=[o_b])
                    cur, cur_b = o, o_b
                    sh *= 2
                    lvl += 1
                sw, sw_b = cur, cur_b
                m.op(m.DVE, lambda: nc.vector.scalar_tensor_tensor(out=dT[:, 4 * g:4 * g + 4, :], in0=sw[:, :, NHALO:NT], scalar=1.0 / w,
                                                                   in1=pT[:, :, NHALO:NT], op0=ALU.mult, op1=ALU.subtract),
                     reads=[sw_b, pT_b], writes=[dT_b])
            m.barrier()
        wgr = es.enter_context(nc.sbuf_tensor("wgr", [128, 16, 512], BF16))
        wgr_b = m.buf()
        load_w(m, m.POOL, wgr, I["pool_w_group"].rearrange("g (c p) f -> p (g c) f", p=128), bufs=[wgr_b])
        scl = es.enter_context(nc.sbuf_tensor("sclp", [128, D], F32))
        scl_b = m.buf()
        m.dma(m.SP, scl[:, :], I["pool_scale"].partition_broadcast(128), writes=[scl_b])
        ysb = [es.enter_context(nc.sbuf_tensor("ysbp%d" % i, [128, D], BF16)) for i in range(2)]
        ysb_b = [m.buf() for _ in range(2)]
        pq = [es.enter_context(nc.psum_tensor("pqp%d" % i, [128, 512], F32)) for i in range(4)]
        pq_b = [m.buf() for _ in range(4)]
        k = 0
        for bi, (t0, n) in enumerate(tok_blocks(NOWN)):
            y, y_b = ysb[bi % 2], ysb_b[bi % 2]
            for g in range(4):
                p = k % 4
                k += 1
                def f():
                    ins = None
                    for kc in range(4):
                        ins = nc.tensor.matmul(pq[p][:n, :], lhsT=dT[:, 4 * g + kc, t0:t0 + n], rhs=wgr[:, 4 * g + kc, :],
                                               start=(kc == 0), stop=(kc == 3))
                    return ins
                m.op(m.PE, f, reads=[dT_b, wgr_b], writes=[pq_b[p]])
                m.op(m.DVE, lambda: nc.vector.tensor_tensor(out=y[:n, g * 512:(g + 1) * 512], in0=pq[p][:n, :],
                                                            in1=scl[:n, g * 512:(g + 1) * 512], op=ALU.mult),
                     reads=[pq_b[p], scl_b], writes=[y_b])
            m.dma(m.SP, S["PM"][t0:t0 + n, :], y[:n, :], reads=[y_b], writes=[S["PM_b"]])
        m.barrier()
    persist(m, S)
    outproj_phase(m, "op1", S["PM"], S["PM_b"], True, NOWN, 0, I["pool_w_out"], S["H2"], S["H2_b"], NHALO,
                  I["ln_mix_g1"], I["ln_mix_b1"], S["H3"], S["H3_b"], 0)
    persist(m, S)


def make_moe_gates(m, I, S):
    def gates(cx, es, hT_unused, hT_buf_unused, blocks):
        nc = m.nc
        nb = len(blocks)
        gate_tile = cx.gate_tile
        cx.gate_buf = m.buf()
        with ExitStack() as e3:
            identf = e3.enter_context(nc.sbuf_tensor("identf", [128, 128], F32))
            idb = m.buf()
            def fi():
                nc.gpsimd.memset(identf[:], 1.0)
                return nc.gpsimd.affine_select(out=identf[:], in_=identf[:], pattern=[[-1, 128]], compare_op=ALU.is_equal,
                                               fill=0.0, base=0, channel_multiplier=1)
            m.op(m.POOL, fi, writes=[idb])
            wr = e3.enter_context(nc.sbuf_tensor("wrt", [128, DC, NEXP], F32))
            br = e3.enter_context(nc.sbuf_tensor("brt", [128, NEXP], F32))
            wr_b = m.buf()
            m.dma(m.SP, wr[:, :, :], I["moe_w_router"].rearrange("(c p) e -> p c e", p=128), writes=[wr_b])
            m.dma(m.SP, br[:, :], I["moe_b_router"].partition_broadcast(128), writes=[wr_b])
            hr = [e3.enter_context(nc.sbuf_tensor("hrt%d" % i, [128, D], F32)) for i in range(2)]
            hr_b = [m.buf() for _ in range(2)]
            hTf = e3.enter_context(nc.sbuf_tensor("hTft", [128, DC, 128], F32))
            hTf_b = m.buf()
            gsm = e3.enter_context(nc.sbuf_tensor("gsm", [128, 64], F32))
            gsm_b = m.buf()
            ptf, ptf_b = cx.pg, cx.pg_b
            plg, plg_b = cx.pu[0], cx.pu_b[0]
            k = 0
            for bi, (t0, n) in enumerate(blocks):
                h, hb = hr[bi % 2], hr_b[bi % 2]
                m.dma(m.SP, h[:n, :], S["H3"][t0:t0 + n, :], reads=[S["H3_b"]], writes=[hb])
                for q in range(4):
                    p = k % 2
                    k += 1
                    def f():
                        ins = None
                        for j in range(4):
                            c = q * 4 + j
                            ins = nc.tensor.transpose(ptf[p][:, j * 128: j * 128 + n], h[:n, c * 128:(c + 1) * 128], identf[:n, :n])
                        return ins
                    m.op(m.PE, f, reads=[hb, idb], writes=[ptf_b[p]])
                    m.op(m.DVE, lambda: nc.vector.tensor_copy(out=hTf[:, q * 4:(q + 1) * 4, :n],
                                                              in_=ptf[p][:, :].rearrange("p (j t) -> p j t", j=4)[:, :, :n]),
                         reads=[ptf_b[p]], writes=[hTf_b])
                def fl():
                    ins = None
                    for c in range(DC):
                        ins = nc.tensor.matmul(plg[:n, 0:NEXP], lhsT=hTf[:, c, :n], rhs=wr[:, c, :], start=(c == 0), stop=(c == DC - 1))
                    return ins
                m.op(m.PE, fl, reads=[hTf_b, wr_b], writes=[plg_b])
                lg, eq1, lg2, eq2 = gsm[:n, 0:8], gsm[:n, 8:16], gsm[:n, 16:24], gsm[:n, 24:32]
                m1, m2, dd, g1, g2 = gsm[:n, 32:33], gsm[:n, 33:34], gsm[:n, 34:35], gsm[:n, 35:36], gsm[:n, 36:37]
                G = gate_tile[:n, bi, :]
                m.op(m.DVE, lambda: nc.vector.tensor_tensor(out=lg, in0=plg[:n, 0:NEXP], in1=br[:n, :], op=ALU.add),
                     reads=[plg_b, wr_b], writes=[gsm_b])
                m.op(m.DVE, lambda: nc.vector.reduce_max(out=m1, in_=lg, axis=AX.X), reads=[gsm_b], writes=[gsm_b])
                m.op(m.DVE, lambda: nc.vector.tensor_scalar(out=eq1, in0=lg, scalar1=m1, scalar2=None, op0=ALU.is_equal),
                     reads=[gsm_b], writes=[gsm_b])
                m.op(m.DVE, lambda: nc.vector.scalar_tensor_tensor(out=lg2, in0=eq1, scalar=NEG, in1=lg, op0=ALU.mult, op1=ALU.add),
                     reads=[gsm_b], writes=[gsm_b])
                m.op(m.DVE, lambda: nc.vector.reduce_max(out=m2, in_=lg2, axis=AX.X), reads=[gsm_b], writes=[gsm_b])
                m.op(m.DVE, lambda: nc.vector.tensor_scalar(out=eq2, in0=lg2, scalar1=m2, scalar2=None, op0=ALU.is_equal),
                     reads=[gsm_b], writes=[gsm_b])
                if getattr(cx, "sel_tile", None) is not None:
                    SEL = cx.sel_tile[:n, bi, :]
                    m.op(m.DVE, lambda: nc.vector.tensor_tensor(out=SEL, in0=eq1, in1=eq2, op=ALU.add), reads=[gsm_b],
                         writes=[cx.gate_buf])
                m.op(m.DVE, lambda: nc.vector.tensor_tensor(out=dd, in0=m1, in1=m2, op=ALU.subtract), reads=[gsm_b], writes=[gsm_b])
                m.op(m.ACT, lambda: nc.scalar.activation(out=g1, in_=dd, func=AF.Sigmoid), reads=[gsm_b], writes=[gsm_b])
                m.op(m.DVE, lambda: nc.vector.tensor_scalar(out=g2, in0=g1, scalar1=-1.0, scalar2=1.0, op0=ALU.mult, op1=ALU.add),
                     reads=[gsm_b], writes=[gsm_b])
                m.op(m.DVE, lambda: nc.vector.tensor_scalar(out=G, in0=eq1, scalar1=g1, scalar2=None, op0=ALU.mult),
                     reads=[gsm_b], writes=[cx.gate_buf])
                m.op(m.DVE, lambda: nc.vector.scalar_tensor_tensor(out=G, in0=eq2, scalar=g2, in1=G, op0=ALU.mult, op1=ALU.add),
                     reads=[gsm_b], writes=[cx.gate_buf])
            m.barrier()
        m.bufs.append(cx.gate_buf)
        return gate_tile
    return gates


I32 = mybir.dt.int32
USE_COMPACT_MOE = False
MOE_CAP = 384


def moe_phase(m, I, S, out, out_b):
    nc = m.nc
    C0 = MOE_CAP
    NJB = C0 // 128
    FC = 256
    F = FFN_EXPERT
    nfc = F // FC
    NB = NOWN // 128
    tag = "mo"
    H3, H3_b = S["H3"], S["H3_b"]
    with ExitStack() as es:
        cx = Ctx()
        blocks = tok_blocks(NOWN)
        ident = es.enter_context(nc.sbuf_tensor("identmo", [128, 128], BF16))
        ident_buf, _ = make_identity(m, ident)
        yacc = es.enter_context(nc.sbuf_tensor("yaccmo", [128, NB, D], F32))
        yacc_bufs = [m.buf() for _ in range(NB)]
        gate_tile = es.enter_context(nc.sbuf_tensor("gatemo", [128, NB, NEXP], F32))
        sel_tile = es.enter_context(nc.sbuf_tensor("selmo", [128, NB, NEXP], F32))
        pos_tile = es.enter_context(nc.sbuf_tensor("posmo", [128, NB, NEXP], F32))
        selb = es.enter_context(nc.sbuf_tensor("selbmo", [128, NB, NEXP], BF16))
        iota_i = es.enter_context(nc.sbuf_tensor("iotai", [128, C0], I32))
        iota_f = es.enter_context(nc.sbuf_tensor("iotaf", [128, C0], F32))
        ones_m = es.enter_context(nc.sbuf_tensor("onesmo", [128, 128], BF16))
        ustr = es.enter_context(nc.sbuf_tensor("ustrmo", [128, 128], BF16))
        cst_b = m.buf()
        m.op(m.POOL, lambda: nc.gpsimd.iota(iota_i[:, :], pattern=[[1, C0]], base=0, channel_multiplier=0), writes=[cst_b])
        m.op(m.DVE, lambda: nc.vector.tensor_copy(out=iota_f[:, :], in_=iota_i[:, :]), reads=[cst_b], writes=[cst_b])
        def fu():
            nc.gpsimd.memset(ones_m[:, :], 1.0)
            nc.gpsimd.memset(ustr[:, :], 1.0)
            return nc.gpsimd.affine_select(out=ustr[:, :], in_=ustr[:, :], pattern=[[1, 128]], compare_op=ALU.is_gt,
                                           fill=0.0, base=0, channel_multiplier=-1)
        m.op(m.POOL, fu, writes=[cst_b])
        pg = [es.enter_context(nc.psum_tensor("pgmo%d" % i, [128, 512], F32)) for i in range(2)]
        pu = [es.enter_context(nc.psum_tensor("pumo%d" % i, [128, 512], F32)) for i in range(2)]
        py = [es.enter_context(nc.psum_tensor("pymo%d" % i, [128, 1024], F32)) for i in range(2)]
        pg_b = [m.buf() for _ in range(2)]
        pu_b = [m.buf() for _ in range(2)]
        py_b = [m.buf() for _ in range(2)]
        cx.gate_tile, cx.sel_tile = gate_tile, sel_tile
        cx.pg, cx.pg_b, cx.pu, cx.pu_b = pg, pg_b, pu, pu_b
        make_moe_gates(m, I, S)(cx, es, None, None, blocks)
        gb = cx.gate_buf
        m.op(m.DVE, lambda: nc.vector.tensor_copy(out=selb[:, :, :], in_=sel_tile[:, :, :]), reads=[gb], writes=[gb])
        def fpos():
            ins = None
            for b in range(NB):
                for b2 in range(b):
                    nc.tensor.matmul(pu[0][:, b * 8:(b + 1) * 8], lhsT=ones_m[:, :], rhs=selb[:, b2, :], start=(b2 == 0), stop=False)
                ins = nc.tensor.matmul(pu[0][:, b * 8:(b + 1) * 8], lhsT=ustr[:, :], rhs=selb[:, b, :], start=(b == 0), stop=True)
            return ins
        m.op(m.PE, fpos, reads=[gb, cst_b], writes=[pu_b[0]])
        m.op(m.DVE, lambda: nc.vector.tensor_copy(out=pos_tile[:, :, :],
                                                  in_=pu[0][:, 0:NB * NEXP].rearrange("p (b e) -> p b e", b=NB)),
             reads=[pu_b[0]], writes=[gb])
        esw = ExitStack()
        Pe = esw.enter_context(nc.sbuf_tensor("Pemo", [128, NB, C0], BF16))
        PTe = esw.enter_context(nc.sbuf_tensor("PTemo", [128, NJB, NOWN], BF16))
        xgT = esw.enter_context(nc.sbuf_tensor("xgTmo", [128, DC, C0], BF16))
        yea = esw.enter_context(nc.sbuf_tensor("yeamo", [128, NJB, D], F32))
        yeb = esw.enter_context(nc.sbuf_tensor("yebmo", [128, NJB, D], BF16))
        xst = [esw.enter_context(nc.sbuf_tensor("xstmo%d" % i, [128, D], BF16)) for i in range(2)]
        wgt = [esw.enter_context(nc.sbuf_tensor("wgmo%d" % i, [128, DC, FC], BF16)) for i in range(2)]
        wut = [esw.enter_context(nc.sbuf_tensor("wumo%d" % i, [128, DC, FC], BF16)) for i in range(2)]
        wdt = [esw.enter_context(nc.sbuf_tensor("wdmo%d" % i, [128, FC // 128, D], BF16)) for i in range(2)]
        hid = [esw.enter_context(nc.sbuf_tensor("hidmo%d" % i, [128, FC // 128, C0], BF16)) for i in range(2)]
        gs = [esw.enter_context(nc.sbuf_tensor("gsmo%d" % i, [128, C0], F32)) for i in range(2)]
        Pe_b, PTe_b, xgT_b, yeb_b = m.buf(), m.buf(), m.buf(), m.buf()
        yea_b = [m.buf() for _ in range(NJB)]
        xst_b = [m.buf() for _ in range(2)]
        wg_b = [m.buf() for _ in range(2)]
        wu_b = [m.buf() for _ in range(2)]
        wd_b = [m.buf() for _ in range(2)]
        hid_b = [m.buf() for _ in range(2)]
        gs_b = [m.buf() for _ in range(2)]
        pgt = [pg[i][:, :].bitcast(BF16) for i in range(2)]
        acc = [(pg[0], pg_b[0], 0), (pg[1], pg_b[1], 0), (pu[0], pu_b[0], 0), (pu[1], pu_b[1], 0),
               (py[0], py_b[0], 0), (py[0], py_b[0], 512), (py[1], py_b[1], 0), (py[1], py_b[1], 512)]
        kk = 0
        gi = 0
        xi = 0
        ti = 0
        for e in range(NEXP):
            for b in range(NB):
                m.op(m.DVE, lambda: nc.vector.tensor_scalar(out=Pe[:, b, :], in0=iota_f[:, :], scalar1=pos_tile[:, b, e:e + 1],
                                                            scalar2=sel_tile[:, b, e:e + 1], op0=ALU.is_equal, op1=ALU.mult),
                     reads=[gb, cst_b], writes=[Pe_b])
            for b in range(NB):
                tp = ti % 2
                ti += 1
                def ftr():
                    ins = None
                    for jb in range(NJB):
                        ins = nc.tensor.transpose(pgt[tp][:, jb * 128:(jb + 1) * 128], Pe[:, b, jb * 128:(jb + 1) * 128], ident[:, :])
                    return ins
                m.op(m.PE, ftr, reads=[Pe_b, ident_buf], writes=[pg_b[tp]])
                m.op(m.ACT, lambda: nc.scalar.copy(out=PTe[:, :, b * 128:(b + 1) * 128],
                                                   in_=pgt[tp][:, 0:NJB * 128].rearrange("p (j t) -> p j t", j=NJB)),
                     reads=[pg_b[tp]], writes=[PTe_b])
            for half in range(2):
                for b in range(NB):
                    xs, xsb = xst[xi % 2], xst_b[xi % 2]
                    xi += 1
                    m.dma(m.POOL, xs[:, :], H3[b * 128:(b + 1) * 128, :], reads=[H3_b], writes=[xsb])
                    def fg():
                        ins = None
                        for c8 in range(8):
                            c = half * 8 + c8
                            pt_, ptb_, off = acc[c8]
                            ins = nc.tensor.matmul(pt_[:, off:off + C0], lhsT=xs[:, c * 128:(c + 1) * 128], rhs=Pe[:, b, :],
                                                   start=(b == 0), stop=(b == NB - 1))
                        return ins
                    m.op(m.PE, fg, reads=[xsb, Pe_b], writes=[pg_b[0], pg_b[1], pu_b[0], pu_b[1], py_b[0], py_b[1]])
                for c8 in range(8):
                    c = half * 8 + c8
                    pt_, ptb_, off = acc[c8]
                    evac(m, None, c8, xgT[:, c, :], pt_[:, off:off + C0], [ptb_], [xgT_b])
            wg_v = I["moe_w_gate"][e].rearrange("(c p) f -> p c f", p=128)
            wu_v = I["moe_w_up"][e].rearrange("(c p) f -> p c f", p=128)
            wd_v = I["moe_w_down"][e].rearrange("(j p) d -> p j d", p=128)
            for fc in range(nfc):
                s = kk % 2
                kk += 1
                f0 = fc * FC
                for c0 in range(0, DC, 4):
                    m.dma(m.POOL, wgt[s][:, c0:c0 + 4, :], wg_v[:, c0:c0 + 4, f0:f0 + FC], writes=[wg_b[s]])
                    m.dma(m.POOL, wut[s][:, c0:c0 + 4, :], wu_v[:, c0:c0 + 4, f0:f0 + FC], writes=[wu_b[s]])
                m.dma(m.POOL, wdt[s][:, :, :], wd_v[:, f0 // 128: f0 // 128 + FC // 128, :], writes=[wd_b[s]])
                for fb in range(FC // 128):
                    p = gi % 2
                    gi += 1
                    def fgate():
                        ins = None
                        for c in range(DC):
                            ins = nc.tensor.matmul(pg[p][:, :C0], lhsT=wgt[s][:, c, fb * 128:(fb + 1) * 128], rhs=xgT[:, c, :],
                                                   start=(c == 0), stop=(c == DC - 1))
                        return ins
                    m.op(m.PE, fgate, reads=[wg_b[s], xgT_b], writes=[pg_b[p]])
                    def fup():
                        ins = None
                        for c in range(DC):
                            ins = nc.tensor.matmul(pu[p][:, :C0], lhsT=wut[s][:, c, fb * 128:(fb + 1) * 128], rhs=xgT[:, c, :],
                                                   start=(c == 0), stop=(c == DC - 1))
                        return ins
                    m.op(m.PE, fup, reads=[wu_b[s], xgT_b], writes=[pu_b[p]])
                    m.op(m.ACT, lambda: nc.scalar.activation(out=gs[p][:, :], in_=pg[p][:, :C0], func=AF.Silu),
                         reads=[pg_b[p]], writes=[gs_b[p]])
                    m.op(m.DVE, lambda: nc.vector.tensor_tensor(out=hid[s][:, fb, :], in0=gs[p][:, :], in1=pu[p][:, :C0], op=ALU.mult),
                         reads=[gs_b[p], pu_b[p]], writes=[hid_b[s]])
                for jb in range(NJB):
                    for half in range(2):
                        p = gi % 2
                        gi += 1
                        def fdown():
                            ins = None
                            for q in range(2):
                                for j in range(FC // 128):
                                    ins = nc.tensor.matmul(py[p][:, q * 512:(q + 1) * 512], lhsT=hid[s][:, j, jb * 128:(jb + 1) * 128],
                                                           rhs=wdt[s][:, j, half * 1024 + q * 512: half * 1024 + (q + 1) * 512],
                                                           start=(j == 0), stop=(j == FC // 128 - 1))
                            return ins
                        m.op(m.PE, fdown, reads=[hid_b[s], wd_b[s]], writes=[py_b[p]])
                        ya = yea[:, jb, half * 1024:(half + 1) * 1024]
                        if fc == 0:
                            m.op(m.DVE, lambda: nc.vector.tensor_copy(out=ya, in_=py[p][:, :]), reads=[py_b[p]], writes=[yea_b[jb]])
                        else:
                            m.op(m.DVE, lambda: nc.vector.scalar_tensor_tensor(out=ya, in0=py[p][:, :], scalar=1.0, in1=ya,
                                                                               op0=ALU.mult, op1=ALU.add),
                                 reads=[py_b[p]], writes=[yea_b[jb]])
            for jb in range(NJB):
                if jb % 2 == 0:
                    m.op(m.ACT, lambda: nc.scalar.copy(out=yeb[:, jb, :], in_=yea[:, jb, :]), reads=[yea_b[jb]], writes=[yeb_b])
                else:
                    m.op(m.DVE, lambda: nc.vector.tensor_copy(out=yeb[:, jb, :], in_=yea[:, jb, :]), reads=[yea_b[jb]], writes=[yeb_b])
            for tb in range(NB):
                for half in range(2):
                    p = gi % 2
                    gi += 1
                    def fsc():
                        ins = None
                        for q in range(2):
                            for jb in range(NJB):
                                ins = nc.tensor.matmul(py[p][:, q * 512:(q + 1) * 512], lhsT=PTe[:, jb, tb * 128:(tb + 1) * 128],
                                                       rhs=yeb[:, jb, half * 1024 + q * 512: half * 1024 + (q + 1) * 512],
                                                       start=(jb == 0), stop=(jb == NJB - 1))
                        return ins
                    m.op(m.PE, fsc, reads=[PTe_b, yeb_b], writes=[py_b[p]])
                    ya = yacc[:, tb, half * 1024:(half + 1) * 1024]
                    if e == 0:
                        m.op(m.DVE, lambda: nc.vector.tensor_scalar(out=ya, in0=py[p][:, :], scalar1=gate_tile[:, tb, e:e + 1],
                                                                    scalar2=None, op0=ALU.mult),
                             reads=[py_b[p], gb], writes=[yacc_bufs[tb]])
                    else:
                        m.op(m.DVE, lambda: nc.vector.scalar_tensor_tensor(out=ya, in0=py[p][:, :], scalar=gate_tile[:, tb, e:e + 1],
                                                                           in1=ya, op0=ALU.mult, op1=ALU.add),
                             reads=[py_b[p], gb], writes=[yacc_bufs[tb]])
        m.barrier()
        esw.close()
        ln_setup(m, cx, es, tag, I["ln_ffn_g1"], I["ln_ffn_b1"], [H3_b], out_b)
        for bi, (t0, n) in enumerate(blocks):
            layer_norm_block(m, cx, yacc[:n, bi, :], [yacc_bufs[bi]], H3, t0, n, out, cx.gam, cx.bet)
        m.barrier()


REST_INPUTS = [("ffn_w_gate", (D, FFN_DENSE)), ("ffn_w_up", (D, FFN_DENSE)), ("ffn_w_down", (FFN_DENSE, D)),
               ("ln_ffn_g0", (D,)), ("ln_ffn_b0", (D,)),
               ("pool_w_in", (D, D)), ("pool_w_group", (4, 512, 512)), ("pool_scale", (D,)), ("pool_w_out", (D, D)),
               ("ln_mix_g1", (D,)), ("ln_mix_b1", (D,)),
               ("moe_w_router", (D, NEXP)), ("moe_b_router", (NEXP,)),
               ("moe_w_gate", (NEXP, D, FFN_EXPERT)), ("moe_w_up", (NEXP, D, FFN_EXPERT)), ("moe_w_down", (NEXP, FFN_EXPERT, D)),
               ("ln_ffn_g1", (D,)), ("ln_ffn_b1", (D,))]


def build_full(stop_after=None):
    m = MK()
    nc = m.nc
    I = declare_inputs(nc, L0_INPUTS + REST_INPUTS)
    out = nc.dram_tensor("out", [NOWN, D], F32, kind="ExternalOutput").ap()
    out_b = m.buf()
    S = make_scratch(m)
    layer0_mixer(m, I, S)
    swiglu_phase(m, "f0", S["H1"], S["H1_b"], 0, NT, I["ffn_w_gate"], I["ffn_w_up"], I["ffn_w_down"], FFN_DENSE,
                 I["ln_ffn_g0"], I["ln_ffn_b0"], S["H2"], S["H2_b"])
    persist(m, S)
    pool_phase(m, I, S)
    if USE_COMPACT_MOE:
        moe_phase(m, I, S, out, out_b)
    else:
        swiglu_phase(m, "f1", S["H3"], S["H3_b"], 0, NOWN, I["moe_w_gate"], I["moe_w_up"], I["moe_w_down"], FFN_EXPERT,
                     I["ln_ffn_g1"], I["ln_ffn_b1"], out, out_b, gates=make_moe_gates(m, I, S), n_exp=NEXP)
    m.SP.wait(m.all_tokens())
    return m


def host_prep_all(inp):
    maps = host_prep_l0(inp)
    g = lambda k: np.asarray(inp[k], np.float32)
    common = {
        "ffn_w_gate": g("ffn_w_gate")[0], "ffn_w_up": g("ffn_w_up")[0], "ffn_w_down": g("ffn_w_down")[0],
        "ln_ffn_g0": g("ln_ffn_g")[0], "ln_ffn_b0": g("ln_ffn_b")[0],
        "pool_w_in": g("pool_w_in")[0], "pool_w_group": g("pool_w_group")[0], "pool_scale": g("pool_scale")[0],
        "pool_w_out": g("pool_w_out")[0],
        "ln_mix_g1": g("ln_mix_g")[1], "ln_mix_b1": g("ln_mix_b")[1],
        "moe_w_router": g("moe_w_router")[0], "moe_b_router": g("moe_b_router")[0],
        "moe_w_gate": g("moe_w_gate")[0], "moe_w_up": g("moe_w_up")[0], "moe_w_down": g("moe_w_down")[0],
        "ln_ffn_g1": g("ln_ffn_g")[1], "ln_ffn_b1": g("ln_ffn_b")[1],
    }
    for d in maps:
        d.update(common)
    return maps


_NC_CACHE = {}


def kernel(**inputs):
    if "m" not in _NC_CACHE:
        _NC_CACHE["m"] = build_full()
    m = _NC_CACHE["m"]
    maps = host_prep_all(inputs)
    res = run_bass_kernel_spmd(m.nc, maps, core_ids=list(range(NCORE)))
    out = np.concatenate([np.asarray(res.results[c]["out"], np.float32) for c in range(NCORE)], axis=0)
    return out.reshape(1, NCORE * NOWN, D)
```

```python
from contextlib import ExitStack
import numpy as np
import concourse.bass as bass
import concourse.mybir as mybir
from concourse.bass_utils import run_bass_kernel_spmd

F32 = mybir.dt.float32
BF16 = mybir.dt.bfloat16
AF = mybir.ActivationFunctionType
ALU = mybir.AluOpType
AX = mybir.AxisListType

D = 2048
DC = 16
NCORE = 8
NOWN = 1024
NHALO = 16
NT = NOWN + NHALO
LFULL = 8192 + 16
NOTH = LFULL - NT
ALPHA = 4 ** 0.25
LN_EPS = 1e-5
RMS_EPS = 1e-6
FFN_DENSE = 5632
FFN_EXPERT = 7168
NEXP = 8
NEG = -1e30


class EngW:
    def __init__(self, m, e, name):
        self.m = m
        self.e = e
        self.name = name
        self.sem = m.es.enter_context(m.nc.semaphore("s_" + name))
        self.n = 0
        self.seen = {}

    def wait(self, toks):
        best = {}
        for t in toks:
            if t is None:
                continue
            sem, val = t
            if best.get(sem, 0) < val:
                best[sem] = val
        for sem, val in best.items():
            if self.seen.get(sem, 0) >= val:
                continue
            self.e.wait_ge(sem, val)
            self.seen[sem] = val

    def done(self, ins):
        self.n += 1
        ins.then_inc(self.sem, 1)
        return (self.sem, self.n)


class Buf:
    __slots__ = ("w", "r", "name")

    def __init__(self, name=""):
        self.w = None
        self.r = {}
        self.name = name

    def add_read(self, tok):
        sem, val = tok
        if self.r.get(sem, 0) < val:
            self.r[sem] = val


class MK:
    def __init__(self):
        self.es = ExitStack()
        self.nc = bass.Bass("TRN2", target_bir_lowering=False)
        nc = self.nc
        self.PE = EngW(self, nc.tensor, "pe")
        self.ACT = EngW(self, nc.scalar, "act")
        self.DVE = EngW(self, nc.vector, "dve")
        self.POOL = EngW(self, nc.gpsimd, "pool")
        self.SP = EngW(self, nc.sync, "sp")
        self.engs = [self.PE, self.ACT, self.DVE, self.POOL, self.SP]
        self.dsem = {}
        for q, n in ((self.SP, 20), (self.POOL, 20), (self.ACT, 8)):
            self.dsem[q.name] = [[self.es.enter_context(nc.semaphore("d_%s%d" % (q.name, i))), 0] for i in range(n)]
        self.dptr = {"sp": 0, "pool": 0, "act": 0}
        self.bufs = []

    def buf(self, name=""):
        b = Buf(name)
        self.bufs.append(b)
        return b

    def sb(self, name, shape, dt):
        return self.es.enter_context(self.nc.sbuf_tensor(name, shape, dt))

    def _deps(self, reads, writes):
        toks = []
        for b in reads:
            toks.append(b.w)
        for b in writes:
            toks.append(b.w)
            for sem, val in b.r.items():
                toks.append((sem, val))
        return toks

    def _commit(self, tok, reads, writes):
        for b in reads:
            b.add_read(tok)
        for b in writes:
            b.w = tok
            b.r = {}

    def op(self, eng, fn, reads=(), writes=()):
        eng.wait(self._deps(reads, writes))
        ins = fn()
        tok = eng.done(ins)
        self._commit(tok, reads, writes)
        return tok

    def dma(self, q, out, in_, reads=(), writes=()):
        lst = self.dsem[q.name]
        i = self.dptr[q.name]
        self.dptr[q.name] = (i + 1) % len(lst)
        sem, tot = lst[i]
        deps = self._deps(reads, writes)
        if tot > 0:
            deps.append((sem, tot))
        q.wait(deps)
        q.e.dma_start(out=out, in_=in_).then_inc(sem, 16)
        lst[i][1] = tot + 16
        tok = (sem, tot + 16)
        self._commit(tok, reads, writes)
        return tok

    def all_tokens(self):
        toks = []
        for e in self.engs:
            if e.n > 0:
                toks.append((e.sem, e.n))
        for name, lst in self.dsem.items():
            for sem, tot in lst:
                if tot > 0:
                    toks.append((sem, tot))
        return toks

    def barrier(self):
        toks = self.all_tokens()
        for e in self.engs:
            e.wait(toks)
        for b in self.bufs:
            b.w = None
            b.r = {}
        self.bufs = []


def tok_blocks(ntok):
    out = []
    r = ntok % 128
    t = 0
    if r:
        out.append((0, r))
        t = r
    while t < ntok:
        out.append((t, 128))
        t += 128
    return out


def tok_groups(ntok, maxn=512):
    ng = -(-ntok // maxn)
    base = -(-ntok // ng)
    out = []
    t = 0
    while t < ntok:
        n = min(base, ntok - t)
        out.append((t, n))
        t += n
    return out


class Ctx:
    pass


def make_identity(m, ident_bf, ident_f32=None):
    nc = m.nc
    b = m.buf("ident")
    def f():
        nc.gpsimd.memset(ident_bf[:], 1.0)
        return nc.gpsimd.affine_select(out=ident_bf[:], in_=ident_bf[:], pattern=[[-1, 128]],
                                       compare_op=ALU.is_equal, fill=0.0, base=0, channel_multiplier=1)
    m.op(m.POOL, f, writes=[b])
    b2 = None
    if ident_f32 is not None:
        b2 = m.buf("identf")
        def g():
            nc.gpsimd.memset(ident_f32[:], 1.0)
            return nc.gpsimd.affine_select(out=ident_f32[:], in_=ident_f32[:], pattern=[[-1, 128]],
                                           compare_op=ALU.is_equal, fill=0.0, base=0, channel_multiplier=1)
        m.op(m.POOL, g, writes=[b2])
    return b, b2


def load_fm(m, H_dram, row0, ntok, hT, hT_buf, ident, ident_buf, stage, stage_bufs, ps_t, ps_bufs, Hbuf=None, q=None):
    nc = m.nc
    k = 0
    first = True
    for bi, (t0, n) in enumerate(tok_blocks(ntok)):
        st, sb_ = stage[bi % 2], stage_bufs[bi % 2]
        m.dma(q or m.POOL, st[:n, :], H_dram[row0 + t0: row0 + t0 + n, :], reads=([Hbuf] if Hbuf else []), writes=[sb_])
        for half in range(2):
            ps, pb = ps_t[k % 2], ps_bufs[k % 2]
            k += 1
            def f():
                ins = None
                for j in range(8):
                    c = half * 8 + j
                    ins = nc.tensor.transpose(ps[:, j * 128: j * 128 + n], st[:n, c * 128:(c + 1) * 128], ident[:n, :n])
                return ins
            m.op(m.PE, f, reads=[sb_, ident_buf], writes=[pb])
            src = ps[:, :].rearrange("p (j t) -> p j t", j=8)[:, :, :n]
            dst = hT[:, half * 8:(half + 1) * 8, t0:t0 + n]
            if k % 2 == 0:
                m.op(m.DVE, lambda: nc.vector.tensor_copy(out=dst, in_=src), reads=[pb], writes=[hT_buf] if first else [hT_buf])
            else:
                m.op(m.ACT, lambda: nc.scalar.copy(out=dst, in_=src), reads=[pb], writes=[hT_buf])
            first = False


def layer_norm_block(m, cx, src_ps, src_bufs, Hin_dram, row, n, Hout_dram, gam, bet, out_row=None):
    nc = m.nc
    i = cx.ln_i
    cx.ln_i += 1
    hres, hb = cx.hres[i % 2], cx.hres_bufs[i % 2]
    tln, tb = cx.tln[i % 2], cx.tln_bufs[i % 2]
    st, sbf = cx.stat[i % 2], cx.stat_bufs[i % 2]
    m.dma(m.SP, hres[:n, :], Hin_dram[row:row + n, :], reads=cx.Hin_bufs, writes=[hb])
    m.op(m.DVE, lambda: nc.vector.scalar_tensor_tensor(out=tln[:n, :], in0=hres[:n, :], scalar=ALPHA, in1=src_ps,
                                                       op0=ALU.mult, op1=ALU.add),
         reads=[hb] + list(src_bufs), writes=[tb])
    def fstats():
        ins = None
        for j in range(4):
            ins = nc.vector.bn_stats(out=st[:n, j * 6:(j + 1) * 6], in_=tln[:n, j * 512:(j + 1) * 512])
        return ins
    m.op(m.DVE, fstats, reads=[tb], writes=[sbf])
    m.op(m.DVE, lambda: nc.vector.bn_aggr(out=st[:n, 24:26], in_=st[:n, 0:24]), reads=[sbf], writes=[sbf])
    m.op(m.DVE, lambda: nc.vector.tensor_scalar(out=st[:n, 28:29], in0=st[:n, 25:26], scalar1=LN_EPS, scalar2=None,
                                                op0=ALU.add), reads=[sbf], writes=[sbf])
    m.op(m.ACT, lambda: nc.scalar.sqrt(out=st[:n, 29:30], in_=st[:n, 28:29]), reads=[sbf], writes=[sbf])
    m.op(m.DVE, lambda: nc.vector.reciprocal(out=st[:n, 26:27], in_=st[:n, 29:30]), reads=[sbf], writes=[sbf])
    m.op(m.DVE, lambda: nc.vector.scalar_tensor_tensor(out=st[:n, 27:28], in0=st[:n, 24:25], scalar=-1.0, in1=st[:n, 26:27],
                                                       op0=ALU.mult, op1=ALU.mult), reads=[sbf], writes=[sbf])
    m.op(m.ACT, lambda: nc.scalar.activation(out=tln[:n, :], in_=tln[:n, :], func=AF.Identity,
                                             bias=st[:n, 27:28], scale=st[:n, 26:27]), reads=[sbf], writes=[tb])
    m.op(m.POOL, lambda: nc.gpsimd.tensor_tensor(out=tln[:n, :], in0=tln[:n, :], in1=gam[:n, :], op=ALU.mult),
         reads=[cx.gb_buf], writes=[tb])
    m.op(m.POOL, lambda: nc.gpsimd.tensor_tensor(out=tln[:n, :], in0=tln[:n, :], in1=bet[:n, :], op=ALU.add),
         reads=[cx.gb_buf], writes=[tb])
    orow = row if out_row is None else out_row
    m.dma(m.SP, Hout_dram[orow:orow + n, :], tln[:n, :], reads=[tb], writes=[cx.Hout_buf])


def ln_setup(m, cx, es, tag, g_dram, b_dram, Hin_bufs, Hout_buf):
    nc = m.nc
    cx.ln_i = 0
    cx.hres = [es.enter_context(nc.sbuf_tensor("hres%s%d" % (tag, i), [128, D], F32)) for i in range(2)]
    cx.tln = [es.enter_context(nc.sbuf_tensor("tln%s%d" % (tag, i), [128, D], F32)) for i in range(2)]
    cx.stat = [es.enter_context(nc.sbuf_tensor("stat%s%d" % (tag, i), [128, 32], F32)) for i in range(2)]
    cx.hres_bufs = [m.buf() for _ in range(2)]
    cx.tln_bufs = [m.buf() for _ in range(2)]
    cx.stat_bufs = [m.buf() for _ in range(2)]
    cx.gam = es.enter_context(nc.sbuf_tensor("gam" + tag, [128, D], F32))
    cx.bet = es.enter_context(nc.sbuf_tensor("bet" + tag, [128, D], F32))
    cx.gb_buf = m.buf()
    m.dma(m.SP, cx.gam[:, :], g_dram.partition_broadcast(128), writes=[cx.gb_buf])
    m.dma(m.SP, cx.bet[:, :], b_dram.partition_broadcast(128), writes=[cx.gb_buf])
    cx.Hin_bufs = Hin_bufs
    cx.Hout_buf = Hout_buf


def swiglu_phase(m, tag, Hin, Hin_buf, row0, ntok, wg, wu, wd, F, g_dram, b_dram, Hout, Hout_buf,
                 gates=None, n_exp=1):
    nc = m.nc
    FC = 256
    nfc = F // FC
    with ExitStack() as es:
        cx = Ctx()
        blocks = tok_blocks(ntok)
        groups = tok_groups(ntok)
        nb = len(blocks)
        hT = es.enter_context(nc.sbuf_tensor("hT" + tag, [128, DC, ntok], BF16))
        hT_buf = m.buf()
        ident = es.enter_context(nc.sbuf_tensor("ident" + tag, [128, 128], BF16))
        ident_buf, _ = make_identity(m, ident)
        yacc = es.enter_context(nc.sbuf_tensor("yacc" + tag, [128, nb, D], F32))
        yacc_bufs = [m.buf() for _ in range(nb)]
        esw = ExitStack()
        wgt = [esw.enter_context(nc.sbuf_tensor("wg%s%d" % (tag, i), [128, DC, FC], BF16)) for i in range(2)]
        wut = [esw.enter_context(nc.sbuf_tensor("wu%s%d" % (tag, i), [128, DC, FC], BF16)) for i in range(2)]
        wdt = [esw.enter_context(nc.sbuf_tensor("wd%s%d" % (tag, i), [128, FC // 128, D], BF16)) for i in range(2)]
        wg_b = [m.buf() for _ in range(2)]
        wu_b = [m.buf() for _ in range(2)]
        wd_b = [m.buf() for _ in range(2)]
        hid = [esw.enter_context(nc.sbuf_tensor("hid%s%d" % (tag, i), [128, FC // 128, ntok], BF16)) for i in range(2)]
        hid_b = [m.buf() for _ in range(2)]
        gs = [esw.enter_context(nc.sbuf_tensor("gs%s%d" % (tag, i), [128, 512], F32)) for i in range(2)]
        gs_b = [m.buf() for _ in range(2)]
        pg = [es.enter_context(nc.psum_tensor("pg%s%d" % (tag, i), [128, 512], F32)) for i in range(2)]
        pu = [es.enter_context(nc.psum_tensor("pu%s%d" % (tag, i), [128, 512], F32)) for i in range(2)]
        py = [es.enter_context(nc.psum_tensor("py%s%d" % (tag, i), [128, 1024], F32)) for i in range(2)]
        pg_b = [m.buf() for _ in range(2)]
        pu_b = [m.buf() for _ in range(2)]
        py_b = [m.buf() for _ in range(2)]
        gate_tile = None
        if gates is not None:
            gate_tile = esw.enter_context(nc.sbuf_tensor("gatet" + tag, [128, nb, n_exp], F32))
            cx.gate_tile = gate_tile
            cx.pg, cx.pg_b, cx.pu, cx.pu_b = pg, pg_b, pu, pu_b
        with ExitStack() as es2:
            stage = [es2.enter_context(nc.sbuf_tensor("stg%s%d" % (tag, i), [128, D], BF16)) for i in range(2)]
            stage_b = [m.buf() for _ in range(2)]
            pst = [pg[i][:, :].bitcast(BF16) for i in range(2)]
            load_fm(m, Hin, row0, ntok, hT, hT_buf, ident, ident_buf, stage, stage_b, pst, pg_b, Hbuf=Hin_buf)
            m.barrier()
        if gates is not None:
            gates(cx, es, hT, hT_buf, blocks)
        kk = 0
        gi = 0
        for e in range(n_exp):
            wg_e = wg[e] if n_exp > 1 else wg
            wu_e = wu[e] if n_exp > 1 else wu
            wd_e = wd[e] if n_exp > 1 else wd
            wg_v = wg_e.rearrange("(c p) f -> p c f", p=128)
            wu_v = wu_e.rearrange("(c p) f -> p c f", p=128)
            wd_v = wd_e.rearrange("(j p) d -> p j d", p=128)
            for fc in range(nfc):
                s = kk % 2
                kk += 1
                f0 = fc * FC
                for c0 in range(0, DC, 4):
                    m.dma(m.POOL, wgt[s][:, c0:c0 + 4, :], wg_v[:, c0:c0 + 4, f0:f0 + FC], writes=[wg_b[s]])
                    m.dma(m.POOL, wut[s][:, c0:c0 + 4, :], wu_v[:, c0:c0 + 4, f0:f0 + FC], writes=[wu_b[s]])
                m.dma(m.POOL, wdt[s][:, :, :], wd_v[:, f0 // 128: f0 // 128 + FC // 128, :], writes=[wd_b[s]])
                for (t0, n) in groups:
                    for fb in range(FC // 128):
                        p = gi % 2
                        gi += 1
                        def fgate():
                            ins = None
                            for c in range(DC):
                                ins = nc.tensor.matmul(pg[p][:, :n], lhsT=wgt[s][:, c, fb * 128:(fb + 1) * 128],
                                                       rhs=hT[:, c, t0:t0 + n], start=(c == 0), stop=(c == DC - 1))
                            return ins
                        m.op(m.PE, fgate, reads=[wg_b[s], hT_buf], writes=[pg_b[p]])
                        def fup():
                            ins = None
                            for c in range(DC):
                                ins = nc.tensor.matmul(pu[p][:, :n], lhsT=wut[s][:, c, fb * 128:(fb + 1) * 128],
                                                       rhs=hT[:, c, t0:t0 + n], start=(c == 0), stop=(c == DC - 1))
                            return ins
                        m.op(m.PE, fup, reads=[wu_b[s], hT_buf], writes=[pu_b[p]])
                        m.op(m.ACT, lambda: nc.scalar.activation(out=gs[p][:, :n], in_=pg[p][:, :n], func=AF.Silu),
                             reads=[pg_b[p]], writes=[gs_b[p]])
                        m.op(m.DVE, lambda: nc.vector.tensor_tensor(out=hid[s][:, fb, t0:t0 + n], in0=gs[p][:, :n],
                                                                    in1=pu[p][:, :n], op=ALU.mult),
                             reads=[gs_b[p], pu_b[p]], writes=[hid_b[s]])
                for bi, (t0, n) in enumerate(blocks):
                    for half in range(2):
                        p = gi % 2
                        gi += 1
                        def fdown():
                            ins = None
                            for q in range(2):
                                for j in range(FC // 128):
                                    ins = nc.tensor.matmul(py[p][:n, q * 512:(q + 1) * 512], lhsT=hid[s][:, j, t0:t0 + n],
                                                           rhs=wdt[s][:, j, half * 1024 + q * 512: half * 1024 + (q + 1) * 512],
                                                           start=(j == 0), stop=(j == FC // 128 - 1))
                            return ins
                        m.op(m.PE, fdown, reads=[hid_b[s], wd_b[s]], writes=[py_b[p]])
                        ya = yacc[:n, bi, half * 1024:(half + 1) * 1024]
                        if e == 0 and fc == 0:
                            if gate_tile is None:
                                m.op(m.DVE, lambda: nc.vector.tensor_copy(out=ya, in_=py[p][:n, :]),
                                     reads=[py_b[p]], writes=[yacc_bufs[bi]])
                            else:
                                m.op(m.DVE, lambda: nc.vector.tensor_scalar(out=ya, in0=py[p][:n, :],
                                                                            scalar1=gate_tile[:n, bi, e:e + 1], scalar2=None,
                                                                            op0=ALU.mult),
                                     reads=[py_b[p], cx.gate_buf], writes=[yacc_bufs[bi]])
                        else:
                            sc = 1.0 if gate_tile is None else gate_tile[:n, bi, e:e + 1]
                            rd = [py_b[p]] + ([cx.gate_buf] if gate_tile is not None else [])
                            m.op(m.DVE, lambda: nc.vector.scalar_tensor_tensor(out=ya, in0=py[p][:n, :], scalar=sc, in1=ya,
                                                                               op0=ALU.mult, op1=ALU.add),
                                 reads=rd, writes=[yacc_bufs[bi]])
        m.barrier()
        esw.close()
        ln_setup(m, cx, es, tag, g_dram, b_dram, [Hin_buf], Hout_buf)
        for bi, (t0, n) in enumerate(blocks):
            layer_norm_block(m, cx, yacc[:n, bi, :], [yacc_bufs[bi]], Hin, row0 + t0, n, Hout, cx.gam, cx.bet)
        m.barrier()


KGROUPS = [(0, 16)] + [(16 + 512 * i, 512) for i in range(16)]
SC_MLA = 192 ** -0.5
SC_FOX = 128 ** -0.5


def load_w(m, q, tile, wv, c0=0, nchunk=None, step=4, bufs=None):
    nchunk = tile.shape[1] if nchunk is None else nchunk
    for c in range(0, nchunk, step):
        e = min(nchunk, c + step)
        m.dma(q, tile[:, c:e, :], wv[:, c:e, :], writes=bufs)


def xgroup_fm(m, cx, xkv, r0, n, xT, xT_buf):
    nc = m.nc
    blocks = [(0, n)] if n <= 128 else [(i * 128, 128) for i in range(n // 128)]
    st, sb_ = cx.stage[cx.si % 2], cx.stage_b[cx.si % 2]
    cx.si += 1
    if n <= 128:
        m.dma(m.POOL, st[:n, 0, :], xkv[r0:r0 + n, :], writes=[sb_])
    else:
        m.dma(m.POOL, st[:, :n // 128, :], xkv[r0:r0 + n, :].rearrange("(b p) d -> p b d", p=128), writes=[sb_])
    for bi, (t0, bn) in enumerate(blocks):
        for half in range(2):
            k = cx.ti % 2
            cx.ti += 1
            ps, pb = cx.pst[k], cx.pst_b[k]
            def f():
                ins = None
                for j in range(8):
                    c = half * 8 + j
                    ins = nc.tensor.transpose(ps[:, j * 128: j * 128 + bn], st[:bn, bi, c * 128:(c + 1) * 128],
                                              cx.ident[:bn, :bn])
                return ins
            m.op(m.PE, f, reads=[sb_, cx.ident_buf], writes=[pb])
            src = ps[:, :].rearrange("p (j t) -> p j t", j=8)[:, :, :bn]
            dst = xT[:, half * 8:(half + 1) * 8, t0:t0 + bn]
            if k == 0:
                m.op(m.DVE, lambda: nc.vector.tensor_copy(out=dst, in_=src), reads=[pb], writes=[xT_buf])
            else:
                m.op(m.ACT, lambda: nc.scalar.copy(out=dst, in_=src), reads=[pb], writes=[xT_buf])
    return blocks


def rms_norm_tm(m, cx, src_ps, src_buf, n, gam_bc, out_bf, out_buf):
    nc = m.nc
    i = cx.ri % 2
    cx.ri += 1
    sq, sqb = cx.rsq[i], cx.rsq_b[i]
    st, stb = cx.rst[i], cx.rst_b[i]
    m.op(m.ACT, lambda: nc.scalar.activation(out=sq[:n, :], in_=src_ps, func=AF.Square, accum_out=st[:n, 0:1]),
         reads=[src_buf], writes=[sqb, stb])
    m.op(m.DVE, lambda: nc.vector.tensor_scalar(out=st[:n, 1:2], in0=st[:n, 0:1], scalar1=1.0 / 512, scalar2=RMS_EPS,
                                                op0=ALU.mult, op1=ALU.add), reads=[stb], writes=[stb])
    m.op(m.ACT, lambda: nc.scalar.sqrt(out=st[:n, 3:4], in_=st[:n, 1:2]), reads=[stb], writes=[stb])
    m.op(m.DVE, lambda: nc.vector.reciprocal(out=st[:n, 2:3], in_=st[:n, 3:4]), reads=[stb], writes=[stb])
    m.op(m.DVE, lambda: nc.vector.scalar_tensor_tensor(out=out_bf, in0=src_ps, scalar=st[:n, 2:3], in1=gam_bc[:n, :],
                                                       op0=ALU.mult, op1=ALU.mult),
         reads=[src_buf, stb, cx.g_buf], writes=[out_buf])


def transpose_tm_to_fm(m, cx, src_bf, src_buf, n, nchunk, dstT, dst_buf, t0):
    nc = m.nc
    k = cx.ti % 2
    cx.ti += 1
    ps, pb = cx.pst[k], cx.pst_b[k]
    def f():
        ins = None
        for j in range(nchunk):
            ins = nc.tensor.transpose(ps[:, j * 128: j * 128 + n], src_bf[:n, j * 128:(j + 1) * 128], cx.ident[:n, :n])
        return ins
    m.op(m.PE, f, reads=[src_buf, cx.ident_buf], writes=[pb])
    src = ps[:, 0:nchunk * 128].rearrange("p (j t) -> p j t", j=nchunk)[:, :, :n]
    m.op(m.DVE, lambda: nc.vector.tensor_copy(out=dstT[:, 0:nchunk, t0:t0 + n], in_=src), reads=[pb], writes=[dst_buf])


def rope_tm(m, cx, src, src_buf, n, nh, cs, cs_buf, out_bf, out_buf, scale):
    nc = m.nc
    i = cx.rpi % 2
    cx.rpi += 1
    t1, t1b = cx.rp[i], cx.rp_b[i]
    for h in range(nh):
        x1 = src[:, h * 64: h * 64 + 32]
        x2 = src[:, h * 64 + 32: h * 64 + 64]
        cos = cs[:n, 0:32]
        sin = cs[:n, 32:64]
        a = t1[:n, 0:32]
        b = t1[:n, 32:64]
        c_ = t1[:n, 64:96]
        d_ = t1[:n, 96:128]
        def f():
            nc.vector.tensor_tensor(out=a, in0=x1, in1=cos, op=ALU.mult)
            nc.vector.tensor_tensor(out=b, in0=x2, in1=sin, op=ALU.mult)
            nc.vector.tensor_tensor(out=c_, in0=x2, in1=cos, op=ALU.mult)
            return nc.vector.tensor_tensor(out=d_, in0=x1, in1=sin, op=ALU.mult)
        m.op(m.DVE, f, reads=[src_buf, cs_buf], writes=[t1b])
        def g():
            nc.vector.scalar_tensor_tensor(out=out_bf[:n, h * 64: h * 64 + 32], in0=a, scalar=scale, in1=a, op0=ALU.mult, op1=ALU.bypass) if False else None
            nc.vector.tensor_tensor(out=a, in0=a, in1=b, op=ALU.subtract)
            return nc.vector.tensor_tensor(out=c_, in0=c_, in1=d_, op=ALU.add)
        m.op(m.DVE, g, reads=[t1b], writes=[t1b])
        def h2():
            nc.vector.tensor_scalar(out=out_bf[:n, h * 64: h * 64 + 32], in0=a, scalar1=scale, scalar2=None, op0=ALU.mult)
            return nc.vector.tensor_scalar(out=out_bf[:n, h * 64 + 32: h * 64 + 64], in0=c_, scalar1=scale, scalar2=None,
                                           op0=ALU.mult)
        m.op(m.DVE, h2, reads=[t1b], writes=[out_buf])


def kv_common_setup(m, cx, es, tag):
    nc = m.nc
    cx.si = cx.ti = cx.ri = cx.rpi = cx.ei = 0
    cx.ident = es.enter_context(nc.sbuf_tensor("ident" + tag, [128, 128], BF16))
    cx.ident_buf, _ = make_identity(m, cx.ident)
    cx.stage = [es.enter_context(nc.sbuf_tensor("stg%s%d" % (tag, i), [128, 4, D], BF16)) for i in range(2)]
    cx.stage_b = [m.buf() for _ in range(2)]
    cx.pst_f = [es.enter_context(nc.psum_tensor("pst%s%d" % (tag, i), [128, 512], F32)) for i in range(2)]
    cx.pst = [t[:, :].bitcast(BF16) for t in cx.pst_f]
    cx.pst_b = [m.buf() for _ in range(2)]
    cx.pm = [es.enter_context(nc.psum_tensor("pm%s%d" % (tag, i), [128, 1024], F32)) for i in range(3)]
    cx.pm_b = [m.buf() for _ in range(3)]
    cx.pmi = 0
    cx.ev = [es.enter_context(nc.sbuf_tensor("ev%s%d" % (tag, i), [128, 1024], BF16)) for i in range(3)]
    cx.ev_b = [m.buf() for _ in range(3)]


def next_pm(cx):
    i = cx.pmi % 3
    cx.pmi += 1
    return cx.pm[i], cx.pm_b[i]


def next_ev(cx):
    i = cx.ei % 3
    cx.ei += 1
    return cx.ev[i], cx.ev_b[i]


def evac(m, cx, k, out, in_, reads, writes, scale=None):
    nc = m.nc
    if k % 2 == 0:
        if scale is None:
            m.op(m.DVE, lambda: nc.vector.tensor_copy(out=out, in_=in_), reads=reads, writes=writes)
        else:
            m.op(m.DVE, lambda: nc.vector.tensor_scalar(out=out, in0=in_, scalar1=scale, scalar2=None, op0=ALU.mult),
                 reads=reads, writes=writes)
    else:
        if scale is None:
            m.op(m.ACT, lambda: nc.scalar.copy(out=out, in_=in_), reads=reads, writes=writes)
        else:
            m.op(m.ACT, lambda: nc.scalar.mul(out=out, in_=in_, mul=scale), reads=reads, writes=writes)


def fm_heads(m, cx, w_t, w_buf, nkc, rhsT, rhs_buf, n, heads, dst_dram, col0, dst_buf, scale=None, hw=128):
    nc = m.nc
    for h in range(heads):
        ps, pb = next_pm(cx)
        def f():
            ins = None
            for kc in range(nkc):
                ins = nc.tensor.matmul(ps[:hw, :n], lhsT=w_t[:, kc, h * hw:(h + 1) * hw], rhs=rhsT[:, kc, :n],
                                       start=(kc == 0), stop=(kc == nkc - 1))
            return ins
        m.op(m.PE, f, reads=[w_buf, rhs_buf], writes=[pb])
        ev, evb = next_ev(cx)
        evac(m, cx, h, ev[:hw, :n], ps[:hw, :n], [pb], [evb], scale)
        m.dma(m.SP, dst_dram[h, :, col0:col0 + n], ev[:hw, :n], reads=[evb], writes=[dst_buf])


def tm_out(m, cx, lhsT, lhs_buf, nkc, t0, bn, w_t, w_buf, wc0, ncols):
    nc = m.nc
    ps, pb = next_pm(cx)
    def f():
        ins = None
        for q0 in range(0, ncols, 512):
            qn = min(512, ncols - q0)
            for kc in range(nkc):
                ins = nc.tensor.matmul(ps[:bn, q0:q0 + qn], lhsT=lhsT[:, kc, t0:t0 + bn],
                                       rhs=w_t[:, kc, wc0 + q0: wc0 + q0 + qn], start=(kc == 0), stop=(kc == nkc - 1))
        return ins
    m.op(m.PE, f, reads=[lhs_buf, w_buf], writes=[pb])
    return ps, pb


def kv_pass_mla(m, xkv, cs_all, w_in, w_ukv_p, w_uq_p, kvn, qn_, kvalid, S):
    nc = m.nc
    with ExitStack() as es0:
        es = es0
        kvs = es.enter_context(nc.sbuf_tensor("kvs", [1, LFULL], F32))
        kvs16 = es.enter_context(nc.sbuf_tensor("kvs16", [1, LFULL], BF16))
        kvs_b = m.buf()
        m.dma(m.SP, kvs[:, :], kvalid.rearrange("(o n) -> o n", o=1), writes=[kvs_b])
        m.op(m.DVE, lambda: nc.vector.tensor_copy(out=kvs16[:, :], in_=kvs[:, :]), reads=[kvs_b], writes=[kvs_b])
        m.dma(m.SP, S["KAUX_M"][64:65, :], kvs16[:, :], reads=[kvs_b], writes=[S["KAUX_M_b"]])
        m.op(m.DVE, lambda: nc.vector.memset(kvs16[:, 0:NT], 1.0), writes=[kvs_b])
        for h in range(8):
            m.dma(m.SP, S["QAUX_M"][h, 64:65, :], kvs16[:, 0:NT], reads=[kvs_b], writes=[S["QAUX_M_b"]])
        m.barrier()
    persist(m, S)
    with ExitStack() as es:
        cx = Ctx()
        kv_common_setup(m, cx, es, "a1")
        win_v = w_in.rearrange("(c p) f -> p c f", p=128)
        wa = es.enter_context(nc.sbuf_tensor("wa", [128, DC, 1088], BF16))
        wukv = es.enter_context(nc.sbuf_tensor("wukv", [128, 4, 2048], BF16))
        wuq = es.enter_context(nc.sbuf_tensor("wuq", [128, 4, 1536], BF16))
        w_b = m.buf()
        load_w(m, m.POOL, wa, win_v[:, :, 0:1088], bufs=[w_b])
        load_w(m, m.POOL, wukv, w_ukv_p.rearrange("(c p) f -> p c f", p=128), bufs=[w_b])
        load_w(m, m.POOL, wuq, w_uq_p.rearrange("(c p) f -> p c f", p=128), bufs=[w_b])
        gkv = es.enter_context(nc.sbuf_tensor("gkv", [128, 512], F32))
        gq = es.enter_context(nc.sbuf_tensor("gq", [128, 512], F32))
        cx.g_buf = m.buf()
        m.dma(m.SP, gkv[:, :], kvn.partition_broadcast(128), writes=[cx.g_buf])
        m.dma(m.SP, gq[:, :], qn_.partition_broadcast(128), writes=[cx.g_buf])
        xT = [es.enter_context(nc.sbuf_tensor("xTa%d" % i, [128, DC, 512], BF16)) for i in range(2)]
        xT_b = [m.buf() for _ in range(2)]
        cT = [es.enter_context(nc.sbuf_tensor("cTa%d" % i, [128, 4, 512], BF16)) for i in range(2)]
        cT_b = [m.buf() for _ in range(2)]
        cqT = [es.enter_context(nc.sbuf_tensor("cqTa%d" % i, [128, 4, 512], BF16)) for i in range(2)]
        cqT_b = [m.buf() for _ in range(2)]
        cx.rsq = [es.enter_context(nc.sbuf_tensor("rsq%d" % i, [128, 512], F32)) for i in range(2)]
        cx.rsq_b = [m.buf() for _ in range(2)]
        cx.rst = [es.enter_context(nc.sbuf_tensor("rst%d" % i, [128, 4], F32)) for i in range(2)]
        cx.rst_b = [m.buf() for _ in range(2)]
        cx.rp = [es.enter_context(nc.sbuf_tensor("rp%d" % i, [128, 128], F32)) for i in range(2)]
        cx.rp_b = [m.buf() for _ in range(2)]
        cn = [es.enter_context(nc.sbuf_tensor("cn%d" % i, [128, 512], BF16)) for i in range(2)]
        cn_b = [m.buf() for _ in range(2)]
        cst = [es.enter_context(nc.sbuf_tensor("cst%d" % i, [128, 64], F32)) for i in range(2)]
        cst_b = [m.buf() for _ in range(2)]
        kr = [es.enter_context(nc.sbuf_tensor("kr%d" % i, [128, 64], BF16)) for i in range(2)]
        kr_b = [m.buf() for _ in range(2)]
        qr = [es.enter_context(nc.sbuf_tensor("qr%d" % i, [128, 512], BF16)) for i in range(2)]
        qr_b = [m.buf() for _ in range(2)]
        krT = [es.enter_context(nc.sbuf_tensor("krT%d" % i, [64, 512], BF16)) for i in range(2)]
        krT_b = [m.buf() for _ in range(2)]
        qrT = [es.enter_context(nc.sbuf_tensor("qrT%d" % i, [64, 8, 128], BF16)) for i in range(2)]
        qrT_b = [m.buf() for _ in range(2)]
        bi_glob = 0
        for gi, (r0, n) in enumerate(KGROUPS):
            own = r0 < NT
            x_t, x_b = xT[gi % 2], xT_b[gi % 2]
            c_t, c_b = cT[gi % 2], cT_b[gi % 2]
            blocks = xgroup_fm(m, cx, xkv, r0, n, x_t, x_b)
            for (t0, bn) in blocks:
                j = bi_glob % 2
                bi_glob += 1
                m.dma(m.SP, cst[j][:bn, :], cs_all[r0 + t0: r0 + t0 + bn, :], writes=[cst_b[j]])
                ps, pb = tm_out(m, cx, x_t, x_b, DC, t0, bn, wa, w_b, 512, 576)
                rms_norm_tm(m, cx, ps[:bn, 0:512], pb, bn, gkv, cn[j][:bn, :], cn_b[j])
                transpose_tm_to_fm(m, cx, cn[j], cn_b[j], bn, 4, c_t, c_b, t0)
                rope_tm(m, cx, ps[:bn, 512:576], pb, bn, 1, cst[j], cst_b[j], kr[j], kr_b[j], 1.0)
                k = cx.ti % 2
                cx.ti += 1
                pt, ptb = cx.pst[k], cx.pst_b[k]
                m.op(m.PE, lambda: nc.tensor.transpose(pt[:64, :bn], kr[j][:bn, 0:64], cx.ident[:bn, :bn]),
                     reads=[kr_b[j], cx.ident_buf], writes=[ptb])
                m.op(m.ACT, lambda: nc.scalar.copy(out=krT[gi % 2][:, t0:t0 + bn], in_=pt[:64, :bn]), reads=[ptb],
                     writes=[krT_b[gi % 2]])
                ps2, pb2 = tm_out(m, cx, c_t, c_b, 4, t0, bn, wukv, w_b, 1024, 1024)
                ev, evb = next_ev(cx)
                evac(m, cx, bi_glob, ev[:bn, :], ps2[:bn, :], [pb2], [evb])
                m.dma(m.SP, S["VV"][r0 + t0: r0 + t0 + bn, 0:8, :], ev[:bn, :].rearrange("p (h d) -> p h d", h=8),
                      reads=[evb], writes=[S["VV_b"]])
                if own:
                    cq_t, cq_b = cqT[gi % 2], cqT_b[gi % 2]
                    ps3, pb3 = tm_out(m, cx, x_t, x_b, DC, t0, bn, wa, w_b, 0, 512)
                    rms_norm_tm(m, cx, ps3[:bn, 0:512], pb3, bn, gq, cn[j][:bn, :], cn_b[j])
                    transpose_tm_to_fm(m, cx, cn[j], cn_b[j], bn, 4, cq_t, cq_b, t0)
                    ps4, pb4 = tm_out(m, cx, cq_t, cq_b, 4, t0, bn, wuq, w_b, 1024, 512)
                    rope_tm(m, cx, ps4[:bn, 0:512], pb4, bn, 8, cst[j], cst_b[j], qr[j], qr_b[j], SC_MLA)
                    k = cx.ti % 2
                    cx.ti += 1
                    pt, ptb = cx.pst[k], cx.pst_b[k]
                    def ftr():
                        ins = None
                        for h in range(8):
                            ins = nc.tensor.transpose(pt[:64, h * 128: h * 128 + bn], qr[j][:bn, h * 64:(h + 1) * 64],
                                                      cx.ident[:bn, :bn])
                        return ins
                    m.op(m.PE, ftr, reads=[qr_b[j], cx.ident_buf], writes=[ptb])
                    qq, qqb = qrT[j], qrT_b[j]
                    m.op(m.ACT, lambda: nc.scalar.copy(out=qq[:, :, :bn],
                                                       in_=pt[:64, :].rearrange("p (h t) -> p h t", h=8)[:, :, :bn]),
                         reads=[ptb], writes=[qqb])
                    m.dma(m.SP, S["QAUX_M"][:, 0:64, r0 + t0: r0 + t0 + bn].rearrange("h r t -> r h t"), qq[:, :, :bn],
                          reads=[qqb], writes=[S["QAUX_M_b"]])
            m.dma(m.SP, S["KAUX_M"][0:64, r0:r0 + n], krT[gi % 2][:, :n], reads=[krT_b[gi % 2]], writes=[S["KAUX_M_b"]])
            fm_heads(m, cx, wukv, w_b, 4, c_t, c_b, n, 8, S["KM"][0:8], r0, S["KM_b"])
            if own:
                fm_heads(m, cx, wuq, w_b, 4, cqT[gi % 2], cqT_b[gi % 2], n, 8, S["QM"][0:8], r0, S["QM_b"], scale=SC_MLA)
        m.barrier()


def kv_pass_fox(m, xkv, w_in, fox_b, kvalid, kind, S):
    nc = m.nc
    with ExitStack() as es:
        cx = Ctx()
        FL = es.enter_context(nc.sbuf_tensor("FL", [8, LFULL], F32))
        FL_b = m.buf()
        esw = ExitStack()
        kv_common_setup(m, cx, esw, "a2")
        win_v = w_in.rearrange("(c p) f -> p c f", p=128)
        wq = esw.enter_context(nc.sbuf_tensor("wqf", [128, DC, 1024], BF16))
        wk = esw.enter_context(nc.sbuf_tensor("wkf", [128, DC, 1024], BF16))
        wv = esw.enter_context(nc.sbuf_tensor("wvf", [128, DC, 1024], BF16))
        wf = esw.enter_context(nc.sbuf_tensor("wff", [128, DC, 8], BF16))
        w_b = m.buf()
        load_w(m, m.POOL, wq, win_v[:, :, 1088:2112], bufs=[w_b])
        load_w(m, m.POOL, wk, win_v[:, :, 2112:3136], bufs=[w_b])
        load_w(m, m.POOL, wv, win_v[:, :, 3136:4160], bufs=[w_b])
        load_w(m, m.POOL, wf, win_v[:, :, 4160:4168], step=16, bufs=[w_b])
        xT = esw.enter_context(nc.sbuf_tensor("xTf", [128, DC, 512], BF16))
        xT_b = m.buf()
        bi_glob = 0
        for gi, (r0, n) in enumerate(KGROUPS):
            own = r0 < NT
            blocks = xgroup_fm(m, cx, xkv, r0, n, xT, xT_b)
            fm_heads(m, cx, wk, w_b, DC, xT, xT_b, n, 8, S["KM"][8:16], r0, S["KM_b"])
            if own:
                fm_heads(m, cx, wq, w_b, DC, xT, xT_b, n, 8, S["QM"][8:16], r0, S["QM_b"], scale=SC_FOX)
            ps, pb = next_pm(cx)
            def f():
                ins = None
                for kc in range(DC):
                    ins = nc.tensor.matmul(ps[:8, :n], lhsT=wf[:, kc, 0:8], rhs=xT[:, kc, :n], start=(kc == 0), stop=(kc == DC - 1))
                return ins
            m.op(m.PE, f, reads=[w_b, xT_b], writes=[pb])
            m.op(m.DVE, lambda: nc.vector.tensor_copy(out=FL[:, r0:r0 + n], in_=ps[:8, :n]), reads=[pb], writes=[FL_b])
            for (t0, bn) in blocks:
                bi_glob += 1
                ps2, pb2 = tm_out(m, cx, xT, xT_b, DC, t0, bn, wv, w_b, 0, 1024)
                ev, evb = next_ev(cx)
                evac(m, cx, bi_glob, ev[:bn, :], ps2[:bn, :], [pb2], [evb])
                m.dma(m.SP, S["VV"][r0 + t0: r0 + t0 + bn, 8:16, :], ev[:bn, :].rearrange("p (h d) -> p h d", h=8),
                      reads=[evb], writes=[S["VV_b"]])
        m.barrier()
        esw.close()
        fb = es.enter_context(nc.sbuf_tensor("fb", [8, 2], F32))
        T1 = es.enter_context(nc.sbuf_tensor("T1", [8, LFULL], F32))
        T2 = es.enter_context(nc.sbuf_tensor("T2", [8, LFULL], F32))
        KV = es.enter_context(nc.sbuf_tensor("KVd", [8, LFULL], BF16))
        KI = es.enter_context(nc.sbuf_tensor("KId", [8, LFULL], BF16))
        B1 = es.enter_context(nc.sbuf_tensor("B1", [8, LFULL], BF16))
        B2 = es.enter_context(nc.sbuf_tensor("B2", [8, LFULL], BF16))
        B3 = es.enter_context(nc.sbuf_tensor("B3", [8, LFULL], BF16))
        tb = m.buf()
        m.dma(m.SP, fb[:, 0:1], fox_b.rearrange("(h o) -> h o", o=1), writes=[tb])
        m.dma(m.POOL, KV[:, :], kvalid.partition_broadcast(8), writes=[tb])
        m.dma(m.POOL, KI[:, :], kind.partition_broadcast(8), writes=[tb])
        m.op(m.DVE, lambda: nc.vector.tensor_scalar(out=fb[:, 1:2], in0=fb[:, 0:1], scalar1=-1.0, scalar2=None, op0=ALU.mult),
             reads=[tb], writes=[tb])
        m.op(m.ACT, lambda: nc.scalar.activation(out=T1[:, :], in_=FL[:, :], func=AF.Exp, bias=fb[:, 1:2], scale=-1.0),
             reads=[FL_b, tb], writes=[tb])
        m.op(m.ACT, lambda: nc.scalar.activation(out=T1[:, :], in_=T1[:, :], func=AF.Ln, bias=1.0, scale=1.0),
             reads=[tb], writes=[tb])
        m.op(m.DVE, lambda: nc.vector.tensor_scalar(out=T1[:, :], in0=T1[:, :], scalar1=-1.0, scalar2=None, op0=ALU.mult),
             reads=[tb], writes=[tb])
        m.op(m.DVE, lambda: nc.vector.tensor_scalar(out=T2[:, NT:], in0=KV[:, NT:], scalar1=1e-30, scalar2=1.0, op0=ALU.mult,
                                                    op1=ALU.add), reads=[tb], writes=[tb])
        m.op(m.DVE, lambda: nc.vector.tensor_tensor(out=T2[:, NT:], in0=T2[:, NT:], in1=T1[:, NT:], op=ALU.mult),
             reads=[tb], writes=[tb])
        m.op(m.DVE, lambda: nc.vector.reduce_sum(out=fb[:, 0:1], in_=T2[:, NT:], axis=AX.X), reads=[tb], writes=[tb])
        m.op(m.DVE, lambda: nc.vector.memset(T2[:, :], 1.0), reads=[tb], writes=[tb])
        m.op(m.DVE, lambda: nc.vector.tensor_tensor_scan(out=FL[:, 0:NT], data0=T2[:, 0:NT], data1=T1[:, 0:NT],
                                                         initial=fb[:, 0:1], op0=ALU.mult, op1=ALU.add),
             reads=[tb], writes=[FL_b])
        m.op(m.DVE, lambda: nc.vector.tensor_tensor_scan(out=FL[:, NT:], data0=T2[:, NT:], data1=T1[:, NT:],
                                                         initial=0.0, op0=ALU.mult, op1=ALU.add),
             reads=[tb], writes=[FL_b])
        m.op(m.DVE, lambda: nc.vector.tensor_copy(out=B1[:, :], in_=FL[:, :]), reads=[FL_b], writes=[tb])
        m.op(m.DVE, lambda: nc.vector.tensor_tensor(out=T1[:, :], in0=FL[:, :], in1=B1[:, :], op=ALU.subtract),
             reads=[tb], writes=[tb])
        m.op(m.DVE, lambda: nc.vector.tensor_copy(out=B2[:, :], in_=T1[:, :]), reads=[tb], writes=[tb])
        m.dma(m.SP, S["QAUX_F"][:, 0, :], B1[:, 0:NT], reads=[tb], writes=[S["QAUX_F_b"]])
        m.dma(m.SP, S["QAUX_F"][:, 1, :], B2[:, 0:NT], reads=[tb], writes=[S["QAUX_F_b"]])
        m.op(m.DVE, lambda: nc.vector.memset(B3[:, :], 1.0), reads=[tb], writes=[tb])
        for r in (2, 3, 4):
            m.dma(m.SP, S["QAUX_F"][:, r, :], B3[:, 0:NT], reads=[tb], writes=[S["QAUX_F_b"]])
        m.op(m.DVE, lambda: nc.vector.tensor_scalar(out=B3[:, :], in0=KI[:, :], scalar1=-1.0, scalar2=None, op0=ALU.add),
             reads=[tb], writes=[tb])
        for r in (0, 1):
            m.dma(m.SP, S["KAUX_F"][:, r, :], B3[:, :], reads=[tb], writes=[S["KAUX_F_b"]])
        m.op(m.DVE, lambda: nc.vector.scalar_tensor_tensor(out=T1[:, :], in0=B1[:, :], scalar=-1.0, in1=KI[:, :], op0=ALU.mult,
                                                           op1=ALU.mult), reads=[tb], writes=[tb])
        m.op(m.DVE, lambda: nc.vector.tensor_copy(out=B1[:, :], in_=T1[:, :]), reads=[tb], writes=[tb])
        m.dma(m.SP, S["KAUX_F"][:, 2, :], B1[:, :], reads=[tb], writes=[S["KAUX_F_b"]])
        m.op(m.DVE, lambda: nc.vector.scalar_tensor_tensor(out=T1[:, :], in0=B2[:, :], scalar=-1.0, in1=KI[:, :], op0=ALU.mult,
                                                           op1=ALU.mult), reads=[tb], writes=[tb])
        m.op(m.DVE, lambda: nc.vector.tensor_copy(out=B2[:, :], in_=T1[:, :]), reads=[tb], writes=[tb])
        m.dma(m.SP, S["KAUX_F"][:, 3, :], B2[:, :], reads=[tb], writes=[S["KAUX_F_b"]])
        m.dma(m.SP, S["KAUX_F"][:, 4, :], KV[:, :], reads=[tb], writes=[S["KAUX_F_b"]])
        m.barrier()


def attention_phase(m, S):
    nc = m.nc
    NKB = 65
    with ExitStack() as es:
        ident = es.enter_context(nc.sbuf_tensor("identat", [128, 128], BF16))
        ident_buf, _ = make_identity(m, ident)
        km = es.enter_context(nc.sbuf_tensor("km", [128, LFULL], BF16))
        ka = [es.enter_context(nc.sbuf_tensor("ka%d" % i, [65, LFULL], BF16)) for i in range(2)]
        vv = es.enter_context(nc.sbuf_tensor("vv", [128, NKB, 128], BF16))
        qm = [es.enter_context(nc.sbuf_tensor("qm%d" % i, [128, NT], BF16)) for i in range(2)]
        qa = [es.enter_context(nc.sbuf_tensor("qa%d" % i, [65, NT], BF16)) for i in range(2)]
        km_b = m.buf()
        ka_b = [m.buf() for _ in range(2)]
        vv_b = m.buf()
        qm_b = [m.buf() for _ in range(2)]
        qa_b = [m.buf() for _ in range(2)]
        Sall = [es.enter_context(nc.sbuf_tensor("Sall%d" % i, [128, LFULL], F32)) for i in range(2)]
        Sall_b = [m.buf() for _ in range(2)]
        Pt = [es.enter_context(nc.sbuf_tensor("Pt%d" % i, [128, LFULL], BF16)) for i in range(2)]
        Pt_b = [m.buf() for _ in range(2)]
        PT = [es.enter_context(nc.sbuf_tensor("PTs%d" % i, [128, 8, 128], BF16)) for i in range(2)]
        PT_b = [m.buf() for _ in range(2)]
        mask = es.enter_context(nc.sbuf_tensor("maskT", [128, 2 * NT], F32))
        mask_b = m.buf()
        def fmask():
            nc.gpsimd.memset(mask[:, :], 0.0)
            return nc.gpsimd.affine_select(out=mask[:, :], in_=mask[:, :], pattern=[[-1, 2 * NT]], compare_op=ALU.is_ge,
                                           fill=NEG, base=NT, channel_multiplier=1)
        m.op(m.POOL, fmask, writes=[mask_b])
        stt = [es.enter_context(nc.sbuf_tensor("stt%d" % i, [128, 16], F32)) for i in range(2)]
        stt_b = [m.buf() for _ in range(2)]
        osb = [es.enter_context(nc.sbuf_tensor("osb%d" % i, [128, 128], BF16)) for i in range(2)]
        osb_b = [m.buf() for _ in range(2)]
        ps_s = [es.enter_context(nc.psum_tensor("ps_s%d" % i, [128, 512], F32)) for i in range(4)]
        ps_s_b = [m.buf() for _ in range(4)]
        ps_t_f = [es.enter_context(nc.psum_tensor("ps_t%d" % i, [128, 512], F32)) for i in range(2)]
        ps_t = [t[:, :].bitcast(BF16) for t in ps_t_f]
        ps_t_b = [m.buf() for _ in range(2)]
        ps_o = [es.enter_context(nc.psum_tensor("ps_o%d" % i, [128, 128], F32)) for i in range(2)]
        ps_o_b = [m.buf() for _ in range(2)]
        qblocks = tok_blocks(NT)
        items = [(h, qb) for h in range(16) for qb in range(len(qblocks))]
        st8 = {"ci": 0, "ti": 0, "mla_aux": False, "kh": -1, "vh": -1}

        def head_aux(h):
            s = h % 2
            mla = h < 8
            if mla:
                return ka[0], ka_b[0], 65, s
            return ka[s], ka_b[s], 5, s

        def load_head_k(h):
            s = h % 2
            mla = h < 8
            m.dma(m.SP, km[:, :], S["KM"][h], reads=[S["KM_b"]], writes=[km_b])
            m.dma(m.SP, qm[s][:, :], S["QM"][h], reads=[S["QM_b"]], writes=[qm_b[s]])
            if mla:
                if not st8["mla_aux"]:
                    m.dma(m.SP, ka[0][:, :], S["KAUX_M"][:, :], reads=[S["KAUX_M_b"]], writes=[ka_b[0]])
                    st8["mla_aux"] = True
                m.dma(m.SP, qa[s][:65, :], S["QAUX_M"][h], reads=[S["QAUX_M_b"]], writes=[qa_b[s]])
            else:
                m.dma(m.SP, ka[s][:5, :], S["KAUX_F"][h - 8], reads=[S["KAUX_F_b"]], writes=[ka_b[s]])
                m.dma(m.SP, qa[s][:5, :], S["QAUX_F"][h - 8], reads=[S["QAUX_F_b"]], writes=[qa_b[s]])

        def load_head_v(h):
            m.dma(m.SP, vv[:16, 0, :], S["VV"][0:16, h, :], reads=[S["VV_b"]], writes=[vv_b])
            for b0 in range(0, 64, 16):
                m.dma(m.SP, vv[:, 1 + b0: 1 + b0 + 16, :],
                      S["VV"][16 + b0 * 128: 16 + (b0 + 16) * 128, h, :].rearrange("(b p) d -> p b d", p=128),
                      reads=[S["VV_b"]], writes=[vv_b])

        def pass1(idx):
            h, qb = items[idx]
            q0, n = qblocks[qb]
            if st8["kh"] != h:
                load_head_k(h)
                st8["kh"] = h
            ka_t, ka_tb, ra, s = head_aux(h)
            sa, sab = Sall[idx % 2], Sall_b[idx % 2]
            nk_own = q0 + n
            chunks = []
            c0 = 0
            while c0 < nk_own:
                ln = min(512, nk_own - c0)
                chunks.append((c0, ln, c0, True))
                c0 += ln
            for j in range(14):
                chunks.append((NT + 512 * j, 512, nk_own + 512 * j, False))
            for (k0, ln, d0, is_own) in chunks:
                p = st8["ci"] % 4
                st8["ci"] += 1
                def fqk():
                    nc.tensor.matmul(ps_s[p][:n, :ln], lhsT=qm[s][:, q0:q0 + n], rhs=km[:, k0:k0 + ln], start=True, stop=False)
                    return nc.tensor.matmul(ps_s[p][:n, :ln], lhsT=qa[s][:ra, q0:q0 + n], rhs=ka_t[:ra, k0:k0 + ln],
                                            start=False, stop=True)
                m.op(m.PE, fqk, reads=[qm_b[s], km_b, qa_b[s], ka_tb], writes=[ps_s_b[p]])
                if is_own:
                    mo = (NT - q0) + k0
                    m.op(m.DVE, lambda: nc.vector.tensor_tensor(out=sa[:n, d0:d0 + ln], in0=ps_s[p][:n, :ln],
                                                                in1=mask[:n, mo:mo + ln], op=ALU.add),
                         reads=[ps_s_b[p], mask_b], writes=[sab])
                elif st8["ci"] % 2 == 0:
                    m.op(m.DVE, lambda: nc.vector.tensor_copy(out=sa[:n, d0:d0 + ln], in_=ps_s[p][:n, :ln]),
                         reads=[ps_s_b[p]], writes=[sab])
                else:
                    m.op(m.ACT, lambda: nc.scalar.copy(out=sa[:n, d0:d0 + ln], in_=ps_s[p][:n, :ln]),
                         reads=[ps_s_b[p]], writes=[sab])

        def pass2(idx):
            h, qb = items[idx]
            q0, n = qblocks[qb]
            nk_own = q0 + n
            slen = nk_own + NOTH
            sti = idx % 2
            sa, sab = Sall[sti], Sall_b[sti]
            st, stb = stt[sti], stt_b[sti]
            pt_, ptb_ = Pt[sti], Pt_b[sti]
            m.op(m.DVE, lambda: nc.vector.reduce_max(out=st[:n, 0:1], in_=sa[:n, 0:slen], axis=AX.X),
                 reads=[sab], writes=[stb])
            m.op(m.DVE, lambda: nc.vector.tensor_scalar(out=st[:n, 1:2], in0=st[:n, 0:1], scalar1=-1.0, scalar2=None,
                                                        op0=ALU.mult), reads=[stb], writes=[stb])
            npieces = 4
            pl = -(-slen // npieces)
            for pi in range(npieces):
                a_ = pi * pl
                b_ = min(slen, a_ + pl)
                m.op(m.ACT, lambda: nc.scalar.activation(out=pt_[:n, a_:b_], in_=sa[:n, a_:b_], func=AF.Exp,
                                                         bias=st[:n, 1:2], scale=1.0, accum_out=st[:n, 4 + pi:5 + pi]),
                     reads=[sab, stb], writes=[ptb_, stb])
            m.op(m.DVE, lambda: nc.vector.reduce_sum(out=st[:n, 2:3], in_=st[:n, 4:4 + npieces], axis=AX.X),
                 reads=[stb], writes=[stb])
            m.op(m.DVE, lambda: nc.vector.reciprocal(out=st[:n, 3:4], in_=st[:n, 2:3]), reads=[stb], writes=[stb])
            if st8["vh"] != h:
                load_head_v(h)
                st8["vh"] = h
            kblocks = [(0, 16, 0)] + [(16 + 128 * (j - 1), 128, j) for j in range(1, qb + 1)]
            for j in range(56):
                kblocks.append((nk_own + 128 * j, 128, 9 + j))
            po, pob = ps_o[sti], ps_o_b[sti]
            nkb = len(kblocks)
            first_pv = True
            for g0 in range(0, nkb, 8):
                grp = kblocks[g0:g0 + 8]
                tp = st8["ti"] % 2
                st8["ti"] += 1
                def ftr():
                    ins = None
                    for jj, (pc, kl, vb) in enumerate(grp):
                        ins = nc.tensor.transpose(ps_t[tp][:kl, jj * 128: jj * 128 + n], pt_[:n, pc:pc + kl], ident[:n, :n])
                    return ins
                m.op(m.PE, ftr, reads=[ptb_, ident_buf], writes=[ps_t_b[tp]])
                kl0 = grp[0][1]
                ng = len(grp)
                if kl0 == 16:
                    m.op(m.DVE, lambda: nc.vector.tensor_copy(out=PT[tp][:16, 0, :n], in_=ps_t[tp][:16, 0:n]),
                         reads=[ps_t_b[tp]], writes=[PT_b[tp]])
                    if ng > 1:
                        m.op(m.DVE, lambda: nc.vector.tensor_copy(
                            out=PT[tp][:, 1:ng, :n],
                            in_=ps_t[tp][:, 128:ng * 128].rearrange("p (j t) -> p j t", j=ng - 1)[:, :, :n]),
                            reads=[ps_t_b[tp]], writes=[PT_b[tp]])
                elif tp == 0:
                    m.op(m.DVE, lambda: nc.vector.tensor_copy(
                        out=PT[tp][:, 0:ng, :n], in_=ps_t[tp][:, 0:ng * 128].rearrange("p (j t) -> p j t", j=ng)[:, :, :n]),
                        reads=[ps_t_b[tp]], writes=[PT_b[tp]])
                else:
                    m.op(m.ACT, lambda: nc.scalar.copy(
                        out=PT[tp][:, 0:ng, :n], in_=ps_t[tp][:, 0:ng * 128].rearrange("p (j t) -> p j t", j=ng)[:, :, :n]),
                        reads=[ps_t_b[tp]], writes=[PT_b[tp]])
                last_grp = (g0 + 8 >= nkb)
                def fpv():
                    ins = None
                    for jj, (pc, kl, vb) in enumerate(grp):
                        ins = nc.tensor.matmul(po[:n, :], lhsT=PT[tp][:kl, jj, :n], rhs=vv[:kl, vb, :],
                                               start=(first_pv and jj == 0), stop=(last_grp and jj == ng - 1))
                    return ins
                m.op(m.PE, fpv, reads=[PT_b[tp], vv_b], writes=[pob])
                first_pv = False
            ob, obb = osb[sti], osb_b[sti]
            m.op(m.ACT, lambda: nc.scalar.mul(out=ob[:n, :], in_=po[:n, :], mul=st[:n, 3:4]), reads=[pob, stb], writes=[obb])
            m.dma(m.SP, S["AO"][q0:q0 + n, h * 128:(h + 1) * 128], ob[:n, :], reads=[obb], writes=[S["AO_b"]])

        pass1(0)
        for idx in range(len(items)):
            if idx + 1 < len(items):
                pass1(idx + 1)
            pass2(idx)
        m.barrier()


def outproj_phase(m, tag, A_dram, A_buf, a_is_bf16, ntok, arow0, w_out, Hin, Hin_buf, hrow0, g_dram, b_dram, Hout, Hout_buf, orow0,
                  pre=None):
    nc = m.nc
    with ExitStack() as es:
        cx = Ctx()
        blocks = tok_blocks(ntok)
        aT = es.enter_context(nc.sbuf_tensor("aT" + tag, [128, DC, ntok], BF16))
        aT_b = m.buf()
        ident = es.enter_context(nc.sbuf_tensor("ident" + tag, [128, 128], BF16))
        ident_buf, _ = make_identity(m, ident)
        wo = es.enter_context(nc.sbuf_tensor("wo" + tag, [128, DC, D], BF16))
        wo_b = m.buf()
        load_w(m, m.POOL, wo, w_out.rearrange("(c p) f -> p c f", p=128), step=2, bufs=[wo_b])
        py = [es.enter_context(nc.psum_tensor("py%s%d" % (tag, i), [128, 2048], F32)) for i in range(1)]
        py_b = [m.buf() for _ in range(1)]
        pt_f = [es.enter_context(nc.psum_tensor("pt%s%d" % (tag, i), [128, 512], F32)) for i in range(2)]
        pt_b = [m.buf() for _ in range(2)]
        with ExitStack() as es2:
            stage = [es2.enter_context(nc.sbuf_tensor("stg%s%d" % (tag, i), [128, D], BF16)) for i in range(2)]
            stage_b = [m.buf() for _ in range(2)]
            pst = [t[:, :].bitcast(BF16) for t in pt_f]
            load_fm(m, A_dram, arow0, ntok, aT, aT_b, ident, ident_buf, stage, stage_b, pst, pt_b, Hbuf=A_buf,
                    q=(m.SP if a_is_bf16 else m.POOL))
            m.barrier()
        ln_setup(m, cx, es, tag, g_dram, b_dram, [Hin_buf], Hout_buf)
        for bi, (t0, n) in enumerate(blocks):
            def f():
                ins = None
                for q in range(4):
                    for c in range(DC):
                        ins = nc.tensor.matmul(py[0][:n, q * 512:(q + 1) * 512], lhsT=aT[:, c, t0:t0 + n],
                                               rhs=wo[:, c, q * 512:(q + 1) * 512], start=(c == 0), stop=(c == DC - 1))
                return ins
            m.op(m.PE, f, reads=[aT_b, wo_b], writes=[py_b[0]])
            layer_norm_block(m, cx, py[0][:n, :], [py_b[0]], Hin, hrow0 + t0, n, Hout, cx.gam, cx.bet, out_row=orow0 + t0)
        m.barrier()


def make_scratch(m):
    nc = m.nc
    S = {}
    def mk(name, shape, dt):
        S[name] = nc.dram_tensor("scr_" + name, shape, dt).ap()
        S[name + "_b"] = m.buf()
    mk("KM", [16, 128, LFULL], BF16)
    mk("KAUX_M", [65, LFULL], BF16)
    mk("KAUX_F", [8, 5, LFULL], BF16)
    mk("VV", [LFULL, 16, 128], BF16)
    mk("QM", [16, 128, NT], BF16)
    mk("QAUX_M", [8, 65, NT], BF16)
    mk("QAUX_F", [8, 5, NT], BF16)
    mk("AO", [NT, D], BF16)
    mk("H1", [NT, D], F32)
    mk("H2", [NT, D], F32)
    mk("H3", [NOWN, D], F32)
    mk("PM", [NOWN, D], BF16)
    return S


def persist(m, S):
    for k, v in S.items():
        if k.endswith("_b"):
            m.bufs.append(v)


def layer0_mixer(m, I, S):
    kv_pass_mla(m, I["xkv"], I["cs_all"], I["attn_w_in"], I["w_ukv_p"], I["w_uq_p"], I["mla_kv_norm"], I["mla_q_norm"],
                I["kvalid"], S)
    persist(m, S)
    kv_pass_fox(m, I["xkv"], I["attn_w_in"], I["fox_forget_bias"], I["kvalid"], I["kind"], S)
    persist(m, S)
    attention_phase(m, S)
    persist(m, S)
    xb = m.buf()
    outproj_phase(m, "op0", S["AO"], S["AO_b"], True, NT, 0, I["attn_w_out"], I["xkv"], xb, 0, I["ln_mix_g0"], I["ln_mix_b0"],
                  S["H1"], S["H1_b"], 0)
    persist(m, S)


def declare_inputs(nc, names_shapes):
    I = {}
    for name, shape in names_shapes:
        I[name] = nc.dram_tensor(name, list(shape), F32, kind="ExternalInput").ap()
    return I


L0_INPUTS = [("xkv", (LFULL, D)), ("cs_all", (LFULL, 64)), ("attn_w_in", (D, 4168)), ("w_ukv_p", (512, 2048)),
             ("w_uq_p", (512, 1536)), ("mla_kv_norm", (512,)), ("mla_q_norm", (512,)), ("kvalid", (LFULL,)),
             ("kind", (LFULL,)), ("fox_forget_bias", (8,)), ("attn_w_out", (D, D)), ("ln_mix_g0", (D,)), ("ln_mix_b0", (D,))]


def host_prep_l0(inp):
    x = np.asarray(inp["x"], np.float32)[0]
    hfull = np.concatenate([np.asarray(inp["meta_tokens"], np.float32), x], axis=0)
    pos = np.arange(LFULL, dtype=np.float32)
    inv_freq = (10000.0 ** (-np.arange(0, 64, 2, dtype=np.float32) / 64)).astype(np.float32)
    ang = pos[:, None] * inv_freq[None, :]
    cs = np.concatenate([np.cos(ang), np.sin(ang)], axis=1).astype(np.float32)
    w_ukv = np.asarray(inp["mla_w_ukv"], np.float32)[0].reshape(512, 8, 256)
    w_ukv_p = np.ascontiguousarray(np.concatenate([w_ukv[:, :, :128].reshape(512, 1024), w_ukv[:, :, 128:].reshape(512, 1024)], 1))
    w_uq = np.asarray(inp["mla_w_uq"], np.float32)[0].reshape(512, 8, 192)
    w_uq_p = np.ascontiguousarray(np.concatenate([w_uq[:, :, :128].reshape(512, 1024), w_uq[:, :, 128:].reshape(512, 512)], 1))
    common = {
        "attn_w_in": np.ascontiguousarray(np.asarray(inp["attn_w_in"], np.float32)[0]),
        "w_ukv_p": w_ukv_p, "w_uq_p": w_uq_p,
        "mla_kv_norm": np.asarray(inp["mla_kv_norm"], np.float32)[0], "mla_q_norm": np.asarray(inp["mla_q_norm"], np.float32)[0],
        "fox_forget_bias": np.asarray(inp["fox_forget_bias"], np.float32)[0],
        "attn_w_out": np.ascontiguousarray(np.asarray(inp["attn_w_out"], np.float32)[0]),
        "ln_mix_g0": np.asarray(inp["ln_mix_g"], np.float32)[0], "ln_mix_b0": np.asarray(inp["ln_mix_b"], np.float32)[0],
    }
    maps = []
    for c in range(NCORE):
        own = np.arange(1024 * c, 1024 * c + NT)
        oth = np.concatenate([np.arange(0, 1024 * c), np.arange(1024 * c + NT, LFULL)])
        perm = np.concatenate([own, oth])
        d = dict(common)
        d["xkv"] = np.ascontiguousarray(hfull[perm])
        d["cs_all"] = np.ascontiguousarray(cs[perm])
        kvalid = np.zeros(LFULL, np.float32)
        kvalid[NT:] = np.where(oth < 1024 * c, 0.0, NEG)
        d["kvalid"] = kvalid
        d["kind"] = (perm >= 16).astype(np.float32)
        maps.append(d)
    return maps


def pool_phase(m, I, S):
    nc = m.nc
    H2, H2_b = S["H2"], S["H2_b"]
    groups_all = tok_groups(NT)
    with ExitStack() as es:
        dT = es.enter_context(nc.sbuf_tensor("dTp", [128, DC, NOWN], BF16))
        dT_b = m.buf()
        with ExitStack() as es1:
            hT = es1.enter_context(nc.sbuf_tensor("hTp", [128, DC, NT], BF16))
            hT_b = m.buf()
            ident = es1.enter_context(nc.sbuf_tensor("identp", [128, 128], BF16))
            ident_buf, _ = make_identity(m, ident)
            wpi = es1.enter_context(nc.sbuf_tensor("wpi", [128, DC, D], BF16))
            wpi_b = m.buf()
            load_w(m, m.POOL, wpi, I["pool_w_in"].rearrange("(c p) f -> p c f", p=128), step=2, bufs=[wpi_b])
            pT = es1.enter_context(nc.sbuf_tensor("pTp", [128, 4, NT], F32))
            A = es1.enter_context(nc.sbuf_tensor("Ap", [128, 4, NT], F32))
            B = es1.enter_context(nc.sbuf_tensor("Bp", [128, 4, NT], F32))
            pT_b, A_b, B_b = m.buf(), m.buf(), m.buf()
            pp = [es1.enter_context(nc.psum_tensor("ppp%d" % i, [128, 512], F32)) for i in range(4)]
            pp_b = [m.buf() for _ in range(4)]
            with ExitStack() as es2:
                stage = [es2.enter_context(nc.sbuf_tensor("stgp%d" % i, [128, D], BF16)) for i in range(2)]
                stage_b = [m.buf() for _ in range(2)]
                pst = [pp[i][:, :].bitcast(BF16) for i in range(2)]
                load_fm(m, H2, 0, NT, hT, hT_b, ident, ident_buf, stage, stage_b, pst, pp_b[0:2], Hbuf=H2_b)
                m.barrier()
            k = 0
            for g, w in enumerate((2, 4, 8, 16)):
                for oc in range(4):
                    ch = 4 * g + oc
                    for (t0, n) in groups_all:
                        p = k % 4
                        k += 1
                        def f():
                            ins = None
                            for c in range(DC):
                                ins = nc.tensor.matmul(pp[p][:, :n], lhsT=wpi[:, c, ch * 128:(ch + 1) * 128], rhs=hT[:, c, t0:t0 + n],
                                                       start=(c == 0), stop=(c == DC - 1))
                            return ins
                        m.op(m.PE, f, reads=[wpi_b, hT_b], writes=[pp_b[p]])
                        evac(m, None, k, pT[:, oc, t0:t0 + n], pp[p][:, :n], [pp_b[p]], [pT_b])
                cur, cur_b = pT, pT_b
                dst = [(A, A_b), (B, B_b)]
                sh = 1
                lvl = 0
                while sh < w:
                    o, o_b = dst[lvl % 2]
                    lo = 2 * sh - 1
                    src, src_b = cur, cur_b
                    m.op(m.DVE, lambda: nc.vector.tensor_tensor(out=o[:, :, lo:NT], in0=src[:, :, lo:NT], in1=src[:, :, lo - sh:NT - sh],
                                                                op=ALU.add), reads=[src_b], writes=[o_b])
                    cur, cur_b = o, o_b
                    sh *= 2
                    lvl += 1
                sw, sw_b = cur, cur_b
                m.op(m.DVE, lambda: nc.vector.scalar_tensor_tensor(out=dT[:, 4 * g:4 * g + 4, :], in0=sw[:, :, NHALO:NT], scalar=1.0 / w,
                                                                   in1=pT[:, :, NHALO:NT], op0=ALU.mult, op1=ALU.subtract),
                     reads=[sw_b, pT_b], writes=[dT_b])
            m.barrier()
        wgr = es.enter_context(nc.sbuf_tensor("wgr", [128, 16, 512], BF16))
        wgr_b = m.buf()
        load_w(m, m.POOL, wgr, I["pool_w_group"].rearrange("g (c p) f -> p (g c) f", p=128), bufs=[wgr_b])
        scl = es.enter_context(nc.sbuf_tensor("sclp", [128, D], F32))
        scl_b = m.buf()
        m.dma(m.SP, scl[:, :], I["pool_scale"].partition_broadcast(128), writes=[scl_b])
        ysb = [es.enter_context(nc.sbuf_tensor("ysbp%d" % i, [128, D], BF16)) for i in range(2)]
        ysb_b = [m.buf() for _ in range(2)]
        pq = [es.enter_context(nc.psum_tensor("pqp%d" % i, [128, 512], F32)) for i in range(4)]
        pq_b = [m.buf() for _ in range(4)]
        k = 0
        for bi, (t0, n) in enumerate(tok_blocks(NOWN)):
            y, y_b = ysb[bi % 2], ysb_b[bi % 2]
            for g in range(4):
                p = k % 4
                k += 1
                def f():
                    ins = None
                    for kc in range(4):
                        ins = nc.tensor.matmul(pq[p][:n, :], lhsT=dT[:, 4 * g + kc, t0:t0 + n], rhs=wgr[:, 4 * g + kc, :],
                                               start=(kc == 0), stop=(kc == 3))
                    return ins
                m.op(m.PE, f, reads=[dT_b, wgr_b], writes=[pq_b[p]])
                m.op(m.DVE, lambda: nc.vector.tensor_tensor(out=y[:n, g * 512:(g + 1) * 512], in0=pq[p][:n, :],
                                                            in1=scl[:n, g * 512:(g + 1) * 512], op=ALU.mult),
                     reads=[pq_b[p], scl_b], writes=[y_b])
            m.dma(m.SP, S["PM"][t0:t0 + n, :], y[:n, :], reads=[y_b], writes=[S["PM_b"]])
        m.barrier()
    persist(m, S)
    outproj_phase(m, "op1", S["PM"], S["PM_b"], True, NOWN, 0, I["pool_w_out"], S["H2"], S["H2_b"], NHALO,
                  I["ln_mix_g1"], I["ln_mix_b1"], S["H3"], S["H3_b"], 0)
    persist(m, S)


def make_moe_gates(m, I, S):
    def gates(cx, es, hT_unused, hT_buf_unused, blocks):
        nc = m.nc
        nb = len(blocks)
        gate_tile = cx.gate_tile
        cx.gate_buf = m.buf()
        with ExitStack() as e3:
            identf = e3.enter_context(nc.sbuf_tensor("identf", [128, 128], F32))
            idb = m.buf()
            def fi():
                nc.gpsimd.memset(identf[:], 1.0)
                return nc.gpsimd.affine_select(out=identf[:], in_=identf[:], pattern=[[-1, 128]], compare_op=ALU.is_equal,
                                               fill=0.0, base=0, channel_multiplier=1)
            m.op(m.POOL, fi, writes=[idb])
            wr = e3.enter_context(nc.sbuf_tensor("wrt", [128, DC, NEXP], F32))
            br = e3.enter_context(nc.sbuf_tensor("brt", [128, NEXP], F32))
            wr_b = m.buf()
            m.dma(m.SP, wr[:, :, :], I["moe_w_router"].rearrange("(c p) e -> p c e", p=128), writes=[wr_b])
            m.dma(m.SP, br[:, :], I["moe_b_router"].partition_broadcast(128), writes=[wr_b])
            hr = [e3.enter_context(nc.sbuf_tensor("hrt%d" % i, [128, D], F32)) for i in range(2)]
            hr_b = [m.buf() for _ in range(2)]
            hTf = e3.enter_context(nc.sbuf_tensor("hTft", [128, DC, 128], F32))
            hTf_b = m.buf()
            gsm = e3.enter_context(nc.sbuf_tensor("gsm", [128, 64], F32))
            gsm_b = m.buf()
            ptf, ptf_b = cx.pg, cx.pg_b
            plg, plg_b = cx.pu[0], cx.pu_b[0]
            k = 0
            for bi, (t0, n) in enumerate(blocks):
                h, hb = hr[bi % 2], hr_b[bi % 2]
                m.dma(m.SP, h[:n, :], S["H3"][t0:t0 + n, :], reads=[S["H3_b"]], writes=[hb])
                for q in range(4):
                    p = k % 2
                    k += 1
                    def f():
                        ins = None
                        for j in range(4):
                            c = q * 4 + j
                            ins = nc.tensor.transpose(ptf[p][:, j * 128: j * 128 + n], h[:n, c * 128:(c + 1) * 128], identf[:n, :n])
                        return ins
                    m.op(m.PE, f, reads=[hb, idb], writes=[ptf_b[p]])
                    m.op(m.DVE, lambda: nc.vector.tensor_copy(out=hTf[:, q * 4:(q + 1) * 4, :n],
                                                              in_=ptf[p][:, :].rearrange("p (j t) -> p j t", j=4)[:, :, :n]),
                         reads=[ptf_b[p]], writes=[hTf_b])
                def fl():
                    ins = None
                    for c in range(DC):
                        ins = nc.tensor.matmul(plg[:n, 0:NEXP], lhsT=hTf[:, c, :n], rhs=wr[:, c, :], start=(c == 0), stop=(c == DC - 1))
                    return ins
                m.op(m.PE, fl, reads=[hTf_b, wr_b], writes=[plg_b])
                lg, eq1, lg2, eq2 = gsm[:n, 0:8], gsm[:n, 8:16], gsm[:n, 16:24], gsm[:n, 24:32]
                m1, m2, dd, g1, g2 = gsm[:n, 32:33], gsm[:n, 33:34], gsm[:n, 34:35], gsm[:n, 35:36], gsm[:n, 36:37]
                G = gate_tile[:n, bi, :]
                m.op(m.DVE, lambda: nc.vector.tensor_tensor(out=lg, in0=plg[:n, 0:NEXP], in1=br[:n, :], op=ALU.add),
                     reads=[plg_b, wr_b], writes=[gsm_b])
                m.op(m.DVE, lambda: nc.vector.reduce_max(out=m1, in_=lg, axis=AX.X), reads=[gsm_b], writes=[gsm_b])
                m.op(m.DVE, lambda: nc.vector.tensor_scalar(out=eq1, in0=lg, scalar1=m1, scalar2=None, op0=ALU.is_equal),
                     reads=[gsm_b], writes=[gsm_b])
                m.op(m.DVE, lambda: nc.vector.scalar_tensor_tensor(out=lg2, in0=eq1, scalar=NEG, in1=lg, op0=ALU.mult, op1=ALU.add),
                     reads=[gsm_b], writes=[gsm_b])
                m.op(m.DVE, lambda: nc.vector.reduce_max(out=m2, in_=lg2, axis=AX.X), reads=[gsm_b], writes=[gsm_b])
                m.op(m.DVE, lambda: nc.vector.tensor_scalar(out=eq2, in0=lg2, scalar1=m2, scalar2=None, op0=ALU.is_equal),
                     reads=[gsm_b], writes=[gsm_b])
                if getattr(cx, "sel_tile", None) is not None:
                    SEL = cx.sel_tile[:n, bi, :]
                    m.op(m.DVE, lambda: nc.vector.tensor_tensor(out=SEL, in0=eq1, in1=eq2, op=ALU.add), reads=[gsm_b],
                         writes=[cx.gate_buf])
                m.op(m.DVE, lambda: nc.vector.tensor_tensor(out=dd, in0=m1, in1=m2, op=ALU.subtract), reads=[gsm_b], writes=[gsm_b])
                m.op(m.ACT, lambda: nc.scalar.activation(out=g1, in_=dd, func=AF.Sigmoid), reads=[gsm_b], writes=[gsm_b])
                m.op(m.DVE, lambda: nc.vector.tensor_scalar(out=g2, in0=g1, scalar1=-1.0, scalar2=1.0, op0=ALU.mult, op1=ALU.add),
                     reads=[gsm_b], writes=[gsm_b])
                m.op(m.DVE, lambda: nc.vector.tensor_scalar(out=G, in0=eq1, scalar1=g1, scalar2=None, op0=ALU.mult),
                     reads=[gsm_b], writes=[cx.gate_buf])
                m.op(m.DVE, lambda: nc.vector.scalar_tensor_tensor(out=G, in0=eq2, scalar=g2, in1=G, op0=ALU.mult, op1=ALU.add),
                     reads=[gsm_b], writes=[cx.gate_buf])
            m.barrier()
        m.bufs.append(cx.gate_buf)
        return gate_tile
    return gates


I32 = mybir.dt.int32
USE_COMPACT_MOE = True
MOE_CAP = 384


def moe_phase(m, I, S, out, out_b):
    nc = m.nc
    C0 = MOE_CAP
    NJB = C0 // 128
    FC = 256
    F = FFN_EXPERT
    nfc = F // FC
    NB = NOWN // 128
    tag = "mo"
    H3, H3_b = S["H3"], S["H3_b"]
    with ExitStack() as es:
        cx = Ctx()
        blocks = tok_blocks(NOWN)
        ident = es.enter_context(nc.sbuf_tensor("identmo", [128, 128], BF16))
        ident_buf, _ = make_identity(m, ident)
        yacc = es.enter_context(nc.sbuf_tensor("yaccmo", [128, NB, D], F32))
        yacc_bufs = [m.buf() for _ in range(NB)]
        gate_tile = es.enter_context(nc.sbuf_tensor("gatemo", [128, NB, NEXP], F32))
        sel_tile = es.enter_context(nc.sbuf_tensor("selmo", [128, NB, NEXP], F32))
        pos_tile = es.enter_context(nc.sbuf_tensor("posmo", [128, NB, NEXP], F32))
        selb = es.enter_context(nc.sbuf_tensor("selbmo", [128, NB, NEXP], BF16))
        iota_f = es.enter_context(nc.sbuf_tensor("iotaf", [128, C0], F32))
        ones_m = es.enter_context(nc.sbuf_tensor("onesmo", [128, 128], BF16))
        ustr = es.enter_context(nc.sbuf_tensor("ustrmo", [128, 128], BF16))
        cst_b = m.buf()
        m.dma(m.SP, iota_f[:, :], I["slot_iota"].partition_broadcast(128), writes=[cst_b])
        def fu():
            nc.gpsimd.memset(ones_m[:, :], 1.0)
            nc.gpsimd.memset(ustr[:, :], 1.0)
            return nc.gpsimd.affine_select(out=ustr[:, :], in_=ustr[:, :], pattern=[[1, 128]], compare_op=ALU.is_gt,
                                           fill=0.0, base=0, channel_multiplier=-1)
        m.op(m.POOL, fu, writes=[cst_b])
        pg = [es.enter_context(nc.psum_tensor("pgmo%d" % i, [128, 512], F32)) for i in range(2)]
        pu = [es.enter_context(nc.psum_tensor("pumo%d" % i, [128, 512], F32)) for i in range(2)]
        py = [es.enter_context(nc.psum_tensor("pymo%d" % i, [128, 1024], F32)) for i in range(2)]
        pg_b = [m.buf() for _ in range(2)]
        pu_b = [m.buf() for _ in range(2)]
        py_b = [m.buf() for _ in range(2)]
        cx.gate_tile, cx.sel_tile = gate_tile, sel_tile
        cx.pg, cx.pg_b, cx.pu, cx.pu_b = pg, pg_b, pu, pu_b
        make_moe_gates(m, I, S)(cx, es, None, None, blocks)
        gb = cx.gate_buf
        m.op(m.DVE, lambda: nc.vector.tensor_copy(out=selb[:, :, :], in_=sel_tile[:, :, :]), reads=[gb], writes=[gb])
        def fpos():
            ins = None
            for b in range(NB):
                for b2 in range(b):
                    nc.tensor.matmul(pu[0][:, b * 8:(b + 1) * 8], lhsT=ones_m[:, :], rhs=selb[:, b2, :], start=(b2 == 0), stop=False)
                ins = nc.tensor.matmul(pu[0][:, b * 8:(b + 1) * 8], lhsT=ustr[:, :], rhs=selb[:, b, :], start=(b == 0), stop=True)
            return ins
        m.op(m.PE, fpos, reads=[gb, cst_b], writes=[pu_b[0]])
        m.op(m.DVE, lambda: nc.vector.tensor_copy(out=pos_tile[:, :, :],
                                                  in_=pu[0][:, 0:NB * NEXP].rearrange("p (b e) -> p b e", b=NB)),
             reads=[pu_b[0]], writes=[gb])
        esw = ExitStack()
        Pe = esw.enter_context(nc.sbuf_tensor("Pemo", [128, NB, C0], BF16))
        PTe = esw.enter_context(nc.sbuf_tensor("PTemo", [128, NJB, NOWN], BF16))
        xgT = esw.enter_context(nc.sbuf_tensor("xgTmo", [128, DC, C0], BF16))
        yea = esw.enter_context(nc.sbuf_tensor("yeamo", [128, NJB, D], F32))
        yeb = esw.enter_context(nc.sbuf_tensor("yebmo", [128, NJB, D], BF16))
        xst = [esw.enter_context(nc.sbuf_tensor("xstmo%d" % i, [128, D], BF16)) for i in range(2)]
        wgt = [esw.enter_context(nc.sbuf_tensor("wgmo%d" % i, [128, DC, FC], BF16)) for i in range(2)]
        wut = [esw.enter_context(nc.sbuf_tensor("wumo%d" % i, [128, DC, FC], BF16)) for i in range(2)]
        wdt = [esw.enter_context(nc.sbuf_tensor("wdmo%d" % i, [128, FC // 128, D], BF16)) for i in range(2)]
        hid = [esw.enter_context(nc.sbuf_tensor("hidmo%d" % i, [128, FC // 128, C0], BF16)) for i in range(2)]
        gs = [esw.enter_context(nc.sbuf_tensor("gsmo%d" % i, [128, C0], F32)) for i in range(2)]
        Pe_b, PTe_b, xgT_b, yeb_b = m.buf(), m.buf(), m.buf(), m.buf()
        yea_b = [m.buf() for _ in range(NJB)]
        xst_b = [m.buf() for _ in range(2)]
        wg_b = [m.buf() for _ in range(2)]
        wu_b = [m.buf() for _ in range(2)]
        wd_b = [m.buf() for _ in range(2)]
        hid_b = [m.buf() for _ in range(2)]
        gs_b = [m.buf() for _ in range(2)]
        pgt = [pg[i][:, :].bitcast(BF16) for i in range(2)]
        acc = [(pg[0], pg_b[0], 0), (pg[1], pg_b[1], 0), (pu[0], pu_b[0], 0), (pu[1], pu_b[1], 0),
               (py[0], py_b[0], 0), (py[0], py_b[0], 512), (py[1], py_b[1], 0), (py[1], py_b[1], 512)]
        kk = 0
        gi = 0
        xi = 0
        ti = 0
        for e in range(NEXP):
            for b in range(NB):
                m.op(m.DVE, lambda: nc.vector.tensor_scalar(out=Pe[:, b, :], in0=iota_f[:, :], scalar1=pos_tile[:, b, e:e + 1],
                                                            scalar2=sel_tile[:, b, e:e + 1], op0=ALU.is_equal, op1=ALU.mult),
                     reads=[gb, cst_b], writes=[Pe_b])
            for b in range(NB):
                tp = ti % 2
                ti += 1
                def ftr():
                    ins = None
                    for jb in range(NJB):
                        ins = nc.tensor.transpose(pgt[tp][:, jb * 128:(jb + 1) * 128], Pe[:, b, jb * 128:(jb + 1) * 128], ident[:, :])
                    return ins
                m.op(m.PE, ftr, reads=[Pe_b, ident_buf], writes=[pg_b[tp]])
                m.op(m.ACT, lambda: nc.scalar.copy(out=PTe[:, :, b * 128:(b + 1) * 128],
                                                   in_=pgt[tp][:, 0:NJB * 128].rearrange("p (j t) -> p j t", j=NJB)),
                     reads=[pg_b[tp]], writes=[PTe_b])
            for half in range(2):
                for b in range(NB):
                    xs, xsb = xst[xi % 2], xst_b[xi % 2]
                    xi += 1
                    m.dma(m.POOL, xs[:, :], H3[b * 128:(b + 1) * 128, :], reads=[H3_b], writes=[xsb])
                    def fg():
                        ins = None
                        for c8 in range(8):
                            c = half * 8 + c8
                            pt_, ptb_, off = acc[c8]
                            ins = nc.tensor.matmul(pt_[:, off:off + C0], lhsT=xs[:, c * 128:(c + 1) * 128], rhs=Pe[:, b, :],
                                                   start=(b == 0), stop=(b == NB - 1))
                        return ins
                    m.op(m.PE, fg, reads=[xsb, Pe_b], writes=[pg_b[0], pg_b[1], pu_b[0], pu_b[1], py_b[0], py_b[1]])
                for c8 in range(8):
                    c = half * 8 + c8
                    pt_, ptb_, off = acc[c8]
                    evac(m, None, c8, xgT[:, c, :], pt_[:, off:off + C0], [ptb_], [xgT_b])
            wg_v = I["moe_w_gate"][e].rearrange("(c p) f -> p c f", p=128)
            wu_v = I["moe_w_up"][e].rearrange("(c p) f -> p c f", p=128)
            wd_v = I["moe_w_down"][e].rearrange("(j p) d -> p j d", p=128)
            for fc in range(nfc):
                s = kk % 2
                kk += 1
                f0 = fc * FC
                for c0 in range(0, DC, 4):
                    m.dma(m.POOL, wgt[s][:, c0:c0 + 4, :], wg_v[:, c0:c0 + 4, f0:f0 + FC], writes=[wg_b[s]])
                    m.dma(m.POOL, wut[s][:, c0:c0 + 4, :], wu_v[:, c0:c0 + 4, f0:f0 + FC], writes=[wu_b[s]])
                m.dma(m.POOL, wdt[s][:, :, :], wd_v[:, f0 // 128: f0 // 128 + FC // 128, :], writes=[wd_b[s]])
                for fb in range(FC // 128):
                    p = gi % 2
                    gi += 1
                    def fgate():
                        ins = None
                        for c in range(DC):
                            ins = nc.tensor.matmul(pg[p][:, :C0], lhsT=wgt[s][:, c, fb * 128:(fb + 1) * 128], rhs=xgT[:, c, :],
                                                   start=(c == 0), stop=(c == DC - 1))
                        return ins
                    m.op(m.PE, fgate, reads=[wg_b[s], xgT_b], writes=[pg_b[p]])
                    def fup():
                        ins = None
                        for c in range(DC):
                            ins = nc.tensor.matmul(pu[p][:, :C0], lhsT=wut[s][:, c, fb * 128:(fb + 1) * 128], rhs=xgT[:, c, :],
                                                   start=(c == 0), stop=(c == DC - 1))
                        return ins
                    m.op(m.PE, fup, reads=[wu_b[s], xgT_b], writes=[pu_b[p]])
                    m.op(m.ACT, lambda: nc.scalar.activation(out=gs[p][:, :], in_=pg[p][:, :C0], func=AF.Silu),
                         reads=[pg_b[p]], writes=[gs_b[p]])
                    m.op(m.DVE, lambda: nc.vector.tensor_tensor(out=hid[s][:, fb, :], in0=gs[p][:, :], in1=pu[p][:, :C0], op=ALU.mult),
                         reads=[gs_b[p], pu_b[p]], writes=[hid_b[s]])
                for jb in range(NJB):
                    for half in range(2):
                        p = gi % 2
                        gi += 1
                        def fdown():
                            ins = None
                            for q in range(2):
                                for j in range(FC // 128):
                                    ins = nc.tensor.matmul(py[p][:, q * 512:(q + 1) * 512], lhsT=hid[s][:, j, jb * 128:(jb + 1) * 128],
                                                           rhs=wdt[s][:, j, half * 1024 + q * 512: half * 1024 + (q + 1) * 512],
                                                           start=(j == 0), stop=(j == FC // 128 - 1))
                            return ins
                        m.op(m.PE, fdown, reads=[hid_b[s], wd_b[s]], writes=[py_b[p]])
                        ya = yea[:, jb, half * 1024:(half + 1) * 1024]
                        if fc == 0:
                            m.op(m.DVE, lambda: nc.vector.tensor_copy(out=ya, in_=py[p][:, :]), reads=[py_b[p]], writes=[yea_b[jb]])
                        else:
                            m.op(m.DVE, lambda: nc.vector.scalar_tensor_tensor(out=ya, in0=py[p][:, :], scalar=1.0, in1=ya,
                                                                               op0=ALU.mult, op1=ALU.add),
                                 reads=[py_b[p]], writes=[yea_b[jb]])
            for jb in range(NJB):
                if jb % 2 == 0:
                    m.op(m.ACT, lambda: nc.scalar.copy(out=yeb[:, jb, :], in_=yea[:, jb, :]), reads=[yea_b[jb]], writes=[yeb_b])
                else:
                    m.op(m.DVE, lambda: nc.vector.tensor_copy(out=yeb[:, jb, :], in_=yea[:, jb, :]), reads=[yea_b[jb]], writes=[yeb_b])
            for tb in range(NB):
                for half in range(2):
                    p = gi % 2
                    gi += 1
                    def fsc():
                        ins = None
                        for q in range(2):
                            for jb in range(NJB):
                                ins = nc.tensor.matmul(py[p][:, q * 512:(q + 1) * 512], lhsT=PTe[:, jb, tb * 128:(tb + 1) * 128],
                                                       rhs=yeb[:, jb, half * 1024 + q * 512: half * 1024 + (q + 1) * 512],
                                                       start=(jb == 0), stop=(jb == NJB - 1))
                        return ins
                    m.op(m.PE, fsc, reads=[PTe_b, yeb_b], writes=[py_b[p]])
                    ya = yacc[:, tb, half * 1024:(half + 1) * 1024]
                    if e == 0:
                        m.op(m.DVE, lambda: nc.vector.tensor_scalar(out=ya, in0=py[p][:, :], scalar1=gate_tile[:, tb, e:e + 1],
                                                                    scalar2=None, op0=ALU.mult),
                             reads=[py_b[p], gb], writes=[yacc_bufs[tb]])
                    else:
                        m.op(m.DVE, lambda: nc.vector.scalar_tensor_tensor(out=ya, in0=py[p][:, :], scalar=gate_tile[:, tb, e:e + 1],
                                                                           in1=ya, op0=ALU.mult, op1=ALU.add),
                             reads=[py_b[p], gb], writes=[yacc_bufs[tb]])
        m.barrier()
        esw.close()
        ln_setup(m, cx, es, tag, I["ln_ffn_g1"], I["ln_ffn_b1"], [H3_b], out_b)
        for bi, (t0, n) in enumerate(blocks):
            layer_norm_block(m, cx, yacc[:n, bi, :], [yacc_bufs[bi]], H3, t0, n, out, cx.gam, cx.bet)
        m.barrier()


REST_INPUTS = [("ffn_w_gate", (D, FFN_DENSE)), ("ffn_w_up", (D, FFN_DENSE)), ("ffn_w_down", (FFN_DENSE, D)),
               ("ln_ffn_g0", (D,)), ("ln_ffn_b0", (D,)),
               ("pool_w_in", (D, D)), ("pool_w_group", (4, 512, 512)), ("pool_scale", (D,)), ("pool_w_out", (D, D)),
               ("ln_mix_g1", (D,)), ("ln_mix_b1", (D,)),
               ("moe_w_router", (D, NEXP)), ("moe_b_router", (NEXP,)),
               ("moe_w_gate", (NEXP, D, FFN_EXPERT)), ("moe_w_up", (NEXP, D, FFN_EXPERT)), ("moe_w_down", (NEXP, FFN_EXPERT, D)),
               ("ln_ffn_g1", (D,)), ("ln_ffn_b1", (D,)), ("slot_iota", (MOE_CAP,))]


def build_full(stop_after=None):
    m = MK()
    nc = m.nc
    I = declare_inputs(nc, L0_INPUTS + REST_INPUTS)
    out = nc.dram_tensor("out", [NOWN, D], F32, kind="ExternalOutput").ap()
    out_b = m.buf()
    S = make_scratch(m)
    layer0_mixer(m, I, S)
    swiglu_phase(m, "f0", S["H1"], S["H1_b"], 0, NT, I["ffn_w_gate"], I["ffn_w_up"], I["ffn_w_down"], FFN_DENSE,
                 I["ln_ffn_g0"], I["ln_ffn_b0"], S["H2"], S["H2_b"])
    persist(m, S)
    pool_phase(m, I, S)
    if USE_COMPACT_MOE:
        moe_phase(m, I, S, out, out_b)
    else:
        swiglu_phase(m, "f1", S["H3"], S["H3_b"], 0, NOWN, I["moe_w_gate"], I["moe_w_up"], I["moe_w_down"], FFN_EXPERT,
                     I["ln_ffn_g1"], I["ln_ffn_b1"], out, out_b, gates=make_moe_gates(m, I, S), n_exp=NEXP)
    m.SP.wait(m.all_tokens())
    return m


def host_prep_all(inp):
    maps = host_prep_l0(inp)
    g = lambda k: np.asarray(inp[k], np.float32)
    common = {
        "ffn_w_gate": g("ffn_w_gate")[0], "ffn_w_up": g("ffn_w_up")[0], "ffn_w_down": g("ffn_w_down")[0],
        "ln_ffn_g0": g("ln_ffn_g")[0], "ln_ffn_b0": g("ln_ffn_b")[0],
        "pool_w_in": g("pool_w_in")[0], "pool_w_group": g("pool_w_group")[0], "pool_scale": g("pool_scale")[0],
        "pool_w_out": g("pool_w_out")[0],
        "ln_mix_g1": g("ln_mix_g")[1], "ln_mix_b1": g("ln_mix_b")[1],
        "moe_w_router": g("moe_w_router")[0], "moe_b_router": g("moe_b_router")[0],
        "moe_w_gate": g("moe_w_gate")[0], "moe_w_up": g("moe_w_up")[0], "moe_w_down": g("moe_w_down")[0],
        "ln_ffn_g1": g("ln_ffn_g")[1], "ln_ffn_b1": g("ln_ffn_b")[1],
        "slot_iota": np.arange(MOE_CAP, dtype=np.float32),
    }
    for d in maps:
        d.update(common)
    return maps


_NC_CACHE = {}


def kernel(**inputs):
    if "m" not in _NC_CACHE:
        _NC_CACHE["m"] = build_full()
    m = _NC_CACHE["m"]
    maps = host_prep_all(inputs)
    res = run_bass_kernel_spmd(m.nc, maps, core_ids=list(range(NCORE)))
    out = np.concatenate([np.asarray(res.results[c]["out"], np.float32) for c in range(NCORE)], axis=0)
    return out.reshape(1, NCORE * NOWN, D)
```
